# Optimizing a Trainium2 kernel written in Bass

```python
import math
import jax, jax.numpy as jnp
from jax import lax
import numpy as np

D_MODEL = 1024
BATCH = 32
SEQ = 2048
DEPTH = 1
DEC_BATCH = 32
DEC_SEQ = 64
PAST_LEN = 1024

CHUNK = 64
N_MEM = 256
EPS = 1e-6
HG_HEADS = 8
HG_KD = 128
HG_VD = 128
HG_WIDTH = HG_HEADS * HG_VD
GLA_BLOCK = 16
DF_HEADS = 8
DF_HD = 64
DF_VD = 2 * DF_HD
DF_WIDTH = DF_HEADS * DF_VD
Q_BLOCK = 128
MEM_HEADS = 4
MEM_HD = 256
MEM_WIDTH = MEM_HEADS * MEM_HD
N_BRANCH = 3
BR_WIDTH = HG_WIDTH
COL_SIZES = (HG_HEADS * HG_KD, HG_HEADS * HG_KD, HG_WIDTH, HG_WIDTH,
             DF_HEADS * 2 * DF_HD, DF_HEADS * 2 * DF_HD, DF_WIDTH, DF_WIDTH,
             MEM_WIDTH, MEM_WIDTH, N_BRANCH * D_MODEL)
IN_COLS = sum(COL_SIZES)

kernel_name = 'hgrn2_diffattn_gated_streaming_encoder_step'


def rms_norm(x, g):
    xf = x.astype(jnp.float32)
    y = xf * lax.rsqrt(jnp.mean(xf * xf, axis=-1, keepdims=True) + EPS)
    return (y * g.astype(jnp.float32)).astype(x.dtype)


def split_columns(a):
    bounds = tuple(int(b) for b in np.cumsum(COL_SIZES)[:-1])
    return jnp.split(a, bounds, axis=-1)


def hgrn2_recurrence(q, k, v, logf, s0):
    B, H, T, _ = q.shape
    pad = (-T) % GLA_BLOCK
    if pad:
        pw = ((0, 0), (0, 0), (0, pad), (0, 0))
        q, k, v, logf = (jnp.pad(a, pw) for a in (q, k, v, logf))
    nb = (T + pad) // GLA_BLOCK

    def to_blocks(a):
        return jnp.moveaxis(a.reshape(B, H, nb, GLA_BLOCK, a.shape[-1]), 2, 0)

    causal = jnp.tril(jnp.ones((GLA_BLOCK, GLA_BLOCK), dtype=bool))

    def step(s, blk):
        qb, kb, vb, gb = blk
        b = jnp.cumsum(gb, axis=2)
        b_end = b[:, :, -1:, :]
        q_dec = qb * jnp.exp(b)
        k_inv = kb * jnp.exp(-b)
        att = jnp.where(causal, jnp.einsum('bhtk,bhsk->bhts', q_dec, k_inv), 0.0)
        o = jnp.einsum('bhts,bhsv->bhtv', att, vb) + jnp.einsum('bhtk,bhkv->bhtv', q_dec, s)
        s = (jnp.exp(b_end[:, :, 0, :])[..., None] * s
             + jnp.einsum('bhsk,bhsv->bhkv', kb * jnp.exp(b_end - b), vb))
        return s, o

    s_T, o = lax.scan(step, s0, tuple(to_blocks(a) for a in (q, k, v, logf)))
    o = jnp.moveaxis(o, 0, 2).reshape(B, H, nb * GLA_BLOCK, -1)[:, :, :T]
    return o, s_T


def diff_softmax_pair(q, k, v, lam, mask):
    s = jnp.einsum('bqhcd,bshcd->bhcqs', q, k).astype(jnp.float32) * (DF_HD ** -0.5)
    if mask is not None:
        s = jnp.where(mask, s, -jnp.inf)
    p = jax.nn.softmax(s, axis=-1)
    a = p[:, :, 0] - lam * p[:, :, 1]
    return jnp.einsum('bhqs,bshv->bqhv', a.astype(v.dtype), v)


def diff_attention_prompt(q, k, v, lam):
    B, T = q.shape[:2]
    nqb = T // Q_BLOCK
    q_blocks = jnp.moveaxis(q.reshape(B, nqb, Q_BLOCK, DF_HEADS, 2, DF_HD), 1, 0)
    k_chunk = jnp.arange(T) // CHUNK

    def one_block(args):
        qi, i = args
        q_chunk = (i * Q_BLOCK + jnp.arange(Q_BLOCK)) // CHUNK
        mask = k_chunk[None, :] <= q_chunk[:, None]
        return diff_softmax_pair(qi, k, v, lam, mask)

    o = lax.map(one_block, (q_blocks, jnp.arange(nqb)))
    return jnp.moveaxis(o, 0, 1).reshape(B, T, DF_HEADS, DF_VD)


def memory_kv(mem, g_mem, w_mkv, g_mk):
    B, N, _ = mem.shape
    k, v = jnp.split(rms_norm(mem, g_mem) @ w_mkv, 2, axis=-1)
    k = rms_norm(k.reshape(B, N, MEM_HEADS, MEM_HD), g_mk)
    return k, v.reshape(B, N, MEM_HEADS, MEM_HD)


def memory_attention(q, k, v):
    s = jnp.einsum('bthd,bnhd->bhtn', q, k).astype(jnp.float32) * (MEM_HD ** -0.5)
    p = jax.nn.softmax(s, axis=-1)
    return jnp.einsum('bhtn,bnhd->bthd', p.astype(v.dtype), v)


def encoder_layer(x, s0, past_k, past_v, mem_k, mem_v, lb, lam_init,
                  g_norm, w_in, g_hg_out, g_dq, g_dk, lam_q1, lam_k1, lam_q2, lam_k2,
                  g_dsub, g_mq, w_branch, w_out):
    B, T, _ = x.shape
    f32 = jnp.float32
    xn = rms_norm(x, g_norm)
    hq, hf, hi, za, dq, dk, dv, zb, mq, zm, gr = split_columns(xn @ w_in)

    hf = hf.astype(f32)
    logf = jnp.log(lb + (1.0 - lb) * jax.nn.sigmoid(hf))
    hk = (1.0 - lb) * jax.nn.sigmoid(-hf)

    def heads(a):
        return a.astype(f32).reshape(B, T, HG_HEADS, -1).transpose(0, 2, 1, 3)

    o_hg, s_new = hgrn2_recurrence(heads(hq), heads(hk), heads(hi), heads(logf), s0.astype(f32))
    y_a = rms_norm(o_hg.transpose(0, 2, 1, 3), g_hg_out).reshape(B, T, HG_WIDTH).astype(x.dtype)

    q = rms_norm(dq.reshape(B, T, DF_HEADS, 2, DF_HD), g_dq)
    k = rms_norm(dk.reshape(B, T, DF_HEADS, 2, DF_HD), g_dk)
    v = dv.reshape(B, T, DF_HEADS, DF_VD)
    lam = (jnp.exp(jnp.sum(lam_q1.astype(f32) * lam_k1.astype(f32)))
           - jnp.exp(jnp.sum(lam_q2.astype(f32) * lam_k2.astype(f32))) + lam_init)
    if past_k is None:
        o_df = diff_attention_prompt(q, k, v, lam)
    else:
        k_all = jnp.concatenate([past_k.astype(k.dtype), k], axis=1)
        v_all = jnp.concatenate([past_v.astype(v.dtype), v], axis=1)
        o_df = diff_softmax_pair(q, k_all, v_all, lam, None)
    y_b = (rms_norm(o_df, g_dsub) * (1.0 - lam_init)).reshape(B, T, DF_WIDTH)

    q_m = rms_norm(mq.reshape(B, T, MEM_HEADS, MEM_HD), g_mq)
    y_m = memory_attention(q_m, mem_k.astype(x.dtype), mem_v.astype(x.dtype)).reshape(B, T, MEM_WIDTH)

    gates = jax.nn.sigmoid(gr.astype(f32)).astype(x.dtype).reshape(B, T, N_BRANCH, D_MODEL)
    h = 0
    for n, (y_n, z_n) in enumerate(((y_a, za), (y_b, zb), (y_m, zm))):
        h = h + gates[:, :, n] * ((y_n * jax.nn.silu(z_n)) @ w_branch[n])
    y = x + h @ w_out
    return y, s_new, k, v


def setup_inputs(seed: int = 0) -> dict:
    key = jax.random.key(seed)
    ks = jax.random.split(key, 26)

    def nrm(k, shape, scale=1.0):
        return scale * jax.random.normal(k, shape, jnp.float32)

    def gain(k, shape):
        return 1.0 + 0.02 * jax.random.normal(k, shape, jnp.float32)

    return {
        'x_prompt': nrm(ks[0], (BATCH, SEQ, D_MODEL)),
        'x_sample': nrm(ks[1], (DEC_BATCH, DEC_SEQ, D_MODEL)),
        'mem_prompt': nrm(ks[2], (BATCH, N_MEM, D_MODEL)),
        'cache_diff_k': nrm(ks[3], (DEPTH, DEC_BATCH, PAST_LEN, DF_HEADS, 2, DF_HD)),
        'cache_diff_v': nrm(ks[4], (DEPTH, DEC_BATCH, PAST_LEN, DF_HEADS, DF_VD)),
        'cache_mem_k': nrm(ks[5], (DEPTH, DEC_BATCH, N_MEM, MEM_HEADS, MEM_HD)),
        'cache_mem_v': nrm(ks[6], (DEPTH, DEC_BATCH, N_MEM, MEM_HEADS, MEM_HD)),
        'state_hgrn': nrm(ks[7], (DEPTH, DEC_BATCH, HG_HEADS, HG_KD, HG_VD), 0.3),
        'g_norm': gain(ks[8], (DEPTH, D_MODEL)),
        'w_in': nrm(ks[9], (DEPTH, D_MODEL, IN_COLS), D_MODEL ** -0.5),
        'hg_lb_logits': nrm(ks[10], (DEPTH + 1, HG_HEADS * HG_KD), 0.1),
        'g_hg_out': gain(ks[11], (DEPTH, HG_VD)),
        'g_dq': gain(ks[12], (DEPTH, DF_HD)),
        'g_dk': gain(ks[13], (DEPTH, DF_HD)),
        'lam_q1': nrm(ks[14], (DEPTH, DF_HD), 0.1),
        'lam_k1': nrm(ks[15], (DEPTH, DF_HD), 0.1),
        'lam_q2': nrm(ks[16], (DEPTH, DF_HD), 0.1),
        'lam_k2': nrm(ks[17], (DEPTH, DF_HD), 0.1),
        'g_dsub': gain(ks[18], (DEPTH, DF_VD)),
        'g_mem': gain(ks[19], (DEPTH, D_MODEL)),
        'w_mkv': nrm(ks[20], (DEPTH, D_MODEL, 2 * MEM_WIDTH), D_MODEL ** -0.5),
        'g_mq': gain(ks[21], (DEPTH, MEM_HD)),
        'g_mk': gain(ks[22], (DEPTH, MEM_HD)),
        'w_branch': nrm(ks[23], (DEPTH, N_BRANCH, BR_WIDTH, D_MODEL), BR_WIDTH ** -0.5),
        'w_out': nrm(ks[24], (DEPTH, D_MODEL, D_MODEL), D_MODEL ** -0.5),
    }


def reference(x_prompt, x_sample, mem_prompt, cache_diff_k, cache_diff_v, cache_mem_k, cache_mem_v,
              state_hgrn, g_norm, w_in, hg_lb_logits, g_hg_out, g_dq, g_dk, lam_q1, lam_k1,
              lam_q2, lam_k2, g_dsub, g_mem, w_mkv, g_mq, g_mk, w_branch, w_out):
    lower_bounds = jnp.cumsum(jax.nn.softmax(hg_lb_logits.astype(jnp.float32), axis=0), axis=0)
    s0_prompt = jnp.zeros((x_prompt.shape[0], HG_HEADS, HG_KD, HG_VD), jnp.float32)
    h_p, h_s = x_prompt, x_sample
    st_p, st_s, dk_p, dv_p, dk_s, dv_s, mk_p, mv_p = ([] for _ in range(8))
    for l in range(DEPTH):
        lam_init = 0.8 - 0.6 * math.exp(-0.3 * l)
        shared = (lower_bounds[l], lam_init, g_norm[l], w_in[l], g_hg_out[l], g_dq[l], g_dk[l],
                  lam_q1[l], lam_k1[l], lam_q2[l], lam_k2[l], g_dsub[l], g_mq[l], w_branch[l], w_out[l])
        mem_k, mem_v = memory_kv(mem_prompt, g_mem[l], w_mkv[l], g_mk[l])
        h_p, s_p, k_p, v_p = encoder_layer(h_p, s0_prompt, None, None, mem_k, mem_v, *shared)
        h_s, s_s, k_s, v_s = encoder_layer(h_s, state_hgrn[l], cache_diff_k[l], cache_diff_v[l],
                                           cache_mem_k[l], cache_mem_v[l], *shared)
        st_p.append(s_p)
        st_s.append(s_s)
        dk_p.append(k_p)
        dv_p.append(v_p)
        dk_s.append(k_s)
        dv_s.append(v_s)
        mk_p.append(mem_k)
        mv_p.append(mem_v)
    return (h_p, h_s, jnp.stack(st_p), jnp.stack(st_s), jnp.stack(dk_p), jnp.stack(dv_p),
            jnp.stack(dk_s), jnp.stack(dv_s), jnp.stack(mk_p), jnp.stack(mv_p))
```

```python
import contextlib
import numpy as np
import concourse.bass as bass
import concourse.mybir as mybir
from concourse.bass_utils import run_bass_kernel_spmd
from concourse.alu_op_type import AluOpType as ALU

F32 = mybir.dt.float32
BF16 = mybir.dt.bfloat16
AF = mybir.ActivationFunctionType
AX = mybir.AxisListType
ENGS = ('pe', 'act', 'dve', 'pool', 'sp')
EPS = 1e-6
NB = 4
SEQ = 2048
NPIECE = 92
DBG_BR = {0, 1, 2}
PIPE_DEPTH = 3
STORE_DELAY = 2
CONV_AHEAD = 3
DBG_CLOSURE = False
DBG_MAXOPS = None
DBG_V = 0
DBG_SKIP = set()
CLOSURE_SNAPS = []


class Buf:
    __slots__ = ('name', 'w', 'r', 'psum')

    def __init__(self, name, psum=False):
        self.name = name
        self.w = None
        self.r = {}
        self.psum = psum


class Op:
    __slots__ = ('eng', 'fn', 'deps', 'inc', 'val', 'sem', 'dma')


def _flat(x):
    out = []
    for b in x:
        if b is None:
            continue
        if isinstance(b, Buf):
            out.append(b)
        else:
            out.extend(_flat(b))
    return out


class Rec:
    def __init__(self):
        self.q = {e: [] for e in ENGS}
        self.dma_keys = {}

    def op(self, eng, fn, reads=(), writes=(), dma_key=None):
        self.nrec = getattr(self, 'nrec', 0) + 1
        if DBG_MAXOPS is not None and self.nrec > DBG_MAXOPS:
            return None
        if self.nrec in DBG_SKIP:
            return None
        if DBG_MAXOPS is not None and self.nrec == DBG_MAXOPS:
            print("LAST OP", eng, "line", fn.__code__.co_firstlineno)
        o = Op()
        o.eng = eng
        o.fn = fn
        o.inc = False
        o.val = None
        o.sem = None
        o.dma = dma_key
        if DBG_CLOSURE and fn.__closure__:
            o_snap = [id(c.cell_contents) for c in fn.__closure__]
            CLOSURE_SNAPS.append((fn, o_snap))
        deps = {}
        rl = _flat(reads)
        wl = _flat(writes)
        for b in rl:
            if b.w is not None:
                deps[id(b.w)] = b.w
            if b.psum:
                for k_, d in b.r.items():
                    if k_ != eng:
                        deps[id(d)] = d
        for b in wl:
            if b.w is not None:
                deps[id(b.w)] = b.w
            for d in b.r.values():
                deps[id(d)] = d
        if dma_key is not None:
            ent = self.dma_keys.setdefault(dma_key, [0, None])
            if ent[1] is not None:
                deps[id(ent[1])] = ent[1]
            ent[0] += 1
            ent[1] = o
            o.val = 16 * ent[0]
        dl = []
        for d in deps.values():
            if d is o:
                continue
            if d.dma is None and d.eng == 'pe' and eng == 'pe' and dma_key is None:
                continue
            d.inc = True
            dl.append(d)
        o.deps = dl
        rkey = eng if dma_key is None else ('dma', dma_key)
        for b in rl:
            b.r[rkey] = o
        for b in wl:
            b.w = o
            b.r = {}
        self.q[eng].append(o)
        return o

    def emit(self, nc, stack):
        esem = {e: stack.enter_context(nc.semaphore("sem_" + e)) for e in ENGS}
        dsem = {k: stack.enter_context(nc.semaphore("dsem_%d" % i)) for i, k in enumerate(self.dma_keys)}
        for e in ENGS:
            c = 0
            for o in self.q[e]:
                if o.dma is not None:
                    o.sem = dsem[o.dma]
                else:
                    o.sem = esem[e]
                    if o.inc:
                        c += 1
                        o.val = c
        engobj = {'pe': 'tensor', 'act': 'scalar', 'dve': 'vector', 'pool': 'gpsimd', 'sp': 'sync'}
        fin = [(dsem[k], 16 * v[0]) for k, v in self.dma_keys.items()]
        block = stack.enter_context(nc.Block())
        for e in ENGS:
            q = self.q[e]

            def body(eng, q=q, e=e):
                waited = {}
                for o in q:
                    for d in o.deps:
                        k = id(d.sem)
                        if waited.get(k, 0) < d.val:
                            eng.wait_ge(d.sem, d.val)
                            waited[k] = d.val
                    inst = o.fn(eng)
                    if o.dma is not None:
                        inst.then_inc(o.sem, 16)
                    elif o.inc:
                        inst.then_inc(o.sem, 1)
                if e == 'sp':
                    for s, v in fin:
                        eng.wait_ge(s, v)
            getattr(block, engobj[e])(body)


class Reg:
    def __init__(self, ar, p0, n):
        self.ar = ar
        self.p0 = p0
        self.n = n

    def bf(self, c0=0, c1=None):
        c1 = self.n * 512 if c1 is None else c1
        return self.ar.t[:, self.p0 * 512 + c0:self.p0 * 512 + c1]

    def f(self, c0=0, c1=None):
        c1 = self.n * 256 if c1 is None else c1
        return self.ar.tf[:, self.p0 * 256 + c0:self.p0 * 256 + c1]

    def rows_bf(self, r0, r1, c0, c1):
        return self.ar.t[r0:r1, self.p0 * 512 + c0:self.p0 * 512 + c1]

    @property
    def b(self):
        return self.ar.bufs[self.p0:self.p0 + self.n]

    def bb(self, c0, c1):
        return self.ar.bufs[self.p0 + c0 // 512:self.p0 + (c1 - 1) // 512 + 1]

    def bF(self, c0, c1):
        return self.ar.bufs[self.p0 + c0 // 256:self.p0 + (c1 - 1) // 256 + 1]

    def free(self):
        self.ar.release(self)


class Arena:
    def __init__(self, t, n):
        self.t = t
        self.tf = t.bitcast(F32)
        self.n = n
        self.used = [False] * n
        self.bufs = [Buf("a%d" % i) for i in range(n)]
        self.ptr = 0
        self.peak = 0

    def alloc(self, k):
        n = self.n
        for s in range(n):
            p = (self.ptr + s) % n
            if p + k > n:
                continue
            if not any(self.used[p:p + k]):
                for i in range(p, p + k):
                    self.used[i] = True
                self.ptr = (p + k) % n
                self.peak = max(self.peak, sum(self.used))
                return Reg(self, p, k)
        raise RuntimeError("arena full: want %d, used %d" % (k, sum(self.used)))

    def release(self, r):
        for i in range(r.p0, r.p0 + r.n):
            assert self.used[i]
            self.used[i] = False


def bview(ap, g, j):
    return ap.rearrange("p (g j) -> p g j", g=g, j=j)


def bcast_mid(ap, g):
    return ap.unsqueeze(1).to_broadcast([ap.shape[0], g, ap.shape[1]])


def bcast_last(ap, j):
    return ap.unsqueeze(2).to_broadcast([ap.shape[0], ap.shape[1], j])


def build_program(nb_run=NB, ntiles=4, do_sample=True):
    nc = bass.Bass("TRN2", target_bir_lowering=False)
    R = Rec()

    def din(name, shape):
        return nc.dram_tensor(name, shape, F32, kind="ExternalInput").ap()

    def dout(name, shape):
        return nc.dram_tensor(name, shape, F32, kind="ExternalOutput").ap()

    xp = din("xp", [NB * SEQ, 1024])
    xsm = din("xsm", [256, 1024])
    mp = din("mp", [NB * 256, 1024])
    cdk = din("cdk", [NB * 1024, 1024])
    cdv = din("cdv", [NB * 1024, 1024])
    cmk = din("cmk", [NB * 256, 1024])
    cmv = din("cmv", [NB * 256, 1024])
    st0 = din("st0", [NB, 8, 128, 128])
    w_in = din("w_in", [1024, 13312])
    w_mkv = din("w_mkv", [1024, 2048])
    w_br = din("w_br", [3072, 1024])
    w_out = din("w_out", [1024, 1024])
    cpk = din("cpk", [128, 40])
    cbc = din("cbc", [128, 576])
    cfix = din("cfix", [128, 320])

    y_p = dout("y_p", [NB * SEQ, 1024])
    y_s = dout("y_s", [256, 1024])
    st_p = dout("st_p", [NB, 8, 128, 128])
    st_s = dout("st_s", [NB, 8, 128, 128])
    dk_p = dout("dk_p", [NB * SEQ, 1024])
    dv_p = dout("dv_p", [NB * SEQ, 1024])
    dk_s = dout("dk_s", [256, 1024])
    dv_s = dout("dv_s", [256, 1024])
    mk_p = dout("mk_p", [NB * 256, 1024])
    mv_p = dout("mv_p", [NB * 256, 1024])

    wb_in = nc.dram_tensor("wb_in", [1024, 13312], BF16, kind="Internal").ap()
    wb_mkv = nc.dram_tensor("wb_mkv", [1024, 2048], BF16, kind="Internal").ap()
    wb_br = nc.dram_tensor("wb_br", [3072, 1024], BF16, kind="Internal").ap()
    wb_out = nc.dram_tensor("wb_out", [1024, 1024], BF16, kind="Internal").ap()
    B_wb = {'in': Buf("wb_in"), 'mkv': Buf("wb_mkv"), 'br': Buf("wb_br"), 'out': Buf("wb_out")}

    with contextlib.ExitStack() as st:
        def sb(name, shape, dt):
            return st.enter_context(nc.sbuf_tensor(name, shape, dt))

        KT = sb("KT", [128, 8 * SEQ], BF16)
        B_KT = [[Buf("KT%d_%d" % (h, j)) for j in range(4)] for h in range(8)]
        VA = sb("VA", [128, 16 * 1040], BF16)
        B_VA = [Buf("VA%d" % i) for i in range(16)]
        KM = sb("KM", [128, 8 * 256], BF16)
        B_KM = Buf("KM")
        VM = sb("VM", [128, 2 * 1032], BF16)
        B_VM = [Buf("VM0"), Buf("VM1")]
        WS = [sb("ws%d" % i, [128, 8 * 512], BF16) for i in range(3)]
        B_WS = [Buf("ws%d" % i) for i in range(3)]
        SF = sb("SF", [128, 1024], F32)
        SB = sb("SB", [128, 1024], BF16)
        B_SF = [Buf("SF%d" % h) for h in range(8)]
        B_SB = [Buf("SB%d" % h) for h in range(8)]
        CPK = sb("CPK", [128, 40], F32)
        CBC = sb("CBC", [128, 576], F32)
        CFX = sb("CFX", [128, 192], F32)
        IDB = sb("IDB", [128, 128], BF16)
        MHG = sb("MHG", [128, 64], BF16)
        DER = sb("DER", [128, 64], F32)
        MH = sb("MH", [128, 16], F32)
        AEND = sb("AEND", [128, 64], F32)
        B_AEND = [Buf("AEND%d" % h) for h in range(8)]
        FM1 = [sb("FM1_%d" % i, [128, 512], F32) for i in range(2)]
        B_FM1 = [Buf("FM1_0"), Buf("FM1_1")]
        SM = sb("SM", [128, 512], F32)
        B_SM = [Buf("SM%d" % i) for i in range(32)]
        B_C = Buf("consts")
        art = sb("arena", [128, NPIECE * 512], BF16)
        A = Arena(art, NPIECE)
        PS = [st.enter_context(nc.psum_tensor("ps%d" % i, [128, 512], F32)) for i in range(8)]
        PSB = [p.bitcast(BF16) for p in PS]
        B_PS = [Buf("ps%d" % i, True) for i in range(8)]
        B_TR = [Buf("tr0", True), Buf("tr1", True)]
        state = {'bk': 0, 'wide': True, 'sm': 0, 'fm1': 0}

        def bank_next():
            bset = (0, 1, 2, 3, 4, 5, 6, 7) if state['wide'] else (0, 1, 2, 3)
            i = bset[state['bk'] % len(bset)]
            state['bk'] += 1
            return i

        def mm_next():
            i = bank_next()
            return PS[i], B_PS[i]

        def tr_next():
            i = bank_next()
            return PSB[i][:, 0:512], B_PS[i]

        def set_wide(w):
            state['wide'] = w

        def acc(i):
            return PS[4 + i], B_PS[4 + i]

        def sm_next():
            i = state['sm']
            state['sm'] = (i + 1) % 32
            return SM[:, i * 16:(i + 1) * 16], B_SM[i]

        C1, C0, NC1 = 0, 8, 16
        GHG, GQ8, GK2, GDS, LAMN = 24, 25, 26, 27, 28
        GMQ = 29
        GMKP = 31
        P_GN, P_GM, P_L0, P_L1, P_GHG, P_GQ, P_GK, P_GDS, P_GMQ, P_GMK = 0, 8, 16, 24, 32, 33, 34, 35, 36, 38

        R.op('sp', lambda e: e.dma_start(out=CPK[:], in_=cpk[:, :]), writes=[B_C], dma_key="c0")
        R.op('sp', lambda e: e.dma_start(out=CBC[:], in_=cbc[:, :]), writes=[B_C], dma_key="c1")
        R.op('sp', lambda e: e.dma_start(out=CFX[:, 0:128], in_=cfix[:, 192:320]), writes=[B_C], dma_key="c2")
        R.op('pool', lambda e: e.dma_start(out=IDB[:], in_=cfix[:, 0:128]), writes=[B_C], dma_key="c3")
        R.op('pool', lambda e: e.dma_start(out=MHG[:], in_=cfix[:, 128:192]), writes=[B_C], dma_key="c4")
        R.op('pool', lambda e: e.memset(MH[:], -0.5), writes=[B_C])
        for i in range(2):
            R.op('pool', lambda e, i=i: e.memset(FM1[i][:], 0.0), writes=[B_FM1[i]])
        R.op('pool', lambda e: e.memset(AEND[:], 1.0), writes=B_AEND)
        R.op('dve', lambda e: e.tensor_tensor(out=DER[:, 32:40], in0=CPK[:, P_L0:P_L0 + 8], in1=CPK[:, P_L1:P_L1 + 8],
                                              op=ALU.subtract), reads=[B_C], writes=[B_C])
        R.op('act', lambda e: e.activation(out=DER[:, 40:48], in_=DER[:, 32:40], func=AF.Tanh, scale=0.5),
             reads=[B_C], writes=[B_C])
        R.op('dve', lambda e: e.tensor_scalar(out=DER[:, C1:C1 + 8], in0=DER[:, 40:48], scalar1=-0.25, scalar2=0.25,
                                              op0=ALU.mult, op1=ALU.add), reads=[B_C], writes=[B_C])
        R.op('dve', lambda e: e.tensor_scalar(out=DER[:, C0:C0 + 8], in0=DER[:, 40:48], scalar1=0.25, scalar2=0.75,
                                              op0=ALU.mult, op1=ALU.add), reads=[B_C], writes=[B_C])
        R.op('dve', lambda e: e.tensor_scalar(out=DER[:, NC1:NC1 + 8], in0=DER[:, 40:48], scalar1=0.25, scalar2=-0.25,
                                              op0=ALU.mult, op1=ALU.add), reads=[B_C], writes=[B_C])

        def cscale(dst, src, n, s):
            R.op('dve', lambda e: e.tensor_scalar(out=DER[:, dst:dst + n], in0=CPK[:, src:src + n], scalar1=s,
                                                  scalar2=None, op0=ALU.mult), reads=[B_C], writes=[B_C])
        cscale(GHG, P_GHG, 1, 0.5)
        cscale(GQ8, P_GQ, 1, 0.125)
        cscale(GK2, P_GK, 1, 1.0)
        cscale(GDS, P_GDS, 1, 0.4)
        cscale(GMQ, P_GMQ, 2, 1.0 / 16.0)
        cscale(GMKP, P_GMK, 2, 1.0)
        R.op('dve', lambda e: e.tensor_tensor(out=DER[:, 48:64].bitcast(F32), in0=CBC[:, 0:16], in1=CBC[:, 0:16], op=ALU.mult),
             reads=[B_C], writes=[B_C])
        sc1 = A.alloc(1)
        R.op('dve', lambda e: e.tensor_tensor(out=sc1.f(0, 64), in0=CBC[:, 0:64], in1=CBC[:, 64:128], op=ALU.mult),
             reads=[B_C], writes=sc1.b)
        R.op('dve', lambda e: e.tensor_tensor(out=sc1.f(64, 128), in0=CBC[:, 128:192], in1=CBC[:, 192:256], op=ALU.mult),
             reads=[B_C], writes=sc1.b)
        R.op('dve', lambda e: e.tensor_reduce(out=DER[:, 48:50], in_=bview(sc1.f(0, 128), 2, 64), axis=AX.X, op=ALU.add),
             reads=sc1.b, writes=[B_C])
        R.op('act', lambda e: e.activation(out=DER[:, 50:52], in_=DER[:, 48:50], func=AF.Exp), reads=[B_C], writes=[B_C])
        R.op('dve', lambda e: e.scalar_tensor_tensor(out=DER[:, LAMN:LAMN + 1], in0=DER[:, 51:52], scalar=-0.2,
                                                     in1=DER[:, 50:51], op0=ALU.add, op1=ALU.subtract),
             reads=[B_C], writes=[B_C])
        sc1.free()
        for blk in range(16):
            R.op('pool', lambda e, blk=blk: e.memset(
                bview(VA[:, blk * 1040:(blk + 1) * 1040], 8, 130)[:, :, 128:129], 1.0), writes=[B_VA[blk]])
        for kb in range(2):
            R.op('pool', lambda e, kb=kb: e.memset(
                bview(VM[:, kb * 1032:(kb + 1) * 1032], 4, 258)[:, :, 256:257], 1.0), writes=[B_VM[kb]])

        def col(c):
            return DER[:, c:c + 1]

        WSRC = {'in': wb_in, 'mkv': wb_mkv, 'br': wb_br, 'out': wb_out}
        plan = []

        def plan_tile(kind):
            p = []
            if kind == 'mem':
                p += [('mkv', 0, c) for c in range(0, 2048, 512)]
                return p
            p += [('in', 0, c) for c in range(0, 4096, 512)]
            for n in range(3):
                if n == 1:
                    for g in range(2):
                        p += [('in', 0, 4096 + g * 512), ('in', 0, 5120 + g * 512),
                              ('in', 0, 6144 + g * 512), ('in', 0, 7168 + g * 512)]
                if n == 2:
                    p += [('in', 0, c) for c in range(8192, 10240, 512)]
                p += [('in', 0, 10240 + n * 1024 + g * 512) for g in range(2)]
                p += [('br', n * 1024, g * 512) for g in range(2)]
            p += [('out', 0, 0), ('out', 0, 512)]
            return p

        for b in range(nb_run):
            plan += plan_tile('mem')
            for j in range(ntiles):
                plan += plan_tile('main')
        if do_sample:
            plan += plan_tile('main')
        ws_state = {'issued': 0, 'cur': 0}

        WF32 = {'in': w_in, 'mkv': w_mkv, 'br': w_br, 'out': w_out}
        B_cvt = {}
        conv_state = {'next': 0}

        def conv_ensure(n):
            while conv_state['next'] < min(n, len(plan)):
                key = plan[conv_state['next']]
                conv_state['next'] += 1
                if key in B_cvt:
                    continue
                src, r0, c0 = key
                B_cvt[key] = Buf("cvt_%s_%d_%d" % key)
                R.op('pool', lambda e, src=src, r0=r0, c0=c0: e.dma_start(
                    out=WSRC[src][r0:r0 + 1024, c0:c0 + 512], in_=WF32[src][r0:r0 + 1024, c0:c0 + 512]),
                    writes=[B_cvt[key]], dma_key="wc%d" % (len(B_cvt) % 2))

        def ws_issue_upto(n):
            while ws_state['issued'] < min(n, len(plan)):
                i = ws_state['issued']
                conv_ensure(i + 1 + CONV_AHEAD)
                src, r0, c0 = plan[i]
                s = i % 3
                srcap = WSRC[src][r0:r0 + 1024, c0:c0 + 512].rearrange("(kc p) c -> p kc c", p=128)
                R.op('sp', lambda e, s=s, srcap=srcap: e.dma_start(
                    out=WS[s][:].rearrange("p (kc c) -> p kc c", kc=8), in_=srcap),
                    reads=[B_cvt[plan[i]]], writes=[B_WS[s]], dma_key="w%d" % s)
                ws_state['issued'] += 1

        pending = []

        def defer(fn, n=STORE_DELAY):
            pending.append([n, fn])

        def flush_pending(everything=False):
            for it in list(pending):
                it[0] -= 1
                if everything or it[0] <= 0:
                    pending.remove(it)
                    it[1]()

        def ws_take(src, r0, c0):
            i = ws_state['cur']
            assert plan[i] == (src, r0, c0), (i, plan[i], (src, r0, c0))
            ws_issue_upto(i + 3)
            flush_pending()
            ws_state['cur'] += 1
            s = i % 3
            return WS[s], B_WS[s]

        def wcol(wt, kc, c0, c1):
            return wt[:, kc * 512 + c0:kc * 512 + c1]

        def rstd_into(dst_ap, dst_b, ss_ap, ss_b, n_groups, inv_n):
            tmp, tb_ = sm_next()
            R.op('dve', lambda e: e.tensor_scalar(out=tmp[:, 0:n_groups], in0=ss_ap, scalar1=inv_n, scalar2=EPS,
                                                  op0=ALU.mult, op1=ALU.add), reads=[ss_b], writes=[tb_])
            R.op('pool', lambda e: e.tensor_tensor(out=dst_ap, in0=tmp[:, 0:n_groups], in1=MH[:, 0:n_groups], op=ALU.pow),
                 reads=[tb_, B_C], writes=[dst_b])

        def group_stats(ps_ap, ps_b, g, j):
            junk = A.alloc(1)
            R.op('act', lambda e: e.activation(out=junk.bf(0, g * j), in_=ps_ap, func=AF.Square), reads=[ps_b], writes=junk.b)
            ss, ssb = sm_next()
            R.op('dve', lambda e: e.tensor_reduce(out=ss[:, 0:g], in_=bview(junk.bf(0, g * j), g, j), axis=AX.X, op=ALU.add),
                 reads=junk.b, writes=[ssb])
            junk.free()
            r, rb = sm_next()
            rstd_into(r[:, 0:g], rb, ss[:, 0:g], ssb, g, 1.0 / j)
            return r, rb

        def xstage_a(src_rows, TB):
            xn = []
            for tb in range(TB):
                xs = A.alloc(4)
                R.op('sp', lambda e, xs=xs, tb=tb: e.dma_start(out=xs.f(0, 1024), in_=src_rows(tb)), writes=xs.b,
                     dma_key="xin%d" % (tb % 2))
                junk = A.alloc(2)
                ss, ssb = sm_next()
                R.op('act', lambda e, xs=xs, junk=junk, ss=ss: e.activation(out=junk.bf(0, 1024), in_=xs.f(0, 1024),
                                                                           func=AF.Square, accum_out=ss[:, 0:1]),
                     reads=xs.b, writes=junk.b + [ssb])
                junk.free()
                r, rb = sm_next()
                rstd_into(r[:, 0:1], rb, ss[:, 0:1], ssb, 1, 1.0 / 1024)
                xb = A.alloc(2)
                R.op('act', lambda e, xs=xs, xb=xb, r=r: e.activation(out=xb.bf(0, 1024), in_=xs.f(0, 1024), func=AF.Copy,
                                                                      scale=r[:, 0:1]), reads=xs.b + [rb], writes=xb.b)
                xs.free()
                xn.append(xb)
            return xn

        def xstage_b(xn, TB, gcol):
            NT = TB * 128
            xnT = []
            for kc in range(8):
                tr, trb = tr_next()
                for tb in range(TB):
                    R.op('pe', lambda e, tr=tr, tb=tb, kc=kc: e.transpose(
                        tr[:, tb * 128:(tb + 1) * 128], xn[tb].bf(kc * 128, (kc + 1) * 128), IDB[:]),
                        reads=xn[tb].b + [B_C], writes=[trb])
                p = A.alloc(1)
                R.op('dve', lambda e, tr=tr, p=p, kc=kc: e.tensor_scalar(
                    out=p.bf(0, NT), in0=tr[:, 0:NT], scalar1=CPK[:, gcol + kc:gcol + kc + 1], scalar2=None, op0=ALU.mult),
                    reads=[trb, B_C], writes=p.b)
                xnT.append(p)
            for x_ in xn:
                x_.free()
            return xnT

        def fproj(src, r0, c0, rhs, rhs_bufs, NT, consume):
            wt, wb_ = ws_take(src, r0, c0)
            for ch in range(4):
                ps, psb_ = mm_next()
                for kc in range(8):
                    R.op('pe', lambda e, ps=ps, kc=kc, ch=ch: e.matmul(
                        ps[:, 0:NT], lhsT=wcol(wt, kc, ch * 128, (ch + 1) * 128), rhs=rhs[kc].bf(0, NT),
                        start=(kc == 0), stop=(kc == 7)),
                        reads=[wb_] + rhs_bufs[kc], writes=[psb_])
                consume(ch, ps, psb_)

        def tproj(src, r0, c0, xnT, TB, consume):
            wt, wb_ = ws_take(src, r0, c0)
            for tb in range(TB):
                ps, psb_ = mm_next()
                for kc in range(8):
                    R.op('pe', lambda e, ps=ps, kc=kc, tb=tb: e.matmul(
                        ps[:, 0:512], lhsT=xnT[kc].bf(tb * 128, (tb + 1) * 128), rhs=wcol(wt, kc, 0, 512),
                        start=(kc == 0), stop=(kc == 7)),
                        reads=[wb_] + xnT[kc].b, writes=[psb_])
                consume(tb, ps, psb_)

        def transpose_blocks(srcs, c0, TB, evac):
            tr, trb = tr_next()
            for tb in range(TB):
                R.op('pe', lambda e, tb=tb: e.transpose(tr[:, tb * 128:(tb + 1) * 128], srcs[tb].bf(c0, c0 + 128), IDB[:]),
                     reads=srcs[tb].bb(c0, c0 + 128) + [B_C], writes=[trb])
            evac(tr, trb)

        def gate_chunk(ps, psb_, NT):
            tz = A.alloc(1)
            R.op('act', lambda e: e.activation(out=tz.bf(0, NT), in_=ps[:, 0:NT], func=AF.Tanh, scale=0.5),
                 reads=[psb_], writes=tz.b)
            u = A.alloc(1)
            R.op('dve', lambda e: e.scalar_tensor_tensor(out=u.bf(0, NT), in0=tz.bf(0, NT), scalar=1.0, in1=ps[:, 0:NT],
                                                         op0=ALU.add, op1=ALU.mult), reads=tz.b + [psb_], writes=u.b)
            tz.free()
            return u

        def norm_rows_bf(src_regs, TB, g, j):
            outs = []
            for tb in range(TB):
                junk = A.alloc(2)
                R.op('act', lambda e, tb=tb, junk=junk: e.activation(out=junk.bf(0, 1024), in_=src_regs[tb].bf(0, 1024),
                                                                     func=AF.Square), reads=src_regs[tb].b, writes=junk.b)
                ss, ssb = sm_next()
                R.op('dve', lambda e, junk=junk, ss=ss: e.tensor_reduce(out=ss[:, 0:g], in_=bview(junk.bf(0, 1024), g, j),
                                                                        axis=AX.X, op=ALU.add), reads=junk.b, writes=[ssb])
                junk.free()
                r, rb = sm_next()
                rstd_into(r[:, 0:g], rb, ss[:, 0:g], ssb, g, 1.0 / j)
                o = A.alloc(2)
                R.op('dve', lambda e, tb=tb, o=o, r=r: e.tensor_tensor(
                    out=bview(o.bf(0, 1024), g, j), in0=bview(src_regs[tb].bf(0, 1024), g, j), in1=bcast_last(r[:, 0:g], j),
                    op=ALU.mult), reads=src_regs[tb].b + [rb], writes=o.b)
                outs.append(o)
            return outs

        def merge_gates(n, NT):
            tgs = []
            for g in range(2):
                def cons_g(ch, ps, psb_):
                    tg = A.alloc(1)
                    R.op('act', lambda e: e.activation(out=tg.bf(0, NT), in_=ps[:, 0:NT], func=AF.Tanh, scale=0.5),
                         reads=[psb_], writes=tg.b)
                    tgs.append(tg)
                fproj('in', 0, 10240 + n * 1024 + g * 512, XNT['r'], XNT['b'], NT, cons_g)
            return tgs

        def merge_proj(n, yg, NT, hacc, tgs):
            yg_b = [p.b for p in yg]
            for g in range(2):
                def cons_b(ch, ps, psb_):
                    dch = g * 4 + ch
                    tg = tgs[dch]
                    if n not in DBG_BR:
                        if n == 0:
                            R.op('pool', lambda e: e.memset(hacc[dch].bf(0, NT), 0.0), writes=hacc[dch].b)
                        R.op('dve', lambda e: e.tensor_copy(out=tg.bf(0, NT), in_=ps[:, 0:NT]), reads=[psb_], writes=tg.b)
                    elif n == 0:
                        R.op('dve', lambda e: e.scalar_tensor_tensor(out=hacc[dch].bf(0, NT), in0=tg.bf(0, NT), scalar=1.0,
                                                                     in1=ps[:, 0:NT], op0=ALU.add, op1=ALU.mult),
                             reads=tg.b + [psb_], writes=hacc[dch].b)
                    else:
                        tmp = A.alloc(1)
                        R.op('dve', lambda e: e.scalar_tensor_tensor(out=tmp.bf(0, NT), in0=tg.bf(0, NT), scalar=1.0,
                                                                     in1=ps[:, 0:NT], op0=ALU.add, op1=ALU.mult),
                             reads=tg.b + [psb_], writes=tmp.b)
                        R.op('pool', lambda e: e.tensor_tensor(out=hacc[dch].bf(0, NT), in0=hacc[dch].bf(0, NT),
                                                               in1=tmp.bf(0, NT), op=ALU.add),
                             reads=tmp.b + hacc[dch].b, writes=hacc[dch].b)
                        tmp.free()
                    tg.free()
                fproj('br', n * 1024, g * 512, yg, yg_b, NT, cons_b)

        XNT = {}

        def hgrn_stage(NT, TB, sample, b):
            xnT, xb_ = XNT['r'], XNT['b']
            NCH = NT // 64
            qraw = [None] * 8
            qdec = [None] * 8
            kinv = [None] * 8
            ua = [None] * 8

            def cons_q(base):
                def f(ch, ps, psb_):
                    h = base + ch
                    q = A.alloc(1)
                    R.op('act', lambda e: e.activation(out=q.bf(0, NT), in_=ps[:, 0:NT], func=AF.Copy), reads=[psb_], writes=q.b)
                    qraw[h] = q
                return f
            for g in range(2):
                fproj('in', 0, g * 512, xnT, xb_, NT, cons_q(g * 4))

            def cons_f(base):
                def f(ch, ps, psb_):
                    h = base + ch
                    t = A.alloc(2)
                    R.op('act', lambda e: e.activation(out=t.f(0, NT), in_=ps[:, 0:NT], func=AF.Tanh, scale=0.5),
                         reads=[psb_], writes=t.b)
                    fm0 = A.alloc(2)
                    R.op('act', lambda e: e.activation(out=fm0.f(0, NT), in_=t.f(0, NT), func=AF.Identity,
                                                       scale=col(C1 + h), bias=col(C0 + h)),
                         reads=t.b + [B_C], writes=fm0.b)
                    pi = state['fm1']
                    state['fm1'] = 1 - pi
                    fm1, fm1b = FM1[pi], B_FM1[pi]
                    st0v = bview(fm0.f(0, NT), NCH, 64)[:, :, 0]
                    R.op('dve', lambda e: e.tensor_copy(out=bview(fm1[:, 0:NT], NCH, 64)[:, :, 0], in_=st0v),
                         reads=fm0.b, writes=[fm1b])
                    R.op('dve', lambda e: e.memset(st0v, 0.0), writes=fm0.b)
                    Acp = A.alloc(2)
                    R.op('dve', lambda e: e.tensor_tensor_scan(out=Acp.f(0, NT), data0=fm0.f(0, NT), data1=fm1[:, 0:NT],
                                                               initial=0.0, op0=ALU.mult, op1=ALU.add),
                         reads=fm0.b + [fm1b], writes=Acp.b)
                    fm0.free()
                    R.op('dve', lambda e: e.tensor_copy(out=AEND[:, h * 8:h * 8 + NCH],
                                                        in_=bview(Acp.f(0, NT), NCH, 64)[:, :, 63]),
                         reads=Acp.b, writes=[B_AEND[h]])
                    rA = A.alloc(2)
                    R.op('dve', lambda e: e.reciprocal(out=rA.f(0, NT), in_=Acp.f(0, NT)), reads=Acp.b, writes=rA.b)
                    k_ = A.alloc(2)
                    R.op('act', lambda e: e.activation(out=k_.f(0, NT), in_=t.f(0, NT), func=AF.Identity,
                                                       scale=col(NC1 + h), bias=col(C1 + h)),
                         reads=t.b + [B_C], writes=k_.b)
                    t.free()
                    ki = A.alloc(1)
                    R.op('pool', lambda e: e.tensor_tensor(out=ki.bf(0, NT), in0=k_.f(0, NT), in1=rA.f(0, NT), op=ALU.mult),
                         reads=k_.b + rA.b, writes=ki.b)
                    k_.free()
                    rA.free()
                    qd = A.alloc(1)
                    R.op('pool', lambda e: e.tensor_tensor(out=qd.bf(0, NT), in0=qraw[h].bf(0, NT), in1=Acp.f(0, NT), op=ALU.mult),
                         reads=qraw[h].b + Acp.b, writes=qd.b)
                    Acp.free()
                    qraw[h].free()
                    kinv[h] = ki
                    qdec[h] = qd
                return f
            for g in range(2):
                fproj('in', 0, 1024 + g * 512, xnT, xb_, NT, cons_f(g * 4))

            vhg = [A.alloc(2) for _ in range(TB)]

            def cons_v(half):
                def f(tb, ps, psb_):
                    R.op('act', lambda e: e.activation(out=vhg[tb].bf(half * 512, half * 512 + 512), in_=ps[:, 0:512], func=AF.Copy),
                         reads=[psb_], writes=vhg[tb].bb(half * 512, half * 512 + 512))
                return f
            for g in range(2):
                tproj('in', 0, 2048 + g * 512, xnT, TB, cons_v(g))

            def cons_z(base):
                def f(ch, ps, psb_):
                    ua[base + ch] = gate_chunk(ps, psb_, NT)
                return f
            for g in range(2):
                fproj('in', 0, 3072 + g * 512, xnT, xb_, NT, cons_z(g * 4))

            set_wide(False)
            kinvT = [A.alloc(2) for _ in range(TB)]
            for tb in range(TB):
                for half in range(2):
                    tr, trb = tr_next()
                    for hh in range(4):
                        h = half * 4 + hh
                        R.op('pe', lambda e, tr=tr, hh=hh, h=h, tb=tb: e.transpose(
                            tr[:, hh * 128:(hh + 1) * 128], kinv[h].bf(tb * 128, (tb + 1) * 128), IDB[:]),
                            reads=kinv[h].b + [B_C], writes=[trb])
                    R.op('act', lambda e, tr=tr, tb=tb, half=half: e.activation(
                        out=kinvT[tb].bf(half * 512, half * 512 + 512), in_=tr[:, 0:512], func=AF.Copy),
                        reads=[trb], writes=kinvT[tb].bb(half * 512, half * 512 + 512))

            on = []
            pend_norm = []
            for tb in range(TB):
                for cc in range(2):
                    c = tb * 2 + cc
                    r0, r1 = cc * 64, cc * 64 + 64
                    if sample:
                        R.op('sp', lambda e, c=c: e.dma_start(out=SF[:].rearrange("p (h v) -> p h v", h=8),
                                                              in_=st0[c].rearrange("h k v -> k h v")),
                             writes=B_SF, dma_key="sld")
                        R.op('act', lambda e: e.activation(out=SB[:], in_=SF[:], func=AF.Copy), reads=B_SF, writes=B_SB)
                    ps1, ps1b = PS[2 + c % 2], B_PS[2 + c % 2]
                    ob = (4, 5) if tb % 2 == 0 else (0, 1)
                    for h in range(8):
                        R.op('pe', lambda e, h=h, c=c, ps1=ps1, r0=r0, r1=r1: e.matmul(
                            ps1[r0:r1, h * 64:(h + 1) * 64], lhsT=kinv[h].bf(c * 64, c * 64 + 64),
                            rhs=qdec[h].bf(c * 64, c * 64 + 64), start=True, stop=True),
                            reads=kinv[h].b + qdec[h].b, writes=[ps1b])
                    attm = A.alloc(1)
                    R.op('dve', lambda e, ps1=ps1, attm=attm, r0=r0, r1=r1: e.tensor_tensor(
                        out=bview(attm.rows_bf(r0, r1, 0, 512), 8, 64), in0=bview(ps1[r0:r1, 0:512], 8, 64),
                        in1=bcast_mid(MHG[r0:r1, :], 8), op=ALU.mult), reads=[ps1b, B_C], writes=attm.b)
                    while pend_norm:
                        pend_norm.pop(0)()
                    for h in range(8):
                        pa, pab = acc(2 + h // 4)
                        cs = (h % 4) * 128
                        R.op('pe', lambda e, h=h, pa=pa, cs=cs, tb=tb, r0=r0, r1=r1: e.matmul(
                            pa[:, cs:cs + 128], lhsT=kinvT[tb].rows_bf(r0, r1, h * 128, h * 128 + 128),
                            rhs=vhg[tb].rows_bf(r0, r1, h * 128, h * 128 + 128), start=True, stop=True),
                            reads=kinvT[tb].b + vhg[tb].b, writes=[pab])
                    for h in range(8):
                        pa, pab = PS[ob[h // 4]], B_PS[ob[h // 4]]
                        cs = (h % 4) * 128
                        R.op('pe', lambda e, h=h, pa=pa, cs=cs, tb=tb, attm=attm, r0=r0, r1=r1: e.matmul(
                            pa[r0:r1, cs:cs + 128], lhsT=attm.rows_bf(r0, r1, h * 64, h * 64 + 64),
                            rhs=vhg[tb].rows_bf(r0, r1, h * 128, h * 128 + 128), start=True, stop=False),
                            reads=attm.b + vhg[tb].b, writes=[pab])
                        R.op('pe', lambda e, h=h, pa=pa, cs=cs, c=c, r0=r0, r1=r1: e.matmul(
                            pa[r0:r1, cs:cs + 128], lhsT=qdec[h].bf(c * 64, c * 64 + 64),
                            rhs=SB[:, h * 128:(h + 1) * 128], start=False, stop=True),
                            reads=qdec[h].b + [B_SB[h]], writes=[pab])
                    attm.free()
                    for hb in range(2):
                        pa, pab = acc(2 + hb)
                        sfv = SF[:, hb * 512:(hb + 1) * 512]
                        aeb = AEND[:, hb * 32:(hb + 1) * 32].rearrange("p (h c) -> p h c", c=8)[:, :, c]
                        bsf = B_SF[hb * 4:hb * 4 + 4]
                        R.op('dve', lambda e, sfv=sfv, pa=pa: e.tensor_tensor(out=sfv, in0=sfv, in1=pa[:, 0:512], op=ALU.add),
                             reads=[pab] + bsf, writes=bsf)
                        R.op('dve', lambda e, sfv=sfv, aeb=aeb: e.tensor_tensor(
                            out=bview(sfv, 4, 128), in0=bview(sfv, 4, 128), in1=bcast_last(aeb, 128), op=ALU.mult),
                            reads=bsf + B_AEND[hb * 4:hb * 4 + 4], writes=bsf)
                        R.op('act', lambda e, sfv=sfv, hb=hb: e.activation(out=SB[:, hb * 512:(hb + 1) * 512], in_=sfv, func=AF.Copy),
                             reads=bsf, writes=B_SB[hb * 4:hb * 4 + 4])
                    if sample:
                        R.op('sp', lambda e, c=c: e.dma_start(out=st_s[c].rearrange("h k v -> k h v"),
                                                              in_=SF[:].rearrange("p (h v) -> p h v", h=8)),
                             reads=B_SF, dma_key="sst")
                def norm_o(ob=ob):
                    o_n = A.alloc(2)
                    for i in range(2):
                        pa, pab = PS[ob[i]], B_PS[ob[i]]
                        r, rb = group_stats(pa[:, 0:512], pab, 4, 128)
                        R.op('dve', lambda e, pa=pa, o_n=o_n, i=i, r=r: e.tensor_tensor(
                            out=bview(o_n.bf(i * 512, i * 512 + 512), 4, 128), in0=bview(pa[:, 0:512], 4, 128),
                            in1=bcast_last(r[:, 0:4], 128), op=ALU.mult), reads=[pab, rb], writes=o_n.bb(i * 512, i * 512 + 512))
                    on.append(o_n)
                pend_norm.append(norm_o)
            while pend_norm:
                pend_norm.pop(0)()
            if (not sample) and b is not None:
                R.op('sp', lambda e: e.dma_start(out=st_p[b].rearrange("h k v -> k h v"),
                                                 in_=SF[:].rearrange("p (h v) -> p h v", h=8)), reads=B_SF, dma_key="sst")
            for x_ in kinv + qdec + vhg + kinvT:
                x_.free()
            set_wide(True)
            return on, ua

        def gated_y(src, us, TB, NT, scale):
            ys = []
            for c in range(8):
                y = A.alloc(1)

                def ev(tr, trb, y=y, c=c):
                    R.op('dve', lambda e: e.scalar_tensor_tensor(out=y.bf(0, NT), in0=tr[:, 0:NT], scalar=scale,
                                                                 in1=us[c].bf(0, NT), op0=ALU.mult, op1=ALU.mult),
                         reads=[trb, B_C] + us[c].b, writes=y.b)
                transpose_blocks(src, c * 128, TB, ev)
                us[c].free()
                ys.append(y)
            for x_ in src:
                x_.free()
            return ys

        def pv_finish_diff(cmap, tbs, rows, h, o0, od):
            for (ai, tb) in tbs:
                pa, pab = acc(ai)
                r0, r1 = rows
                rr, rrb = sm_next()
                R.op('dve', lambda e, pa=pa, rr=rr: e.reciprocal(out=rr[r0:r1, 0:1], in_=pa[r0:r1, 128:129]),
                     reads=[pab], writes=[rrb])
                if cmap == 0:
                    R.op('dve', lambda e, pa=pa, rr=rr, ai=ai: e.tensor_scalar(
                        out=o0.ar.tf[r0:r1, o0.p0 * 256 + ai * 128:o0.p0 * 256 + ai * 128 + 128], in0=pa[r0:r1, 0:128],
                        scalar1=rr[r0:r1, 0:1], scalar2=None, op0=ALU.mult), reads=[pab, rrb], writes=o0.b)
                else:
                    R.op('dve', lambda e, rr=rr: e.tensor_scalar(out=rr[r0:r1, 1:2], in0=rr[r0:r1, 0:1],
                                                                 scalar1=DER[r0:r1, LAMN:LAMN + 1], scalar2=None, op0=ALU.mult),
                         reads=[rrb, B_C], writes=[rrb])
                    R.op('dve', lambda e, pa=pa, rr=rr, ai=ai, tb=tb: e.scalar_tensor_tensor(
                        out=od[tb].rows_bf(r0, r1, h * 128, h * 128 + 128), in0=pa[r0:r1, 0:128], scalar=rr[r0:r1, 1:2],
                        in1=o0.ar.tf[r0:r1, o0.p0 * 256 + ai * 128:o0.p0 * 256 + ai * 128 + 128], op0=ALU.mult, op1=ALU.add),
                        reads=[pab, rrb] + o0.b, writes=od[tb].bb(h * 128, h * 128 + 128))

        def diff_attn_prompt(j, QZ, od):
            NT = 512
            nkb = 4 * j + 4
            steps = [(h, cmap, kb) for h in range(8) for cmap in range(2) for kb in range(nkb)]
            o0s = {}

            def emit_S(h, cmap, kb):
                q0 = max(kb - 4 * j, 0)
                ncol = NT - q0 * 128
                ps, psb_ = mm_next()
                qz = QZ[h][cmap]
                R.op('pe', lambda e: e.matmul(
                    ps[:, 0:ncol], lhsT=KT[:, h * SEQ + kb * 128:h * SEQ + kb * 128 + 128],
                    rhs=qz.bf(q0 * 128, NT), start=True, stop=True),
                    reads=[B_KT[h][kb // 4]] + qz.b, writes=[psb_])
                E = A.alloc(1)
                R.op('act', lambda e: e.activation(out=E.bf(0, ncol), in_=ps[:, 0:ncol], func=AF.Exp),
                     reads=[psb_], writes=E.b)
                if kb >= 4 * j:
                    R.op('pool', lambda e: e.memset(E.rows_bf(64, 128, 0, 64), 0.0), writes=E.b)
                return E

            def emit_PV(h, cmap, kb, E):
                q0 = max(kb - 4 * j, 0)
                if cmap == 0 and kb == 0:
                    o0s[h] = A.alloc(2)
                for qb in range(q0, 4):
                    pa, pab = acc(qb)
                    R.op('pe', lambda e, pa=pa, qb=qb: e.matmul(
                        pa[:, 0:129], lhsT=E.bf((qb - q0) * 128, (qb - q0) * 128 + 128),
                        rhs=VA[:, kb * 1040 + h * 130:kb * 1040 + h * 130 + 129],
                        start=(kb == 0), stop=(kb == 4 * j + qb)),
                        reads=E.b + [B_VA[kb]], writes=[pab])
                    if kb == 4 * j + qb:
                        pv_finish_diff(cmap, [(qb, qb)], (0, 128), h, o0s[h], od)
                E.free()
                if cmap == 1 and kb == nkb - 1:
                    o0s.pop(h).free()

            pend = []
            for stp in steps:
                E = emit_S(*stp)
                pend.append(stp + (E,))
                if len(pend) > PIPE_DEPTH:
                    emit_PV(*pend.pop(0))
            while pend:
                emit_PV(*pend.pop(0))

        def mem_attn(NT, TB, QM, om, qsegs):
            c_lo = min(s[0] for s in qsegs)
            c_hi = max(s[0] + s[1] for s in qsegs)
            for h in range(4):
                Es = []
                for kb in range(2):
                    ps, psb_ = mm_next()
                    for dc in range(2):
                        ci = h * 2 + dc
                        R.op('pe', lambda e, ps=ps, ci=ci, kb=kb, dc=dc: e.matmul(
                            ps[:, c_lo:c_hi], lhsT=KM[:, ci * 256 + kb * 128:ci * 256 + kb * 128 + 128],
                            rhs=QM[ci].bf(c_lo, c_hi), start=(dc == 0), stop=(dc == 1)),
                            reads=[B_KM] + QM[ci].b, writes=[psb_])
                    E = A.alloc(1)
                    R.op('act', lambda e, ps=ps, E=E: e.activation(out=E.bf(c_lo, c_hi), in_=ps[:, c_lo:c_hi], func=AF.Exp),
                         reads=[psb_], writes=E.b)
                    Es.append(E)
                for (c0, ncl, ai, tb, r0) in qsegs:
                    pa, pab = acc(ai)
                    for kb in range(2):
                        R.op('pe', lambda e, pa=pa, kb=kb, c0=c0, ncl=ncl, r0=r0, h=h, Es=Es: e.matmul(
                            pa[r0:r0 + ncl, 0:257], lhsT=Es[kb].bf(c0, c0 + ncl),
                            rhs=VM[:, kb * 1032 + h * 258:kb * 1032 + h * 258 + 257], start=(kb == 0), stop=(kb == 1)),
                            reads=Es[kb].b + [B_VM[kb]], writes=[pab])
                    rr, rrb = sm_next()
                    R.op('dve', lambda e, pa=pa, rr=rr, r0=r0, ncl=ncl: e.reciprocal(out=rr[r0:r0 + ncl, 0:1],
                                                                                 in_=pa[r0:r0 + ncl, 256:257]),
                         reads=[pab], writes=[rrb])
                    R.op('dve', lambda e, pa=pa, rr=rr, r0=r0, ncl=ncl, tb=tb, h=h: e.tensor_scalar(
                        out=om[tb].rows_bf(r0, r0 + ncl, h * 256, h * 256 + 256), in0=pa[r0:r0 + ncl, 0:256],
                        scalar1=rr[r0:r0 + ncl, 0:1], scalar2=None, op0=ALU.mult),
                        reads=[pab, rrb], writes=om[tb].bb(h * 256, h * 256 + 256))
                for E in Es:
                    E.free()

        def main_tile(NT, TB, sample, b, j, xn_pre):
            if sample:
                src_rows = lambda tb: xsm[tb * 128:(tb + 1) * 128, :]
                yout, dkout, dvout, row0 = y_s, dk_s, dv_s, 0
            else:
                row0 = b * SEQ + j * 512
                src_rows = lambda tb: xp[row0 + tb * 128:row0 + (tb + 1) * 128, :]
                yout, dkout, dvout = y_p, dk_p, dv_p
            xnT = xstage_b(xn_pre, TB, P_GN)
            XNT['r'] = xnT
            XNT['b'] = [p.b for p in xnT]
            xb_ = XNT['b']
            if (not sample) and j == 0:
                R.op('pool', lambda e: e.memset(SF[:], 0.0), writes=B_SF)
                R.op('pool', lambda e: e.memset(SB[:], 0.0), writes=B_SB)
            hacc = [A.alloc(1) for _ in range(8)]
            on, ua = hgrn_stage(NT, TB, sample, b if (not sample and j == ntiles - 1) else None)
            tgs = merge_gates(0, NT)
            ya = gated_y(on, ua, TB, NT, col(GHG))
            merge_proj(0, ya, NT, hacc, tgs)
            for y in ya:
                y.free()

            qn = [A.alloc(2) for _ in range(TB)]

            def cons_qk(dst, half, fp32_out=None):
                def f(tb, ps, psb_):
                    r, rb = group_stats(ps[:, 0:512], psb_, 8, 64)
                    if fp32_out is None:
                        R.op('dve', lambda e: e.tensor_tensor(out=bview(dst[tb].bf(half * 512, half * 512 + 512), 8, 64),
                                                              in0=bview(ps[:, 0:512], 8, 64), in1=bcast_last(r[:, 0:8], 64),
                                                              op=ALU.mult), reads=[psb_, rb], writes=dst[tb].bb(half * 512, half * 512 + 512))
                    else:
                        ko = fp32_out[tb]
                        R.op('dve', lambda e: e.tensor_tensor(out=bview(ko.f(half * 512, half * 512 + 512), 8, 64),
                                                              in0=bview(ps[:, 0:512], 8, 64), in1=bcast_last(r[:, 0:8], 64),
                                                              op=ALU.mult), reads=[psb_, rb], writes=ko.bF(half * 512, half * 512 + 512))
                        R.op('dve', lambda e: e.tensor_tensor(out=bview(dst[tb].bf(half * 512, half * 512 + 512), 8, 64),
                                                              in0=bview(ps[:, 0:512], 8, 64), in1=bcast_last(r[:, 0:8], 64),
                                                              op=ALU.mult), reads=[psb_, rb], writes=dst[tb].bb(half * 512, half * 512 + 512))
                        R.op('pool', lambda e: e.tensor_tensor(out=bview(ko.f(half * 512, half * 512 + 512), 8, 64),
                                                               in0=bview(ko.f(half * 512, half * 512 + 512), 8, 64),
                                                               in1=bcast_mid(CBC[:, 256:320], 8), op=ALU.mult),
                             reads=ko.bF(half * 512, half * 512 + 512) + [B_C], writes=ko.bF(half * 512, half * 512 + 512))
                return f
            kn = [A.alloc(2) for _ in range(TB)]
            kof = [A.alloc(4) for _ in range(TB)]
            def st_k(kof=kof):
                for tb in range(TB):
                    R.op('sp', lambda e, tb=tb: e.dma_start(out=dkout[row0 + tb * 128:row0 + (tb + 1) * 128, :], in_=kof[tb].f(0, 1024)),
                         reads=kof[tb].b, dma_key="ko%d" % (tb % 2))
                    kof[tb].free()
            vof = [A.alloc(4) for _ in range(TB)]
            vnew = [A.alloc(2) for _ in range(TB)] if sample else None

            def cons_dv(half):
                def f(tb, ps, psb_):
                    R.op('act', lambda e: e.activation(out=vof[tb].f(half * 512, half * 512 + 512), in_=ps[:, 0:512], func=AF.Copy),
                         reads=[psb_], writes=vof[tb].bF(half * 512, half * 512 + 512))
                    if sample:
                        R.op('dve', lambda e: e.tensor_copy(out=vnew[tb].bf(half * 512, half * 512 + 512), in_=ps[:, 0:512]),
                             reads=[psb_], writes=vnew[tb].bb(half * 512, half * 512 + 512))
                    else:
                        blk = j * 4 + tb
                        R.op('dve', lambda e: e.tensor_copy(
                            out=bview(VA[:, blk * 1040 + half * 520:blk * 1040 + half * 520 + 520], 4, 130)[:, :, 0:128],
                            in_=bview(ps[:, 0:512], 4, 128)), reads=[psb_], writes=[B_VA[blk]])
                return f
            ub = [None] * 8

            def cons_zb(base):
                def f(ch, ps, psb_):
                    ub[base + ch] = gate_chunk(ps, psb_, NT)
                return f
            for g in range(2):
                tproj('in', 0, 4096 + g * 512, xnT, TB, cons_qk(qn, g))
                tproj('in', 0, 5120 + g * 512, xnT, TB, cons_qk(kn, g, kof))
                tproj('in', 0, 6144 + g * 512, xnT, TB, cons_dv(g))
                fproj('in', 0, 7168 + g * 512, xnT, xb_, NT, cons_zb(g * 4))
            defer(st_k)

            def st_v(vof=vof):
                for tb in range(TB):
                    R.op('sp', lambda e, tb=tb: e.dma_start(out=dvout[row0 + tb * 128:row0 + (tb + 1) * 128, :], in_=vof[tb].f(0, 1024)),
                         reads=vof[tb].b, dma_key="vo%d" % (tb % 2))
                    vof[tb].free()
            defer(st_v)
            QT = []
            for h in range(8):
                if sample:
                    q = A.alloc(1)

                    def ev(tr, trb, q=q):
                        R.op('dve', lambda e: e.tensor_scalar(out=q.bf(0, NT), in0=tr[:, 0:NT], scalar1=col(GQ8), scalar2=None,
                                                              op0=ALU.mult), reads=[trb, B_C], writes=q.b)
                    transpose_blocks(qn, h * 128, TB, ev)
                    QT.append(q)
                else:
                    qz = [A.alloc(1), A.alloc(1)]
                    R.op('pool', lambda e, qz=qz: e.memset(qz[0].rows_bf(64, 128, 0, NT), 0.0), writes=qz[0].b)
                    R.op('pool', lambda e, qz=qz: e.memset(qz[1].rows_bf(0, 64, 0, NT), 0.0), writes=qz[1].b)

                    def ev(tr, trb, qz=qz):
                        for m in range(2):
                            r0, r1 = m * 64, m * 64 + 64
                            R.op('dve', lambda e, m=m, r0=r0, r1=r1: e.tensor_scalar(
                                out=qz[m].rows_bf(r0, r1, 0, NT), in0=tr[r0:r1, 0:NT], scalar1=DER[r0:r1, GQ8:GQ8 + 1],
                                scalar2=None, op0=ALU.mult), reads=[trb, B_C], writes=qz[m].b)
                    transpose_blocks(qn, h * 128, TB, ev)
                    QT.append(qz)
            for x_ in qn:
                x_.free()
            ktok0 = 1024 if sample else j * 512
            kbuf = (lambda h: B_KT[h][2]) if sample else (lambda h: B_KT[h][j])
            if not sample:
                for h in range(8):
                    def ev(tr, trb, h=h):
                        R.op('act', lambda e: e.activation(out=KT[:, h * SEQ + ktok0:h * SEQ + ktok0 + NT], in_=tr[:, 0:NT], func=AF.Copy,
                                                           scale=col(GK2)),
                             reads=[trb, B_C], writes=[kbuf(h)])
                    transpose_blocks(kn, h * 128, TB, ev)
                for x_ in kn:
                    x_.free()
            od = [A.alloc(2) for _ in range(TB)]
            set_wide(False)
            if not sample:
                diff_attn_prompt(j, QT, od)
            else:
                sample_diff_attn(QT, kn, vnew, od)
                for x_ in kn + vnew:
                    x_.free()
            set_wide(True)
            for q in QT:
                if sample:
                    q.free()
                else:
                    q[0].free()
                    q[1].free()
            odn = norm_rows_bf(od, TB, 8, 128)
            for x_ in od:
                x_.free()
            tgs = merge_gates(1, NT)
            yb = gated_y(odn, ub, TB, NT, col(GDS))
            merge_proj(1, yb, NT, hacc, tgs)
            for y in yb:
                y.free()

            hoist()
            qmn = [A.alloc(2) for _ in range(TB)]

            def cons_mq(half):
                def f(tb, ps, psb_):
                    r, rb = group_stats(ps[:, 0:512], psb_, 2, 256)
                    R.op('dve', lambda e: e.tensor_tensor(out=bview(qmn[tb].bf(half * 512, half * 512 + 512), 2, 256),
                                                          in0=bview(ps[:, 0:512], 2, 256), in1=bcast_last(r[:, 0:2], 256),
                                                          op=ALU.mult), reads=[psb_, rb], writes=qmn[tb].bb(half * 512, half * 512 + 512))
                return f
            for g in range(2):
                tproj('in', 0, 8192 + g * 512, xnT, TB, cons_mq(g))
            um = [None] * 8

            def cons_zm(base):
                def f(ch, ps, psb_):
                    um[base + ch] = gate_chunk(ps, psb_, NT)
                return f
            for g in range(2):
                fproj('in', 0, 9216 + g * 512, xnT, xb_, NT, cons_zm(g * 4))
            QM = []
            for ci in range(8):
                q = A.alloc(1)

                def ev(tr, trb, q=q, ci=ci):
                    R.op('dve', lambda e: e.tensor_scalar(out=q.bf(0, NT), in0=tr[:, 0:NT], scalar1=col(GMQ + ci % 2),
                                                          scalar2=None, op0=ALU.mult), reads=[trb, B_C], writes=q.b)
                transpose_blocks(qmn, ci * 128, TB, ev)
                QM.append(q)
            for x_ in qmn:
                x_.free()
            om = [A.alloc(2) for _ in range(TB)]
            set_wide(False)
            if not sample:
                mem_attn(NT, TB, QM, om, [(qb * 128, 128, qb, qb, 0) for qb in range(4)])
            else:
                for i in range(4):
                    sample_load_mem(i)
                    mem_attn(NT, TB, QM, om, [(i * 64, 64, i % 4, i // 2, (i % 2) * 64)])
            for q in QM:
                q.free()
            set_wide(True)
            tgs = merge_gates(2, NT)
            ym = gated_y(om, um, TB, NT, 0.5)
            merge_proj(2, ym, NT, hacc, tgs)
            for y in ym:
                y.free()
            for p in xnT:
                p.free()

            xres = []
            for tb in range(TB):
                xr = A.alloc(4)
                R.op('sp', lambda e, xr=xr, tb=tb: e.dma_start(out=xr.f(0, 1024), in_=src_rows(tb)), writes=xr.b,
                     dma_key="xr%d" % (tb % 2))
                xres.append(xr)

            def cons_out(half):
                def f(tb, ps, psb_):
                    xr = xres[tb]
                    R.op('dve', lambda e: e.scalar_tensor_tensor(out=xr.f(half * 512, half * 512 + 512), in0=ps[:, 0:512],
                                                                 scalar=0.5, in1=xr.f(half * 512, half * 512 + 512),
                                                                 op0=ALU.mult, op1=ALU.add),
                         reads=[psb_] + xr.bF(half * 512, half * 512 + 512), writes=xr.bF(half * 512, half * 512 + 512))
                return f
            for g in range(2):
                tproj('out', 0, g * 512, hacc, TB, cons_out(g))
            def st_y(xres=xres):
                for tb in range(TB):
                    R.op('sp', lambda e, tb=tb: e.dma_start(out=yout[row0 + tb * 128:row0 + (tb + 1) * 128, :], in_=xres[tb].f(0, 1024)),
                         reads=xres[tb].b, dma_key="yo%d" % (tb % 2))
                    xres[tb].free()
            defer(st_y)
            for p in hacc:
                p.free()

        def sample_diff_attn(QT, kn, vnew, od):
            NT = 256
            knT = [[None] * 8 for _ in range(2)]
            for tbp in range(2):
                for h in range(8):
                    p = A.alloc(1)

                    def ev(tr, trb, p=p):
                        R.op('act', lambda e: e.activation(out=p.bf(0, 128), in_=tr[:, 0:128], func=AF.Copy, scale=col(GK2)),
                             reads=[trb, B_C], writes=p.b)
                    transpose_blocks([kn[tbp]], h * 128, 1, ev)
                    knT[tbp][h] = p
            for i in range(4):
                tb, r0 = i // 2, (i % 2) * 64
                for blk in range(8):
                    kc_ = A.alloc(2)
                    R.op('pool', lambda e, kc_=kc_, blk=blk, i=i: e.dma_start(
                        out=kc_.bf(0, 1024), in_=cdk[i * 1024 + blk * 128:i * 1024 + (blk + 1) * 128, :]),
                        writes=kc_.b, dma_key="ck%d" % (blk % 2))
                    for half in range(2):
                        tr, trb = tr_next()
                        for hh in range(4):
                            h = half * 4 + hh
                            R.op('pe', lambda e, tr=tr, hh=hh, h=h, kc_=kc_: e.transpose(
                                tr[:, hh * 128:(hh + 1) * 128], kc_.bf(h * 128, h * 128 + 128), IDB[:]),
                                reads=kc_.b + [B_C], writes=[trb])
                        for hh in range(4):
                            h = half * 4 + hh
                            R.op('act', lambda e, tr=tr, hh=hh, h=h, blk=blk: e.activation(
                                out=KT[:, h * SEQ + blk * 128:h * SEQ + blk * 128 + 128], in_=tr[:, hh * 128:(hh + 1) * 128],
                                func=AF.Copy), reads=[trb], writes=[B_KT[h][blk // 4]])
                    kc_.free()
                    R.op('pool', lambda e, blk=blk, i=i: e.dma_start(
                        out=bview(VA[:, blk * 1040:(blk + 1) * 1040], 8, 130)[:, :, 0:128],
                        in_=cdv[i * 1024 + blk * 128:i * 1024 + (blk + 1) * 128, :].rearrange("p (h v) -> p h v", h=8)),
                        writes=[B_VA[blk]], dma_key="cv%d" % (blk % 2))
                for h in range(8):
                    R.op('act', lambda e, h=h, tb=tb: e.activation(out=KT[:, h * SEQ + 1024:h * SEQ + 1152],
                                                                   in_=knT[tb][h].bf(0, 128), func=AF.Copy),
                         reads=knT[tb][h].b, writes=[B_KT[h][2]])
                R.op('dve', lambda e, tb=tb: e.tensor_copy(out=bview(VA[:, 8 * 1040:9 * 1040], 8, 130)[:, :, 0:128],
                                                         in_=bview(vnew[tb].bf(0, 1024), 8, 128)),
                     reads=vnew[tb].b, writes=[B_VA[8]])
                o0s = {}

                def emit_S(h, cmap, kb, i=i, r0=r0):
                    p0, p1 = cmap * 64, cmap * 64 + 64
                    k0, k1 = (0, 128) if kb < 8 else (r0, r0 + 64)
                    ps, psb_ = mm_next()
                    R.op('pe', lambda e: e.matmul(
                        ps[k0:k1, 0:64], lhsT=KT[p0:p1, h * SEQ + kb * 128 + k0:h * SEQ + kb * 128 + k1],
                        rhs=QT[h].rows_bf(p0, p1, i * 64, i * 64 + 64), start=True, stop=True),
                        reads=[B_KT[h][kb // 4]] + QT[h].b, writes=[psb_])
                    E = A.alloc(1)
                    R.op('act', lambda e: e.activation(
                        out=E.rows_bf(k0, k1, 0, 64), in_=ps[k0:k1, 0:64], func=AF.Exp), reads=[psb_], writes=E.b)
                    return E

                def emit_PV(h, cmap, kb, E, i=i, r0=r0, tb=tb):
                    k0, k1 = (0, 128) if kb < 8 else (r0, r0 + 64)
                    if cmap == 0 and kb == 0:
                        o0s[h] = A.alloc(2)
                    pa, pab = acc(i)
                    R.op('pe', lambda e: e.matmul(
                        pa[r0:r0 + 64, 0:129], lhsT=E.rows_bf(k0, k1, 0, 64),
                        rhs=VA[k0:k1, kb * 1040 + h * 130:kb * 1040 + h * 130 + 129], start=(kb == 0), stop=(kb == 8)),
                        reads=E.b + [B_VA[kb]], writes=[pab])
                    E.free()
                    if kb == 8:
                        pv_finish_diff(cmap, [(i, tb)], (r0, r0 + 64), h, o0s[h], od)
                        if cmap == 1:
                            o0s.pop(h).free()

                pend = []
                for stp in [(h, cmap, kb) for h in range(8) for cmap in range(2) for kb in range(9)]:
                    E = emit_S(*stp)
                    pend.append(stp + (E,))
                    if len(pend) > PIPE_DEPTH:
                        emit_PV(*pend.pop(0))
                while pend:
                    emit_PV(*pend.pop(0))
            for tbp in range(2):
                for h in range(8):
                    knT[tbp][h].free()

        def sample_load_mem(i):
            for kb in range(2):
                kc_ = A.alloc(2)
                R.op('pool', lambda e, kc_=kc_, kb=kb: e.dma_start(out=kc_.bf(0, 1024),
                                                                in_=cmk[i * 256 + kb * 128:i * 256 + (kb + 1) * 128, :]),
                     writes=kc_.b, dma_key="cm%d" % kb)
                for half in range(2):
                    tr, trb = tr_next()
                    for cc in range(4):
                        ci = half * 4 + cc
                        R.op('pe', lambda e, tr=tr, cc=cc, ci=ci, kc_=kc_: e.transpose(
                            tr[:, cc * 128:(cc + 1) * 128], kc_.bf(ci * 128, ci * 128 + 128), IDB[:]),
                            reads=kc_.b + [B_C], writes=[trb])
                    for cc in range(4):
                        ci = half * 4 + cc
                        R.op('act', lambda e, tr=tr, cc=cc, ci=ci, kb=kb: e.activation(
                            out=KM[:, ci * 256 + kb * 128:ci * 256 + kb * 128 + 128], in_=tr[:, cc * 128:(cc + 1) * 128],
                            func=AF.Copy), reads=[trb], writes=[B_KM])
                kc_.free()
                R.op('pool', lambda e, kb=kb: e.dma_start(
                    out=bview(VM[:, kb * 1032:(kb + 1) * 1032], 4, 258)[:, :, 0:256],
                    in_=cmv[i * 256 + kb * 128:i * 256 + (kb + 1) * 128, :].rearrange("p (h v) -> p h v", h=4)),
                    writes=[B_VM[kb]], dma_key="cw%d" % kb)

        def mem_kv(b, xn_pre):
            TB = 2
            r0 = b * 256
            xnT = xstage_b(xn_pre, TB, P_GM)
            kn = [A.alloc(2) for _ in range(TB)]
            kof = [A.alloc(4) for _ in range(TB)]

            def cons_k(half):
                def f(tb, ps, psb_):
                    r, rb = group_stats(ps[:, 0:512], psb_, 2, 256)
                    ko = kof[tb]
                    c0, c1 = half * 512, half * 512 + 512
                    R.op('dve', lambda e: e.tensor_tensor(out=bview(ko.f(c0, c1), 2, 256), in0=bview(ps[:, 0:512], 2, 256),
                                                          in1=bcast_last(r[:, 0:2], 256), op=ALU.mult),
                         reads=[psb_, rb], writes=ko.bF(c0, c1))
                    R.op('pool', lambda e: e.tensor_tensor(out=bview(ko.f(c0, c1), 2, 256), in0=bview(ko.f(c0, c1), 2, 256),
                                                           in1=bcast_mid(CBC[:, 320:576], 2), op=ALU.mult),
                         reads=ko.bF(c0, c1) + [B_C], writes=ko.bF(c0, c1))
                    R.op('act', lambda e: e.activation(out=kn[tb].bf(c0, c1), in_=ko.f(c0, c1), func=AF.Copy),
                         reads=ko.bF(c0, c1), writes=kn[tb].bb(c0, c1))
                return f
            for g in range(2):
                tproj('mkv', 0, g * 512, xnT, TB, cons_k(g))
            def st_k(kof=kof):
                for tb in range(TB):
                    R.op('sp', lambda e, tb=tb: e.dma_start(out=mk_p[r0 + tb * 128:r0 + (tb + 1) * 128, :], in_=kof[tb].f(0, 1024)),
                         reads=kof[tb].b, dma_key="ko%d" % (tb % 2))
                    kof[tb].free()
            defer(st_k)
            for ci in range(8):
                def ev(tr, trb, ci=ci):
                    R.op('act', lambda e: e.activation(out=KM[:, ci * 256:ci * 256 + 256], in_=tr[:, 0:256], func=AF.Copy),
                         reads=[trb], writes=[B_KM])
                transpose_blocks(kn, ci * 128, TB, ev)
            for x_ in kn:
                x_.free()
            hoist()
            vof = [A.alloc(4) for _ in range(TB)]

            def cons_v(half):
                def f(tb, ps, psb_):
                    c0, c1 = half * 512, half * 512 + 512
                    R.op('act', lambda e: e.activation(out=vof[tb].f(c0, c1), in_=ps[:, 0:512], func=AF.Copy),
                         reads=[psb_], writes=vof[tb].bF(c0, c1))
                    if DBG_V == 1:
                        for g2 in range(2):
                            R.op('dve', lambda e, g2=g2: e.tensor_copy(
                                out=VM[:, tb * 1032 + half * 516 + g2 * 258:tb * 1032 + half * 516 + g2 * 258 + 256],
                                in_=ps[:, g2 * 256:(g2 + 1) * 256]), reads=[psb_], writes=[B_VM[tb]])
                    elif DBG_V == 2:
                        R.op('act', lambda e: e.activation(
                            out=bview(VM[:, tb * 1032 + half * 516:tb * 1032 + half * 516 + 516], 2, 258)[:, :, 0:256],
                            in_=bview(ps[:, 0:512], 2, 256), func=AF.Copy), reads=[psb_], writes=[B_VM[tb]])
                    else:
                        R.op('dve', lambda e: e.tensor_copy(
                            out=bview(VM[:, tb * 1032 + half * 516:tb * 1032 + half * 516 + 516], 2, 258)[:, :, 0:256],
                            in_=bview(ps[:, 0:512], 2, 256)), reads=[psb_], writes=[B_VM[tb]])
                return f
            for g in range(2):
                tproj('mkv', 0, 1024 + g * 512, xnT, TB, cons_v(g))
            def st_v(vof=vof):
                for tb in range(TB):
                    R.op('sp', lambda e, tb=tb: e.dma_start(out=mv_p[r0 + tb * 128:r0 + (tb + 1) * 128, :], in_=vof[tb].f(0, 1024)),
                         reads=vof[tb].b, dma_key="vo%d" % (tb % 2))
                    vof[tb].free()
            defer(st_v)
            for p in xnT:
                p.free()

        units = []
        for b in range(nb_run):
            units.append(('mem', b, None))
            for j in range(ntiles):
                units.append(('main', b, j))
        if do_sample:
            units.append(('sample', None, None))
        prepared = {}
        cur = {'k': 0}

        def prep(k):
            if k >= len(units) or k in prepared:
                return
            kind, b, j = units[k]
            if kind == 'mem':
                prepared[k] = xstage_a(lambda tb, b=b: mp[b * 256 + tb * 128:b * 256 + (tb + 1) * 128, :], 2)
            elif kind == 'main':
                r0_ = b * SEQ + j * 512
                prepared[k] = xstage_a(lambda tb, r0_=r0_: xp[r0_ + tb * 128:r0_ + (tb + 1) * 128, :], 4)
            else:
                prepared[k] = xstage_a(lambda tb: xsm[tb * 128:(tb + 1) * 128, :], 2)

        def hoist():
            prep(cur['k'] + 1)

        for k, (kind, b, j) in enumerate(units):
            cur['k'] = k
            prep(k)
            xn_pre = prepared.pop(k)
            if kind == 'mem':
                mem_kv(b, xn_pre)
            elif kind == 'main':
                main_tile(512, 4, False, b, j, xn_pre)
            else:
                main_tile(256, 2, True, None, None, xn_pre)
        flush_pending(True)
        assert ws_state['cur'] == len(plan)
        R.emit(nc, st)
    return nc


_CACHE = {}


def pack_inputs(x_prompt, x_sample, mem_prompt, cache_diff_k, cache_diff_v, cache_mem_k, cache_mem_v,
                state_hgrn, g_norm, w_in, hg_lb_logits, g_hg_out, g_dq, g_dk, lam_q1, lam_k1,
                lam_q2, lam_k2, g_dsub, g_mem, w_mkv, g_mq, g_mk, w_branch, w_out, n_cores=8):
    f32 = np.float32
    A_ = lambda a: np.ascontiguousarray(np.asarray(a, dtype=f32))
    pk = lambda v, n: A_(v).reshape(n, 128).T
    cpk = np.zeros((128, 40), f32)
    cpk[:, 0:8] = pk(g_norm[0], 8)
    cpk[:, 8:16] = pk(g_mem[0], 8)
    cpk[:, 16:24] = pk(hg_lb_logits[0], 8)
    cpk[:, 24:32] = pk(hg_lb_logits[1], 8)
    cpk[:, 32] = A_(g_hg_out[0])
    cpk[:, 33] = np.tile(A_(g_dq[0]), 2)
    cpk[:, 34] = np.tile(A_(g_dk[0]), 2)
    cpk[:, 35] = A_(g_dsub[0])
    cpk[:, 36:38] = pk(g_mq[0], 2)
    cpk[:, 38:40] = pk(g_mk[0], 2)
    cbc = np.zeros((128, 576), f32)
    cbc[:, 0:64] = A_(lam_q1[0])[None, :]
    cbc[:, 64:128] = A_(lam_k1[0])[None, :]
    cbc[:, 128:192] = A_(lam_q2[0])[None, :]
    cbc[:, 192:256] = A_(lam_k2[0])[None, :]
    cbc[:, 256:320] = A_(g_dk[0])[None, :]
    cbc[:, 320:576] = A_(g_mk[0])[None, :]
    cfix = np.zeros((128, 320), f32)
    cfix[:, 0:128] = np.eye(128, dtype=f32)
    s_ = np.arange(128)[:, None] % 64
    t_ = np.arange(64)[None, :]
    cfix[:, 128:192] = (s_ <= t_).astype(f32)
    cfix[:, 192:256] = 1.0
    cfix[:, 192] = 0.0
    cfix[:, 256] = 1.0
    w_in_ = A_(w_in[0])
    w_mkv_ = A_(w_mkv[0])
    w_br_ = A_(w_branch[0]).reshape(3072, 1024)
    w_out_ = A_(w_out[0])
    in_maps = []
    for c in range(n_cores):
        sl = slice(c * NB, (c + 1) * NB)
        in_maps.append({
            "xp": A_(x_prompt[sl]).reshape(NB * SEQ, 1024),
            "xsm": A_(x_sample[sl]).reshape(256, 1024),
            "mp": A_(mem_prompt[sl]).reshape(NB * 256, 1024),
            "cdk": A_(cache_diff_k[0, sl]).reshape(NB * 1024, 1024),
            "cdv": A_(cache_diff_v[0, sl]).reshape(NB * 1024, 1024),
            "cmk": A_(cache_mem_k[0, sl]).reshape(NB * 256, 1024),
            "cmv": A_(cache_mem_v[0, sl]).reshape(NB * 256, 1024),
            "st0": A_(state_hgrn[0, sl]),
            "w_in": w_in_, "w_mkv": w_mkv_, "w_br": w_br_, "w_out": w_out_,
            "cpk": cpk, "cbc": cbc, "cfix": cfix,
        })
    return in_maps


def kernel(**inputs):
    f32 = np.float32
    if 'nc' not in _CACHE:
        _CACHE['nc'] = build_program()
    nc = _CACHE['nc']
    in_maps = pack_inputs(**inputs)
    res = run_bass_kernel_spmd(nc, in_maps, core_ids=list(range(8))).results
    cat = lambda k: np.concatenate([np.asarray(r[k], dtype=f32) for r in res], axis=0)
    y_p = cat("y_p").reshape(32, SEQ, 1024)
    y_s = cat("y_s").reshape(32, 64, 1024)
    st_p = cat("st_p").reshape(1, 32, 8, 128, 128)
    st_s = cat("st_s").reshape(1, 32, 8, 128, 128)
    dk_p = cat("dk_p").reshape(1, 32, SEQ, 8, 2, 64)
    dv_p = cat("dv_p").reshape(1, 32, SEQ, 8, 128)
    dk_s = cat("dk_s").reshape(1, 32, 64, 8, 2, 64)
    dv_s = cat("dv_s").reshape(1, 32, 64, 8, 128)
    mk_p = cat("mk_p").reshape(1, 32, 256, 4, 256)
    mv_p = cat("mv_p").reshape(1, 32, 256, 4, 256)
    return (y_p, y_s, st_p, st_s, dk_p, dv_p, dk_s, dv_s, mk_p, mv_p)
```

```python
import contextlib
import numpy as np
import concourse.bass as bass
import concourse.mybir as mybir
from concourse.bass_utils import run_bass_kernel_spmd
from concourse.alu_op_type import AluOpType as ALU

F32 = mybir.dt.float32
BF16 = mybir.dt.bfloat16
AF = mybir.ActivationFunctionType
AX = mybir.AxisListType
ENGS = ('pe', 'act', 'dve', 'pool', 'sp')
EPS = 1e-6
NB = 4
SEQ = 2048
NPIECE = 92
DBG_BR = {0, 1, 2}
PIPE_DEPTH = 3
STORE_DELAY = 2
CONV_AHEAD = 3
DBG_CLOSURE = False
DBG_MAXOPS = None
DBG_V = 0
DBG_SKIP = set()
CLOSURE_SNAPS = []


class Buf:
    __slots__ = ('name', 'w', 'r', 'psum')

    def __init__(self, name, psum=False):
        self.name = name
        self.w = None
        self.r = {}
        self.psum = psum


class Op:
    __slots__ = ('eng', 'fn', 'deps', 'inc', 'val', 'sem', 'dma', 'idx', 'clock', 'waits')


def _flat(x):
    out = []
    for b in x:
        if b is None:
            continue
        if isinstance(b, Buf):
            out.append(b)
        else:
            out.extend(_flat(b))
    return out


class Rec:
    def __init__(self):
        self.q = {e: [] for e in ENGS}
        self.dma_keys = {}

    def op(self, eng, fn, reads=(), writes=(), dma_key=None):
        self.nrec = getattr(self, 'nrec', 0) + 1
        if DBG_MAXOPS is not None and self.nrec > DBG_MAXOPS:
            return None
        if self.nrec in DBG_SKIP:
            return None
        if DBG_MAXOPS is not None and self.nrec == DBG_MAXOPS:
            print("LAST OP", eng, "line", fn.__code__.co_firstlineno)
        o = Op()
        o.eng = eng
        o.fn = fn
        o.inc = False
        o.val = None
        o.sem = None
        o.dma = dma_key
        o.idx = self.nrec
        if DBG_CLOSURE and fn.__closure__:
            o_snap = [id(c.cell_contents) for c in fn.__closure__]
            CLOSURE_SNAPS.append((fn, o_snap))
        deps = {}
        rl = _flat(reads)
        wl = _flat(writes)
        for b in rl:
            if b.w is not None:
                deps[id(b.w)] = b.w
            if b.psum:
                for k_, d in b.r.items():
                    if k_ != eng:
                        deps[id(d)] = d
        for b in wl:
            if b.w is not None:
                deps[id(b.w)] = b.w
            for d in b.r.values():
                deps[id(d)] = d
        if dma_key is not None:
            ent = self.dma_keys.setdefault(dma_key, [0, None])
            if ent[1] is not None:
                deps[id(ent[1])] = ent[1]
            ent[0] += 1
            ent[1] = o
            o.val = 16 * ent[0]
        dl = []
        for d in deps.values():
            if d is o:
                continue
            if d.dma is None and d.eng == 'pe' and eng == 'pe' and dma_key is None:
                continue
            d.inc = True
            dl.append(d)
        o.deps = dl
        rkey = eng if dma_key is None else ('dma', dma_key)
        for b in rl:
            b.r[rkey] = o
        for b in wl:
            b.w = o
            b.r = {}
        self.q[eng].append(o)
        return o

    def emit(self, nc, stack):
        esem = {e: stack.enter_context(nc.semaphore("sem_" + e)) for e in ENGS}
        dsem = {k: stack.enter_context(nc.semaphore("dsem_%d" % i)) for i, k in enumerate(self.dma_keys)}
        for e in ENGS:
            c = 0
            for o in self.q[e]:
                if o.dma is not None:
                    o.sem = dsem[o.dma]
                else:
                    o.sem = esem[e]
                    if o.inc:
                        c += 1
                        o.val = c
        know = {e: {} for e in ENGS}
        for o in sorted((o for e in ENGS for o in self.q[e]), key=lambda o: o.idx):
            kn = know[o.eng]
            o.waits = []
            for d in o.deps:
                if kn.get(id(d.sem), 0) < d.val:
                    o.waits.append((d.sem, d.val))
                    for kk, vv in d.clock.items():
                        if kn.get(kk, 0) < vv:
                            kn[kk] = vv
            if o.dma is not None or o.inc:
                o.clock = dict(kn)
                o.clock[id(o.sem)] = o.val
        engobj = {'pe': 'tensor', 'act': 'scalar', 'dve': 'vector', 'pool': 'gpsimd', 'sp': 'sync'}
        fin = [(dsem[k], 16 * v[0]) for k, v in self.dma_keys.items()]
        block = stack.enter_context(nc.Block())
        for e in ENGS:
            q = self.q[e]

            def body(eng, q=q, e=e):
                for o in q:
                    for ws_, wv_ in o.waits:
                        eng.wait_ge(ws_, wv_)
                    inst = o.fn(eng)
                    if o.dma is not None:
                        inst.then_inc(o.sem, 16)
                    elif o.inc:
                        inst.then_inc(o.sem, 1)
                if e == 'sp':
                    for s, v in fin:
                        eng.wait_ge(s, v)
            getattr(block, engobj[e])(body)


class Reg:
    def __init__(self, ar, p0, n):
        self.ar = ar
        self.p0 = p0
        self.n = n

    def bf(self, c0=0, c1=None):
        c1 = self.n * 512 if c1 is None else c1
        return self.ar.t[:, self.p0 * 512 + c0:self.p0 * 512 + c1]

    def f(self, c0=0, c1=None):
        c1 = self.n * 256 if c1 is None else c1
        return self.ar.tf[:, self.p0 * 256 + c0:self.p0 * 256 + c1]

    def rows_bf(self, r0, r1, c0, c1):
        return self.ar.t[r0:r1, self.p0 * 512 + c0:self.p0 * 512 + c1]

    @property
    def b(self):
        return self.ar.bufs[self.p0:self.p0 + self.n]

    def bb(self, c0, c1):
        return self.ar.bufs[self.p0 + c0 // 512:self.p0 + (c1 - 1) // 512 + 1]

    def bF(self, c0, c1):
        return self.ar.bufs[self.p0 + c0 // 256:self.p0 + (c1 - 1) // 256 + 1]

    def free(self):
        self.ar.release(self)


class Arena:
    def __init__(self, t, n):
        self.t = t
        self.tf = t.bitcast(F32)
        self.n = n
        self.used = [False] * n
        self.bufs = [Buf("a%d" % i) for i in range(n)]
        self.ptr = 0
        self.peak = 0

    def alloc(self, k):
        n = self.n
        for s in range(n):
            p = (self.ptr + s) % n
            if p + k > n:
                continue
            if not any(self.used[p:p + k]):
                for i in range(p, p + k):
                    self.used[i] = True
                self.ptr = (p + k) % n
                self.peak = max(self.peak, sum(self.used))
                return Reg(self, p, k)
        raise RuntimeError("arena full: want %d, used %d" % (k, sum(self.used)))

    def release(self, r):
        for i in range(r.p0, r.p0 + r.n):
            assert self.used[i]
            self.used[i] = False


def bview(ap, g, j):
    return ap.rearrange("p (g j) -> p g j", g=g, j=j)


def bcast_mid(ap, g):
    return ap.unsqueeze(1).to_broadcast([ap.shape[0], g, ap.shape[1]])


def bcast_last(ap, j):
    return ap.unsqueeze(2).to_broadcast([ap.shape[0], ap.shape[1], j])


def build_program(nb_run=NB, ntiles=4, do_sample=True):
    nc = bass.Bass("TRN2", target_bir_lowering=False)
    R = Rec()

    def din(name, shape):
        return nc.dram_tensor(name, shape, F32, kind="ExternalInput").ap()

    def dout(name, shape):
        return nc.dram_tensor(name, shape, F32, kind="ExternalOutput").ap()

    xp = din("xp", [NB * SEQ, 1024])
    xsm = din("xsm", [256, 1024])
    mp = din("mp", [NB * 256, 1024])
    cdk = din("cdk", [NB * 1024, 1024])
    cdv = din("cdv", [NB * 1024, 1024])
    cmk = din("cmk", [NB * 256, 1024])
    cmv = din("cmv", [NB * 256, 1024])
    st0 = din("st0", [NB, 8, 128, 128])
    w_in = din("w_in", [1024, 13312])
    w_mkv = din("w_mkv", [1024, 2048])
    w_br = din("w_br", [3072, 1024])
    w_out = din("w_out", [1024, 1024])
    cpk = din("cpk", [128, 40])
    cbc = din("cbc", [128, 576])
    cfix = din("cfix", [128, 320])

    y_p = dout("y_p", [NB * SEQ, 1024])
    y_s = dout("y_s", [256, 1024])
    st_p = dout("st_p", [NB, 8, 128, 128])
    st_s = dout("st_s", [NB, 8, 128, 128])
    dk_p = dout("dk_p", [NB * SEQ, 1024])
    dv_p = dout("dv_p", [NB * SEQ, 1024])
    dk_s = dout("dk_s", [256, 1024])
    dv_s = dout("dv_s", [256, 1024])
    mk_p = dout("mk_p", [NB * 256, 1024])
    mv_p = dout("mv_p", [NB * 256, 1024])

    wb_in = nc.dram_tensor("wb_in", [1024, 13312], BF16, kind="Internal").ap()
    wb_mkv = nc.dram_tensor("wb_mkv", [1024, 2048], BF16, kind="Internal").ap()
    wb_br = nc.dram_tensor("wb_br", [3072, 1024], BF16, kind="Internal").ap()
    wb_out = nc.dram_tensor("wb_out", [1024, 1024], BF16, kind="Internal").ap()
    B_wb = {'in': Buf("wb_in"), 'mkv': Buf("wb_mkv"), 'br': Buf("wb_br"), 'out': Buf("wb_out")}

    with contextlib.ExitStack() as st:
        def sb(name, shape, dt):
            return st.enter_context(nc.sbuf_tensor(name, shape, dt))

        KT = sb("KT", [128, 8 * SEQ], BF16)
        B_KT = [[Buf("KT%d_%d" % (h, j)) for j in range(4)] for h in range(8)]
        VA = sb("VA", [128, 16 * 1040], BF16)
        B_VA = [Buf("VA%d" % i) for i in range(16)]
        KM = sb("KM", [128, 8 * 256], BF16)
        B_KM = Buf("KM")
        VM = sb("VM", [128, 2 * 1032], BF16)
        B_VM = [Buf("VM0"), Buf("VM1")]
        WS = [sb("ws%d" % i, [128, 8 * 512], BF16) for i in range(3)]
        B_WS = [Buf("ws%d" % i) for i in range(3)]
        SF = sb("SF", [128, 1024], F32)
        SB = sb("SB", [128, 1024], BF16)
        B_SF = [Buf("SF%d" % h) for h in range(8)]
        B_SB = [Buf("SB%d" % h) for h in range(8)]
        CPK = sb("CPK", [128, 40], F32)
        CBC = sb("CBC", [128, 576], F32)
        CFX = sb("CFX", [128, 192], F32)
        IDB = sb("IDB", [128, 128], BF16)
        MHG = sb("MHG", [128, 64], BF16)
        DER = sb("DER", [128, 64], F32)
        MH = sb("MH", [128, 16], F32)
        AEND = sb("AEND", [128, 64], F32)
        B_AEND = [Buf("AEND%d" % h) for h in range(8)]
        FM1 = [sb("FM1_%d" % i, [128, 512], F32) for i in range(2)]
        B_FM1 = [Buf("FM1_0"), Buf("FM1_1")]
        SM = sb("SM", [128, 512], F32)
        B_SM = [Buf("SM%d" % i) for i in range(32)]
        B_C = Buf("consts")
        art = sb("arena", [128, NPIECE * 512], BF16)
        A = Arena(art, NPIECE)
        PS = [st.enter_context(nc.psum_tensor("ps%d" % i, [128, 512], F32)) for i in range(8)]
        PSB = [p.bitcast(BF16) for p in PS]
        B_PS = [Buf("ps%d" % i, True) for i in range(8)]
        B_TR = [Buf("tr0", True), Buf("tr1", True)]
        state = {'bk': 0, 'wide': True, 'sm': 0, 'fm1': 0}

        def bank_next():
            bset = (0, 1, 2, 3, 4, 5, 6, 7) if state['wide'] else (0, 1, 2, 3)
            i = bset[state['bk'] % len(bset)]
            state['bk'] += 1
            return i

        def mm_next():
            i = bank_next()
            return PS[i], B_PS[i]

        def tr_next():
            i = bank_next()
            return PSB[i][:, 0:512], B_PS[i]

        def set_wide(w):
            state['wide'] = w

        def acc(i):
            return PS[4 + i], B_PS[4 + i]

        def sm_next():
            i = state['sm']
            state['sm'] = (i + 1) % 32
            return SM[:, i * 16:(i + 1) * 16], B_SM[i]

        C1, C0, NC1 = 0, 8, 16
        GHG, GQ8, GK2, GDS, LAMN = 24, 25, 26, 27, 28
        GMQ = 29
        GMKP = 31
        P_GN, P_GM, P_L0, P_L1, P_GHG, P_GQ, P_GK, P_GDS, P_GMQ, P_GMK = 0, 8, 16, 24, 32, 33, 34, 35, 36, 38

        R.op('sp', lambda e: e.dma_start(out=CPK[:], in_=cpk[:, :]), writes=[B_C], dma_key="c0")
        R.op('sp', lambda e: e.dma_start(out=CBC[:], in_=cbc[:, :]), writes=[B_C], dma_key="c1")
        R.op('sp', lambda e: e.dma_start(out=CFX[:, 0:128], in_=cfix[:, 192:320]), writes=[B_C], dma_key="c2")
        R.op('pool', lambda e: e.dma_start(out=IDB[:], in_=cfix[:, 0:128]), writes=[B_C], dma_key="c3")
        R.op('pool', lambda e: e.dma_start(out=MHG[:], in_=cfix[:, 128:192]), writes=[B_C], dma_key="c4")
        R.op('pool', lambda e: e.memset(MH[:], -0.5), writes=[B_C])
        for i in range(2):
            R.op('pool', lambda e, i=i: e.memset(FM1[i][:], 0.0), writes=[B_FM1[i]])
        R.op('pool', lambda e: e.memset(AEND[:], 1.0), writes=B_AEND)
        R.op('dve', lambda e: e.tensor_tensor(out=DER[:, 32:40], in0=CPK[:, P_L0:P_L0 + 8], in1=CPK[:, P_L1:P_L1 + 8],
                                              op=ALU.subtract), reads=[B_C], writes=[B_C])
        R.op('act', lambda e: e.activation(out=DER[:, 40:48], in_=DER[:, 32:40], func=AF.Tanh, scale=0.5),
             reads=[B_C], writes=[B_C])
        R.op('dve', lambda e: e.tensor_scalar(out=DER[:, C1:C1 + 8], in0=DER[:, 40:48], scalar1=-0.25, scalar2=0.25,
                                              op0=ALU.mult, op1=ALU.add), reads=[B_C], writes=[B_C])
        R.op('dve', lambda e: e.tensor_scalar(out=DER[:, C0:C0 + 8], in0=DER[:, 40:48], scalar1=0.25, scalar2=0.75,
                                              op0=ALU.mult, op1=ALU.add), reads=[B_C], writes=[B_C])
        R.op('dve', lambda e: e.tensor_scalar(out=DER[:, NC1:NC1 + 8], in0=DER[:, 40:48], scalar1=0.25, scalar2=-0.25,
                                              op0=ALU.mult, op1=ALU.add), reads=[B_C], writes=[B_C])

        def cscale(dst, src, n, s):
            R.op('dve', lambda e: e.tensor_scalar(out=DER[:, dst:dst + n], in0=CPK[:, src:src + n], scalar1=s,
                                                  scalar2=None, op0=ALU.mult), reads=[B_C], writes=[B_C])
        cscale(GHG, P_GHG, 1, 0.5)
        cscale(GQ8, P_GQ, 1, 0.125)
        cscale(GK2, P_GK, 1, 1.0)
        cscale(GDS, P_GDS, 1, 0.4)
        cscale(GMQ, P_GMQ, 2, 1.0 / 16.0)
        cscale(GMKP, P_GMK, 2, 1.0)
        R.op('dve', lambda e: e.tensor_tensor(out=DER[:, 48:64].bitcast(F32), in0=CBC[:, 0:16], in1=CBC[:, 0:16], op=ALU.mult),
             reads=[B_C], writes=[B_C])
        sc1 = A.alloc(1)
        R.op('dve', lambda e: e.tensor_tensor(out=sc1.f(0, 64), in0=CBC[:, 0:64], in1=CBC[:, 64:128], op=ALU.mult),
             reads=[B_C], writes=sc1.b)
        R.op('dve', lambda e: e.tensor_tensor(out=sc1.f(64, 128), in0=CBC[:, 128:192], in1=CBC[:, 192:256], op=ALU.mult),
             reads=[B_C], writes=sc1.b)
        R.op('dve', lambda e: e.tensor_reduce(out=DER[:, 48:50], in_=bview(sc1.f(0, 128), 2, 64), axis=AX.X, op=ALU.add),
             reads=sc1.b, writes=[B_C])
        R.op('act', lambda e: e.activation(out=DER[:, 50:52], in_=DER[:, 48:50], func=AF.Exp), reads=[B_C], writes=[B_C])
        R.op('dve', lambda e: e.scalar_tensor_tensor(out=DER[:, LAMN:LAMN + 1], in0=DER[:, 51:52], scalar=-0.2,
                                                     in1=DER[:, 50:51], op0=ALU.add, op1=ALU.subtract),
             reads=[B_C], writes=[B_C])
        sc1.free()
        for blk in range(16):
            R.op('pool', lambda e, blk=blk: e.memset(
                bview(VA[:, blk * 1040:(blk + 1) * 1040], 8, 130)[:, :, 128:129], 1.0), writes=[B_VA[blk]])
        for kb in range(2):
            R.op('pool', lambda e, kb=kb: e.memset(
                bview(VM[:, kb * 1032:(kb + 1) * 1032], 4, 258)[:, :, 256:257], 1.0), writes=[B_VM[kb]])

        def col(c):
            return DER[:, c:c + 1]

        WSRC = {'in': wb_in, 'mkv': wb_mkv, 'br': wb_br, 'out': wb_out}
        plan = []

        def plan_tile(kind):
            p = []
            if kind == 'mem':
                p += [('mkv', 0, c) for c in range(0, 2048, 512)]
                return p
            p += [('in', 0, c) for c in range(0, 4096, 512)]
            for n in range(3):
                if n == 1:
                    for g in range(2):
                        p += [('in', 0, 4096 + g * 512), ('in', 0, 5120 + g * 512),
                              ('in', 0, 6144 + g * 512), ('in', 0, 7168 + g * 512)]
                if n == 2:
                    p += [('in', 0, c) for c in range(8192, 10240, 512)]
                p += [('in', 0, 10240 + n * 1024 + g * 512) for g in range(2)]
                p += [('br', n * 1024, g * 512) for g in range(2)]
            p += [('out', 0, 0), ('out', 0, 512)]
            return p

        for b in range(nb_run):
            plan += plan_tile('mem')
            for j in range(ntiles):
                plan += plan_tile('main')
        if do_sample:
            plan += plan_tile('main')
        ws_state = {'issued': 0, 'cur': 0}

        WF32 = {'in': w_in, 'mkv': w_mkv, 'br': w_br, 'out': w_out}
        B_cvt = {}
        conv_state = {'next': 0}

        def conv_ensure(n):
            while conv_state['next'] < min(n, len(plan)):
                key = plan[conv_state['next']]
                conv_state['next'] += 1
                if key in B_cvt:
                    continue
                src, r0, c0 = key
                B_cvt[key] = Buf("cvt_%s_%d_%d" % key)
                R.op('pool', lambda e, src=src, r0=r0, c0=c0: e.dma_start(
                    out=WSRC[src][r0:r0 + 1024, c0:c0 + 512], in_=WF32[src][r0:r0 + 1024, c0:c0 + 512]),
                    writes=[B_cvt[key]], dma_key="wc%d" % (len(B_cvt) % 2))

        def ws_issue_upto(n):
            while ws_state['issued'] < min(n, len(plan)):
                i = ws_state['issued']
                conv_ensure(i + 1 + CONV_AHEAD)
                src, r0, c0 = plan[i]
                s = i % 3
                srcap = WSRC[src][r0:r0 + 1024, c0:c0 + 512].rearrange("(kc p) c -> p kc c", p=128)
                R.op('sp', lambda e, s=s, srcap=srcap: e.dma_start(
                    out=WS[s][:].rearrange("p (kc c) -> p kc c", kc=8), in_=srcap),
                    reads=[B_cvt[plan[i]]], writes=[B_WS[s]], dma_key="w%d" % s)
                ws_state['issued'] += 1

        pending = []

        def defer(fn, n=STORE_DELAY):
            pending.append([n, fn])

        def flush_pending(everything=False):
            for it in list(pending):
                it[0] -= 1
                if everything or it[0] <= 0:
                    pending.remove(it)
                    it[1]()

        def ws_take(src, r0, c0):
            i = ws_state['cur']
            assert plan[i] == (src, r0, c0), (i, plan[i], (src, r0, c0))
            ws_issue_upto(i + 3)
            flush_pending()
            ws_state['cur'] += 1
            s = i % 3
            return WS[s], B_WS[s]

        def wcol(wt, kc, c0, c1):
            return wt[:, kc * 512 + c0:kc * 512 + c1]

        def rstd_into(dst_ap, dst_b, ss_ap, ss_b, n_groups, inv_n):
            tmp, tb_ = sm_next()
            R.op('dve', lambda e: e.tensor_scalar(out=tmp[:, 0:n_groups], in0=ss_ap, scalar1=inv_n, scalar2=EPS,
                                                  op0=ALU.mult, op1=ALU.add), reads=[ss_b], writes=[tb_])
            R.op('pool', lambda e: e.tensor_tensor(out=dst_ap, in0=tmp[:, 0:n_groups], in1=MH[:, 0:n_groups], op=ALU.pow),
                 reads=[tb_, B_C], writes=[dst_b])

        def group_stats(ps_ap, ps_b, g, j):
            junk = A.alloc(1)
            R.op('act', lambda e: e.activation(out=junk.bf(0, g * j), in_=ps_ap, func=AF.Square), reads=[ps_b], writes=junk.b)
            ss, ssb = sm_next()
            R.op('dve', lambda e: e.tensor_reduce(out=ss[:, 0:g], in_=bview(junk.bf(0, g * j), g, j), axis=AX.X, op=ALU.add),
                 reads=junk.b, writes=[ssb])
            junk.free()
            r, rb = sm_next()
            rstd_into(r[:, 0:g], rb, ss[:, 0:g], ssb, g, 1.0 / j)
            return r, rb

        def xstage_a(src_rows, TB):
            xn = []
            for tb in range(TB):
                xs = A.alloc(4)
                R.op('sp', lambda e, xs=xs, tb=tb: e.dma_start(out=xs.f(0, 1024), in_=src_rows(tb)), writes=xs.b,
                     dma_key="xin%d" % (tb % 2))
                junk = A.alloc(2)
                ss, ssb = sm_next()
                R.op('act', lambda e, xs=xs, junk=junk, ss=ss: e.activation(out=junk.bf(0, 1024), in_=xs.f(0, 1024),
                                                                           func=AF.Square, accum_out=ss[:, 0:1]),
                     reads=xs.b, writes=junk.b + [ssb])
                junk.free()
                r, rb = sm_next()
                rstd_into(r[:, 0:1], rb, ss[:, 0:1], ssb, 1, 1.0 / 1024)
                xb = A.alloc(2)
                R.op('act', lambda e, xs=xs, xb=xb, r=r: e.activation(out=xb.bf(0, 1024), in_=xs.f(0, 1024), func=AF.Copy,
                                                                      scale=r[:, 0:1]), reads=xs.b + [rb], writes=xb.b)
                xs.free()
                xn.append(xb)
            return xn

        def xstage_b(xn, TB, gcol):
            NT = TB * 128
            xnT = []
            for kc in range(8):
                tr, trb = tr_next()
                for tb in range(TB):
                    R.op('pe', lambda e, tr=tr, tb=tb, kc=kc: e.transpose(
                        tr[:, tb * 128:(tb + 1) * 128], xn[tb].bf(kc * 128, (kc + 1) * 128), IDB[:]),
                        reads=xn[tb].b + [B_C], writes=[trb])
                p = A.alloc(1)
                R.op('dve', lambda e, tr=tr, p=p, kc=kc: e.tensor_scalar(
                    out=p.bf(0, NT), in0=tr[:, 0:NT], scalar1=CPK[:, gcol + kc:gcol + kc + 1], scalar2=None, op0=ALU.mult),
                    reads=[trb, B_C], writes=p.b)
                xnT.append(p)
            for x_ in xn:
                x_.free()
            return xnT

        def fproj(src, r0, c0, rhs, rhs_bufs, NT, consume):
            wt, wb_ = ws_take(src, r0, c0)
            for ch in range(4):
                ps, psb_ = mm_next()
                for kc in range(8):
                    R.op('pe', lambda e, ps=ps, kc=kc, ch=ch: e.matmul(
                        ps[:, 0:NT], lhsT=wcol(wt, kc, ch * 128, (ch + 1) * 128), rhs=rhs[kc].bf(0, NT),
                        start=(kc == 0), stop=(kc == 7)),
                        reads=[wb_] + rhs_bufs[kc], writes=[psb_])
                consume(ch, ps, psb_)

        def tproj(src, r0, c0, xnT, TB, consume):
            wt, wb_ = ws_take(src, r0, c0)
            for tb in range(TB):
                ps, psb_ = mm_next()
                for kc in range(8):
                    R.op('pe', lambda e, ps=ps, kc=kc, tb=tb: e.matmul(
                        ps[:, 0:512], lhsT=xnT[kc].bf(tb * 128, (tb + 1) * 128), rhs=wcol(wt, kc, 0, 512),
                        start=(kc == 0), stop=(kc == 7)),
                        reads=[wb_] + xnT[kc].b, writes=[psb_])
                consume(tb, ps, psb_)

        def transpose_blocks(srcs, c0, TB, evac):
            tr, trb = tr_next()
            for tb in range(TB):
                R.op('pe', lambda e, tb=tb: e.transpose(tr[:, tb * 128:(tb + 1) * 128], srcs[tb].bf(c0, c0 + 128), IDB[:]),
                     reads=srcs[tb].bb(c0, c0 + 128) + [B_C], writes=[trb])
            evac(tr, trb)

        def gate_chunk(ps, psb_, NT):
            tz = A.alloc(1)
            R.op('act', lambda e: e.activation(out=tz.bf(0, NT), in_=ps[:, 0:NT], func=AF.Tanh, scale=0.5),
                 reads=[psb_], writes=tz.b)
            u = A.alloc(1)
            R.op('dve', lambda e: e.scalar_tensor_tensor(out=u.bf(0, NT), in0=tz.bf(0, NT), scalar=1.0, in1=ps[:, 0:NT],
                                                         op0=ALU.add, op1=ALU.mult), reads=tz.b + [psb_], writes=u.b)
            tz.free()
            return u

        def norm_rows_bf(src_regs, TB, g, j):
            outs = []
            for tb in range(TB):
                junk = A.alloc(2)
                R.op('act', lambda e, tb=tb, junk=junk: e.activation(out=junk.bf(0, 1024), in_=src_regs[tb].bf(0, 1024),
                                                                     func=AF.Square), reads=src_regs[tb].b, writes=junk.b)
                ss, ssb = sm_next()
                R.op('dve', lambda e, junk=junk, ss=ss: e.tensor_reduce(out=ss[:, 0:g], in_=bview(junk.bf(0, 1024), g, j),
                                                                        axis=AX.X, op=ALU.add), reads=junk.b, writes=[ssb])
                junk.free()
                r, rb = sm_next()
                rstd_into(r[:, 0:g], rb, ss[:, 0:g], ssb, g, 1.0 / j)
                o = A.alloc(2)
                R.op('dve', lambda e, tb=tb, o=o, r=r: e.tensor_tensor(
                    out=bview(o.bf(0, 1024), g, j), in0=bview(src_regs[tb].bf(0, 1024), g, j), in1=bcast_last(r[:, 0:g], j),
                    op=ALU.mult), reads=src_regs[tb].b + [rb], writes=o.b)
                outs.append(o)
            return outs

        def merge_gates(n, NT):
            tgs = []
            for g in range(2):
                def cons_g(ch, ps, psb_):
                    tg = A.alloc(1)
                    R.op('act', lambda e: e.activation(out=tg.bf(0, NT), in_=ps[:, 0:NT], func=AF.Tanh, scale=0.5),
                         reads=[psb_], writes=tg.b)
                    tgs.append(tg)
                fproj('in', 0, 10240 + n * 1024 + g * 512, XNT['r'], XNT['b'], NT, cons_g)
            return tgs

        def merge_proj(n, yg, NT, hacc, tgs):
            yg_b = [p.b for p in yg]
            for g in range(2):
                def cons_b(ch, ps, psb_):
                    dch = g * 4 + ch
                    tg = tgs[dch]
                    if n not in DBG_BR:
                        if n == 0:
                            R.op('pool', lambda e: e.memset(hacc[dch].bf(0, NT), 0.0), writes=hacc[dch].b)
                        R.op('dve', lambda e: e.tensor_copy(out=tg.bf(0, NT), in_=ps[:, 0:NT]), reads=[psb_], writes=tg.b)
                    elif n == 0:
                        R.op('dve', lambda e: e.scalar_tensor_tensor(out=hacc[dch].bf(0, NT), in0=tg.bf(0, NT), scalar=1.0,
                                                                     in1=ps[:, 0:NT], op0=ALU.add, op1=ALU.mult),
                             reads=tg.b + [psb_], writes=hacc[dch].b)
                    else:
                        tmp = A.alloc(1)
                        R.op('dve', lambda e: e.scalar_tensor_tensor(out=tmp.bf(0, NT), in0=tg.bf(0, NT), scalar=1.0,
                                                                     in1=ps[:, 0:NT], op0=ALU.add, op1=ALU.mult),
                             reads=tg.b + [psb_], writes=tmp.b)
                        R.op('pool', lambda e: e.tensor_tensor(out=hacc[dch].bf(0, NT), in0=hacc[dch].bf(0, NT),
                                                               in1=tmp.bf(0, NT), op=ALU.add),
                             reads=tmp.b + hacc[dch].b, writes=hacc[dch].b)
                        tmp.free()
                    tg.free()
                fproj('br', n * 1024, g * 512, yg, yg_b, NT, cons_b)

        XNT = {}

        def hgrn_stage(NT, TB, sample, b):
            xnT, xb_ = XNT['r'], XNT['b']
            NCH = NT // 64
            qraw = [None] * 8
            qdec = [None] * 8
            kinv = [None] * 8
            ua = [None] * 8

            def cons_q(base):
                def f(ch, ps, psb_):
                    h = base + ch
                    q = A.alloc(1)
                    R.op('act', lambda e: e.activation(out=q.bf(0, NT), in_=ps[:, 0:NT], func=AF.Copy), reads=[psb_], writes=q.b)
                    qraw[h] = q
                return f
            for g in range(2):
                fproj('in', 0, g * 512, xnT, xb_, NT, cons_q(g * 4))

            def cons_f(base):
                def f(ch, ps, psb_):
                    h = base + ch
                    t = A.alloc(2)
                    R.op('act', lambda e: e.activation(out=t.f(0, NT), in_=ps[:, 0:NT], func=AF.Tanh, scale=0.5),
                         reads=[psb_], writes=t.b)
                    fm0 = A.alloc(2)
                    R.op('act', lambda e: e.activation(out=fm0.f(0, NT), in_=t.f(0, NT), func=AF.Identity,
                                                       scale=col(C1 + h), bias=col(C0 + h)),
                         reads=t.b + [B_C], writes=fm0.b)
                    pi = state['fm1']
                    state['fm1'] = 1 - pi
                    fm1, fm1b = FM1[pi], B_FM1[pi]
                    st0v = bview(fm0.f(0, NT), NCH, 64)[:, :, 0]
                    R.op('dve', lambda e: e.tensor_copy(out=bview(fm1[:, 0:NT], NCH, 64)[:, :, 0], in_=st0v),
                         reads=fm0.b, writes=[fm1b])
                    R.op('dve', lambda e: e.memset(st0v, 0.0), writes=fm0.b)
                    Acp = A.alloc(2)
                    R.op('dve', lambda e: e.tensor_tensor_scan(out=Acp.f(0, NT), data0=fm0.f(0, NT), data1=fm1[:, 0:NT],
                                                               initial=0.0, op0=ALU.mult, op1=ALU.add),
                         reads=fm0.b + [fm1b], writes=Acp.b)
                    fm0.free()
                    R.op('dve', lambda e: e.tensor_copy(out=AEND[:, h * 8:h * 8 + NCH],
                                                        in_=bview(Acp.f(0, NT), NCH, 64)[:, :, 63]),
                         reads=Acp.b, writes=[B_AEND[h]])
                    rA = A.alloc(2)
                    R.op('dve', lambda e: e.reciprocal(out=rA.f(0, NT), in_=Acp.f(0, NT)), reads=Acp.b, writes=rA.b)
                    k_ = A.alloc(2)
                    R.op('act', lambda e: e.activation(out=k_.f(0, NT), in_=t.f(0, NT), func=AF.Identity,
                                                       scale=col(NC1 + h), bias=col(C1 + h)),
                         reads=t.b + [B_C], writes=k_.b)
                    t.free()
                    ki = A.alloc(1)
                    R.op('pool', lambda e: e.tensor_tensor(out=ki.bf(0, NT), in0=k_.f(0, NT), in1=rA.f(0, NT), op=ALU.mult),
                         reads=k_.b + rA.b, writes=ki.b)
                    k_.free()
                    rA.free()
                    qd = A.alloc(1)
                    R.op('pool', lambda e: e.tensor_tensor(out=qd.bf(0, NT), in0=qraw[h].bf(0, NT), in1=Acp.f(0, NT), op=ALU.mult),
                         reads=qraw[h].b + Acp.b, writes=qd.b)
                    Acp.free()
                    qraw[h].free()
                    kinv[h] = ki
                    qdec[h] = qd
                return f
            for g in range(2):
                fproj('in', 0, 1024 + g * 512, xnT, xb_, NT, cons_f(g * 4))

            vhg = [A.alloc(2) for _ in range(TB)]

            def cons_v(half):
                def f(tb, ps, psb_):
                    R.op('act', lambda e: e.activation(out=vhg[tb].bf(half * 512, half * 512 + 512), in_=ps[:, 0:512], func=AF.Copy),
                         reads=[psb_], writes=vhg[tb].bb(half * 512, half * 512 + 512))
                return f
            for g in range(2):
                tproj('in', 0, 2048 + g * 512, xnT, TB, cons_v(g))

            def cons_z(base):
                def f(ch, ps, psb_):
                    ua[base + ch] = gate_chunk(ps, psb_, NT)
                return f
            for g in range(2):
                fproj('in', 0, 3072 + g * 512, xnT, xb_, NT, cons_z(g * 4))

            set_wide(False)
            kinvT = [A.alloc(2) for _ in range(TB)]
            for tb in range(TB):
                for half in range(2):
                    tr, trb = tr_next()
                    for hh in range(4):
                        h = half * 4 + hh
                        R.op('pe', lambda e, tr=tr, hh=hh, h=h, tb=tb: e.transpose(
                            tr[:, hh * 128:(hh + 1) * 128], kinv[h].bf(tb * 128, (tb + 1) * 128), IDB[:]),
                            reads=kinv[h].b + [B_C], writes=[trb])
                    R.op('act', lambda e, tr=tr, tb=tb, half=half: e.activation(
                        out=kinvT[tb].bf(half * 512, half * 512 + 512), in_=tr[:, 0:512], func=AF.Copy),
                        reads=[trb], writes=kinvT[tb].bb(half * 512, half * 512 + 512))

            on = []
            pend_norm = []
            for tb in range(TB):
                for cc in range(2):
                    c = tb * 2 + cc
                    r0, r1 = cc * 64, cc * 64 + 64
                    if sample:
                        R.op('sp', lambda e, c=c: e.dma_start(out=SF[:].rearrange("p (h v) -> p h v", h=8),
                                                              in_=st0[c].rearrange("h k v -> k h v")),
                             writes=B_SF, dma_key="sld")
                        R.op('act', lambda e: e.activation(out=SB[:], in_=SF[:], func=AF.Copy), reads=B_SF, writes=B_SB)
                    ps1, ps1b = PS[2 + c % 2], B_PS[2 + c % 2]
                    ob = (4, 5) if tb % 2 == 0 else (0, 1)
                    for h in range(8):
                        R.op('pe', lambda e, h=h, c=c, ps1=ps1, r0=r0, r1=r1: e.matmul(
                            ps1[r0:r1, h * 64:(h + 1) * 64], lhsT=kinv[h].bf(c * 64, c * 64 + 64),
                            rhs=qdec[h].bf(c * 64, c * 64 + 64), start=True, stop=True),
                            reads=kinv[h].b + qdec[h].b, writes=[ps1b])
                    attm = A.alloc(1)
                    R.op('dve', lambda e, ps1=ps1, attm=attm, r0=r0, r1=r1: e.tensor_tensor(
                        out=bview(attm.rows_bf(r0, r1, 0, 512), 8, 64), in0=bview(ps1[r0:r1, 0:512], 8, 64),
                        in1=bcast_mid(MHG[r0:r1, :], 8), op=ALU.mult), reads=[ps1b, B_C], writes=attm.b)
                    while pend_norm:
                        pend_norm.pop(0)()
                    for h in range(8):
                        pa, pab = acc(2 + h // 4)
                        cs = (h % 4) * 128
                        R.op('pe', lambda e, h=h, pa=pa, cs=cs, tb=tb, r0=r0, r1=r1: e.matmul(
                            pa[:, cs:cs + 128], lhsT=kinvT[tb].rows_bf(r0, r1, h * 128, h * 128 + 128),
                            rhs=vhg[tb].rows_bf(r0, r1, h * 128, h * 128 + 128), start=True, stop=True),
                            reads=kinvT[tb].b + vhg[tb].b, writes=[pab])
                    for h in range(8):
                        pa, pab = PS[ob[h // 4]], B_PS[ob[h // 4]]
                        cs = (h % 4) * 128
                        R.op('pe', lambda e, h=h, pa=pa, cs=cs, tb=tb, attm=attm, r0=r0, r1=r1: e.matmul(
                            pa[r0:r1, cs:cs + 128], lhsT=attm.rows_bf(r0, r1, h * 64, h * 64 + 64),
                            rhs=vhg[tb].rows_bf(r0, r1, h * 128, h * 128 + 128), start=True, stop=False),
                            reads=attm.b + vhg[tb].b, writes=[pab])
                        R.op('pe', lambda e, h=h, pa=pa, cs=cs, c=c, r0=r0, r1=r1: e.matmul(
                            pa[r0:r1, cs:cs + 128], lhsT=qdec[h].bf(c * 64, c * 64 + 64),
                            rhs=SB[:, h * 128:(h + 1) * 128], start=False, stop=True),
                            reads=qdec[h].b + [B_SB[h]], writes=[pab])
                    attm.free()
                    for hb in range(2):
                        pa, pab = acc(2 + hb)
                        sfv = SF[:, hb * 512:(hb + 1) * 512]
                        aeb = AEND[:, hb * 32:(hb + 1) * 32].rearrange("p (h c) -> p h c", c=8)[:, :, c]
                        bsf = B_SF[hb * 4:hb * 4 + 4]
                        R.op('dve', lambda e, sfv=sfv, pa=pa: e.tensor_tensor(out=sfv, in0=sfv, in1=pa[:, 0:512], op=ALU.add),
                             reads=[pab] + bsf, writes=bsf)
                        R.op('dve', lambda e, sfv=sfv, aeb=aeb: e.tensor_tensor(
                            out=bview(sfv, 4, 128), in0=bview(sfv, 4, 128), in1=bcast_last(aeb, 128), op=ALU.mult),
                            reads=bsf + B_AEND[hb * 4:hb * 4 + 4], writes=bsf)
                        R.op('act', lambda e, sfv=sfv, hb=hb: e.activation(out=SB[:, hb * 512:(hb + 1) * 512], in_=sfv, func=AF.Copy),
                             reads=bsf, writes=B_SB[hb * 4:hb * 4 + 4])
                    if sample:
                        R.op('sp', lambda e, c=c: e.dma_start(out=st_s[c].rearrange("h k v -> k h v"),
                                                              in_=SF[:].rearrange("p (h v) -> p h v", h=8)),
                             reads=B_SF, dma_key="sst")
                def norm_o(ob=ob):
                    o_n = A.alloc(2)
                    for i in range(2):
                        pa, pab = PS[ob[i]], B_PS[ob[i]]
                        r, rb = group_stats(pa[:, 0:512], pab, 4, 128)
                        R.op('dve', lambda e, pa=pa, o_n=o_n, i=i, r=r: e.tensor_tensor(
                            out=bview(o_n.bf(i * 512, i * 512 + 512), 4, 128), in0=bview(pa[:, 0:512], 4, 128),
                            in1=bcast_last(r[:, 0:4], 128), op=ALU.mult), reads=[pab, rb], writes=o_n.bb(i * 512, i * 512 + 512))
                    on.append(o_n)
                pend_norm.append(norm_o)
            while pend_norm:
                pend_norm.pop(0)()
            if (not sample) and b is not None:
                R.op('sp', lambda e: e.dma_start(out=st_p[b].rearrange("h k v -> k h v"),
                                                 in_=SF[:].rearrange("p (h v) -> p h v", h=8)), reads=B_SF, dma_key="sst")
            for x_ in kinv + qdec + vhg + kinvT:
                x_.free()
            set_wide(True)
            return on, ua

        def gated_y(src, us, TB, NT, scale):
            ys = []
            for c in range(8):
                y = A.alloc(1)

                def ev(tr, trb, y=y, c=c):
                    R.op('dve', lambda e: e.scalar_tensor_tensor(out=y.bf(0, NT), in0=tr[:, 0:NT], scalar=scale,
                                                                 in1=us[c].bf(0, NT), op0=ALU.mult, op1=ALU.mult),
                         reads=[trb, B_C] + us[c].b, writes=y.b)
                transpose_blocks(src, c * 128, TB, ev)
                us[c].free()
                ys.append(y)
            for x_ in src:
                x_.free()
            return ys

        def pv_finish_diff(cmap, tbs, rows, h, o0, od):
            for (ai, tb) in tbs:
                pa, pab = acc(ai)
                r0, r1 = rows
                rr, rrb = sm_next()
                R.op('dve', lambda e, pa=pa, rr=rr: e.reciprocal(out=rr[r0:r1, 0:1], in_=pa[r0:r1, 128:129]),
                     reads=[pab], writes=[rrb])
                if cmap == 0:
                    R.op('dve', lambda e, pa=pa, rr=rr, ai=ai: e.tensor_scalar(
                        out=o0.ar.tf[r0:r1, o0.p0 * 256 + ai * 128:o0.p0 * 256 + ai * 128 + 128], in0=pa[r0:r1, 0:128],
                        scalar1=rr[r0:r1, 0:1], scalar2=None, op0=ALU.mult), reads=[pab, rrb], writes=o0.b)
                else:
                    R.op('dve', lambda e, rr=rr: e.tensor_scalar(out=rr[r0:r1, 1:2], in0=rr[r0:r1, 0:1],
                                                                 scalar1=DER[r0:r1, LAMN:LAMN + 1], scalar2=None, op0=ALU.mult),
                         reads=[rrb, B_C], writes=[rrb])
                    R.op('dve', lambda e, pa=pa, rr=rr, ai=ai, tb=tb: e.scalar_tensor_tensor(
                        out=od[tb].rows_bf(r0, r1, h * 128, h * 128 + 128), in0=pa[r0:r1, 0:128], scalar=rr[r0:r1, 1:2],
                        in1=o0.ar.tf[r0:r1, o0.p0 * 256 + ai * 128:o0.p0 * 256 + ai * 128 + 128], op0=ALU.mult, op1=ALU.add),
                        reads=[pab, rrb] + o0.b, writes=od[tb].bb(h * 128, h * 128 + 128))

        def diff_attn_prompt(j, QZ, od):
            NT = 512
            nkb = 4 * j + 4
            steps = [(h, cmap, kb) for h in range(8) for cmap in range(2) for kb in range(nkb)]
            o0s = {}

            def emit_S(h, cmap, kb):
                q0 = max(kb - 4 * j, 0)
                ncol = NT - q0 * 128
                ps, psb_ = mm_next()
                qz = QZ[h][cmap]
                R.op('pe', lambda e: e.matmul(
                    ps[:, 0:ncol], lhsT=KT[:, h * SEQ + kb * 128:h * SEQ + kb * 128 + 128],
                    rhs=qz.bf(q0 * 128, NT), start=True, stop=True),
                    reads=[B_KT[h][kb // 4]] + qz.b, writes=[psb_])
                E = A.alloc(1)
                R.op('act', lambda e: e.activation(out=E.bf(0, ncol), in_=ps[:, 0:ncol], func=AF.Exp),
                     reads=[psb_], writes=E.b)
                if kb >= 4 * j:
                    R.op('pool', lambda e: e.memset(E.rows_bf(64, 128, 0, 64), 0.0), writes=E.b)
                return E

            def emit_PV(h, cmap, kb, E):
                q0 = max(kb - 4 * j, 0)
                if cmap == 0 and kb == 0:
                    o0s[h] = A.alloc(2)
                for qb in range(q0, 4):
                    pa, pab = acc(qb)
                    R.op('pe', lambda e, pa=pa, qb=qb: e.matmul(
                        pa[:, 0:129], lhsT=E.bf((qb - q0) * 128, (qb - q0) * 128 + 128),
                        rhs=VA[:, kb * 1040 + h * 130:kb * 1040 + h * 130 + 129],
                        start=(kb == 0), stop=(kb == 4 * j + qb)),
                        reads=E.b + [B_VA[kb]], writes=[pab])
                    if kb == 4 * j + qb:
                        pv_finish_diff(cmap, [(qb, qb)], (0, 128), h, o0s[h], od)
                E.free()
                if cmap == 1 and kb == nkb - 1:
                    o0s.pop(h).free()

            pend = []
            for stp in steps:
                E = emit_S(*stp)
                pend.append(stp + (E,))
                if len(pend) > PIPE_DEPTH:
                    emit_PV(*pend.pop(0))
            while pend:
                emit_PV(*pend.pop(0))

        def mem_attn(NT, TB, QM, om, qsegs):
            c_lo = min(s[0] for s in qsegs)
            c_hi = max(s[0] + s[1] for s in qsegs)
            for h in range(4):
                Es = []
                for kb in range(2):
                    ps, psb_ = mm_next()
                    for dc in range(2):
                        ci = h * 2 + dc
                        R.op('pe', lambda e, ps=ps, ci=ci, kb=kb, dc=dc: e.matmul(
                            ps[:, c_lo:c_hi], lhsT=KM[:, ci * 256 + kb * 128:ci * 256 + kb * 128 + 128],
                            rhs=QM[ci].bf(c_lo, c_hi), start=(dc == 0), stop=(dc == 1)),
                            reads=[B_KM] + QM[ci].b, writes=[psb_])
                    E = A.alloc(1)
                    R.op('act', lambda e, ps=ps, E=E: e.activation(out=E.bf(c_lo, c_hi), in_=ps[:, c_lo:c_hi], func=AF.Exp),
                         reads=[psb_], writes=E.b)
                    Es.append(E)
                for (c0, ncl, ai, tb, r0) in qsegs:
                    pa, pab = acc(ai)
                    for kb in range(2):
                        R.op('pe', lambda e, pa=pa, kb=kb, c0=c0, ncl=ncl, r0=r0, h=h, Es=Es: e.matmul(
                            pa[r0:r0 + ncl, 0:257], lhsT=Es[kb].bf(c0, c0 + ncl),
                            rhs=VM[:, kb * 1032 + h * 258:kb * 1032 + h * 258 + 257], start=(kb == 0), stop=(kb == 1)),
                            reads=Es[kb].b + [B_VM[kb]], writes=[pab])
                    rr, rrb = sm_next()
                    R.op('dve', lambda e, pa=pa, rr=rr, r0=r0, ncl=ncl: e.reciprocal(out=rr[r0:r0 + ncl, 0:1],
                                                                                 in_=pa[r0:r0 + ncl, 256:257]),
                         reads=[pab], writes=[rrb])
                    R.op('dve', lambda e, pa=pa, rr=rr, r0=r0, ncl=ncl, tb=tb, h=h: e.tensor_scalar(
                        out=om[tb].rows_bf(r0, r0 + ncl, h * 256, h * 256 + 256), in0=pa[r0:r0 + ncl, 0:256],
                        scalar1=rr[r0:r0 + ncl, 0:1], scalar2=None, op0=ALU.mult),
                        reads=[pab, rrb], writes=om[tb].bb(h * 256, h * 256 + 256))
                for E in Es:
                    E.free()

        def main_tile(NT, TB, sample, b, j, xn_pre):
            if sample:
                src_rows = lambda tb: xsm[tb * 128:(tb + 1) * 128, :]
                yout, dkout, dvout, row0 = y_s, dk_s, dv_s, 0
            else:
                row0 = b * SEQ + j * 512
                src_rows = lambda tb: xp[row0 + tb * 128:row0 + (tb + 1) * 128, :]
                yout, dkout, dvout = y_p, dk_p, dv_p
            xnT = xstage_b(xn_pre, TB, P_GN)
            XNT['r'] = xnT
            XNT['b'] = [p.b for p in xnT]
            xb_ = XNT['b']
            if (not sample) and j == 0:
                R.op('pool', lambda e: e.memset(SF[:], 0.0), writes=B_SF)
                R.op('pool', lambda e: e.memset(SB[:], 0.0), writes=B_SB)
            hacc = [A.alloc(1) for _ in range(8)]
            on, ua = hgrn_stage(NT, TB, sample, b if (not sample and j == ntiles - 1) else None)
            tgs = merge_gates(0, NT)
            ya = gated_y(on, ua, TB, NT, col(GHG))
            merge_proj(0, ya, NT, hacc, tgs)
            for y in ya:
                y.free()

            qn = [A.alloc(2) for _ in range(TB)]

            def cons_qk(dst, half, fp32_out=None):
                def f(tb, ps, psb_):
                    r, rb = group_stats(ps[:, 0:512], psb_, 8, 64)
                    if fp32_out is None:
                        R.op('dve', lambda e: e.tensor_tensor(out=bview(dst[tb].bf(half * 512, half * 512 + 512), 8, 64),
                                                              in0=bview(ps[:, 0:512], 8, 64), in1=bcast_last(r[:, 0:8], 64),
                                                              op=ALU.mult), reads=[psb_, rb], writes=dst[tb].bb(half * 512, half * 512 + 512))
                    else:
                        ko = fp32_out[tb]
                        R.op('dve', lambda e: e.tensor_tensor(out=bview(ko.f(half * 512, half * 512 + 512), 8, 64),
                                                              in0=bview(ps[:, 0:512], 8, 64), in1=bcast_last(r[:, 0:8], 64),
                                                              op=ALU.mult), reads=[psb_, rb], writes=ko.bF(half * 512, half * 512 + 512))
                        R.op('dve', lambda e: e.tensor_tensor(out=bview(dst[tb].bf(half * 512, half * 512 + 512), 8, 64),
                                                              in0=bview(ps[:, 0:512], 8, 64), in1=bcast_last(r[:, 0:8], 64),
                                                              op=ALU.mult), reads=[psb_, rb], writes=dst[tb].bb(half * 512, half * 512 + 512))
                        R.op('pool', lambda e: e.tensor_tensor(out=bview(ko.f(half * 512, half * 512 + 512), 8, 64),
                                                               in0=bview(ko.f(half * 512, half * 512 + 512), 8, 64),
                                                               in1=bcast_mid(CBC[:, 256:320], 8), op=ALU.mult),
                             reads=ko.bF(half * 512, half * 512 + 512) + [B_C], writes=ko.bF(half * 512, half * 512 + 512))
                return f
            kn = [A.alloc(2) for _ in range(TB)]
            kof = [A.alloc(4) for _ in range(TB)]
            def st_k(kof=kof):
                for tb in range(TB):
                    R.op('sp', lambda e, tb=tb: e.dma_start(out=dkout[row0 + tb * 128:row0 + (tb + 1) * 128, :], in_=kof[tb].f(0, 1024)),
                         reads=kof[tb].b, dma_key="ko%d" % (tb % 2))
                    kof[tb].free()
            vof = [A.alloc(4) for _ in range(TB)]
            vnew = [A.alloc(2) for _ in range(TB)] if sample else None

            def cons_dv(half):
                def f(tb, ps, psb_):
                    R.op('act', lambda e: e.activation(out=vof[tb].f(half * 512, half * 512 + 512), in_=ps[:, 0:512], func=AF.Copy),
                         reads=[psb_], writes=vof[tb].bF(half * 512, half * 512 + 512))
                    if sample:
                        R.op('dve', lambda e: e.tensor_copy(out=vnew[tb].bf(half * 512, half * 512 + 512), in_=ps[:, 0:512]),
                             reads=[psb_], writes=vnew[tb].bb(half * 512, half * 512 + 512))
                    else:
                        blk = j * 4 + tb
                        R.op('dve', lambda e: e.tensor_copy(
                            out=bview(VA[:, blk * 1040 + half * 520:blk * 1040 + half * 520 + 520], 4, 130)[:, :, 0:128],
                            in_=bview(ps[:, 0:512], 4, 128)), reads=[psb_], writes=[B_VA[blk]])
                return f
            ub = [None] * 8

            def cons_zb(base):
                def f(ch, ps, psb_):
                    ub[base + ch] = gate_chunk(ps, psb_, NT)
                return f
            for g in range(2):
                tproj('in', 0, 4096 + g * 512, xnT, TB, cons_qk(qn, g))
                tproj('in', 0, 5120 + g * 512, xnT, TB, cons_qk(kn, g, kof))
                tproj('in', 0, 6144 + g * 512, xnT, TB, cons_dv(g))
                fproj('in', 0, 7168 + g * 512, xnT, xb_, NT, cons_zb(g * 4))
            defer(st_k)

            def st_v(vof=vof):
                for tb in range(TB):
                    R.op('sp', lambda e, tb=tb: e.dma_start(out=dvout[row0 + tb * 128:row0 + (tb + 1) * 128, :], in_=vof[tb].f(0, 1024)),
                         reads=vof[tb].b, dma_key="vo%d" % (tb % 2))
                    vof[tb].free()
            defer(st_v)
            QT = []
            for h in range(8):
                if sample:
                    q = A.alloc(1)

                    def ev(tr, trb, q=q):
                        R.op('dve', lambda e: e.tensor_scalar(out=q.bf(0, NT), in0=tr[:, 0:NT], scalar1=col(GQ8), scalar2=None,
                                                              op0=ALU.mult), reads=[trb, B_C], writes=q.b)
                    transpose_blocks(qn, h * 128, TB, ev)
                    QT.append(q)
                else:
                    qz = [A.alloc(1), A.alloc(1)]
                    R.op('pool', lambda e, qz=qz: e.memset(qz[0].rows_bf(64, 128, 0, NT), 0.0), writes=qz[0].b)
                    R.op('pool', lambda e, qz=qz: e.memset(qz[1].rows_bf(0, 64, 0, NT), 0.0), writes=qz[1].b)

                    def ev(tr, trb, qz=qz):
                        for m in range(2):
                            r0, r1 = m * 64, m * 64 + 64
                            R.op('dve', lambda e, m=m, r0=r0, r1=r1: e.tensor_scalar(
                                out=qz[m].rows_bf(r0, r1, 0, NT), in0=tr[r0:r1, 0:NT], scalar1=DER[r0:r1, GQ8:GQ8 + 1],
                                scalar2=None, op0=ALU.mult), reads=[trb, B_C], writes=qz[m].b)
                    transpose_blocks(qn, h * 128, TB, ev)
                    QT.append(qz)
            for x_ in qn:
                x_.free()
            ktok0 = 1024 if sample else j * 512
            kbuf = (lambda h: B_KT[h][2]) if sample else (lambda h: B_KT[h][j])
            if not sample:
                for h in range(8):
                    def ev(tr, trb, h=h):
                        R.op('act', lambda e: e.activation(out=KT[:, h * SEQ + ktok0:h * SEQ + ktok0 + NT], in_=tr[:, 0:NT], func=AF.Copy,
                                                           scale=col(GK2)),
                             reads=[trb, B_C], writes=[kbuf(h)])
                    transpose_blocks(kn, h * 128, TB, ev)
                for x_ in kn:
                    x_.free()
            od = [A.alloc(2) for _ in range(TB)]
            set_wide(False)
            if not sample:
                diff_attn_prompt(j, QT, od)
            else:
                sample_diff_attn(QT, kn, vnew, od)
                for x_ in kn + vnew:
                    x_.free()
            set_wide(True)
            for q in QT:
                if sample:
                    q.free()
                else:
                    q[0].free()
                    q[1].free()
            odn = norm_rows_bf(od, TB, 8, 128)
            for x_ in od:
                x_.free()
            tgs = merge_gates(1, NT)
            yb = gated_y(odn, ub, TB, NT, col(GDS))
            merge_proj(1, yb, NT, hacc, tgs)
            for y in yb:
                y.free()

            hoist()
            qmn = [A.alloc(2) for _ in range(TB)]

            def cons_mq(half):
                def f(tb, ps, psb_):
                    r, rb = group_stats(ps[:, 0:512], psb_, 2, 256)
                    R.op('dve', lambda e: e.tensor_tensor(out=bview(qmn[tb].bf(half * 512, half * 512 + 512), 2, 256),
                                                          in0=bview(ps[:, 0:512], 2, 256), in1=bcast_last(r[:, 0:2], 256),
                                                          op=ALU.mult), reads=[psb_, rb], writes=qmn[tb].bb(half * 512, half * 512 + 512))
                return f
            for g in range(2):
                tproj('in', 0, 8192 + g * 512, xnT, TB, cons_mq(g))
            um = [None] * 8

            def cons_zm(base):
                def f(ch, ps, psb_):
                    um[base + ch] = gate_chunk(ps, psb_, NT)
                return f
            for g in range(2):
                fproj('in', 0, 9216 + g * 512, xnT, xb_, NT, cons_zm(g * 4))
            QM = []
            for ci in range(8):
                q = A.alloc(1)

                def ev(tr, trb, q=q, ci=ci):
                    R.op('dve', lambda e: e.tensor_scalar(out=q.bf(0, NT), in0=tr[:, 0:NT], scalar1=col(GMQ + ci % 2),
                                                          scalar2=None, op0=ALU.mult), reads=[trb, B_C], writes=q.b)
                transpose_blocks(qmn, ci * 128, TB, ev)
                QM.append(q)
            for x_ in qmn:
                x_.free()
            om = [A.alloc(2) for _ in range(TB)]
            set_wide(False)
            if not sample:
                mem_attn(NT, TB, QM, om, [(qb * 128, 128, qb, qb, 0) for qb in range(4)])
            else:
                for i in range(4):
                    sample_load_mem(i)
                    mem_attn(NT, TB, QM, om, [(i * 64, 64, i % 4, i // 2, (i % 2) * 64)])
            for q in QM:
                q.free()
            set_wide(True)
            tgs = merge_gates(2, NT)
            ym = gated_y(om, um, TB, NT, 0.5)
            merge_proj(2, ym, NT, hacc, tgs)
            for y in ym:
                y.free()
            for p in xnT:
                p.free()

            xres = []
            for tb in range(TB):
                xr = A.alloc(4)
                R.op('sp', lambda e, xr=xr, tb=tb: e.dma_start(out=xr.f(0, 1024), in_=src_rows(tb)), writes=xr.b,
                     dma_key="xr%d" % (tb % 2))
                xres.append(xr)

            def cons_out(half):
                def f(tb, ps, psb_):
                    xr = xres[tb]
                    R.op('dve', lambda e: e.scalar_tensor_tensor(out=xr.f(half * 512, half * 512 + 512), in0=ps[:, 0:512],
                                                                 scalar=0.5, in1=xr.f(half * 512, half * 512 + 512),
                                                                 op0=ALU.mult, op1=ALU.add),
                         reads=[psb_] + xr.bF(half * 512, half * 512 + 512), writes=xr.bF(half * 512, half * 512 + 512))
                return f
            for g in range(2):
                tproj('out', 0, g * 512, hacc, TB, cons_out(g))
            def st_y(xres=xres):
                for tb in range(TB):
                    R.op('sp', lambda e, tb=tb: e.dma_start(out=yout[row0 + tb * 128:row0 + (tb + 1) * 128, :], in_=xres[tb].f(0, 1024)),
                         reads=xres[tb].b, dma_key="yo%d" % (tb % 2))
                    xres[tb].free()
            defer(st_y)
            for p in hacc:
                p.free()

        def sample_diff_attn(QT, kn, vnew, od):
            NT = 256
            knT = [[None] * 8 for _ in range(2)]
            for tbp in range(2):
                for h in range(8):
                    p = A.alloc(1)

                    def ev(tr, trb, p=p):
                        R.op('act', lambda e: e.activation(out=p.bf(0, 128), in_=tr[:, 0:128], func=AF.Copy, scale=col(GK2)),
                             reads=[trb, B_C], writes=p.b)
                    transpose_blocks([kn[tbp]], h * 128, 1, ev)
                    knT[tbp][h] = p
            for i in range(4):
                tb, r0 = i // 2, (i % 2) * 64
                for blk in range(8):
                    kc_ = A.alloc(2)
                    R.op('pool', lambda e, kc_=kc_, blk=blk, i=i: e.dma_start(
                        out=kc_.bf(0, 1024), in_=cdk[i * 1024 + blk * 128:i * 1024 + (blk + 1) * 128, :]),
                        writes=kc_.b, dma_key="ck%d" % (blk % 2))
                    for half in range(2):
                        tr, trb = tr_next()
                        for hh in range(4):
                            h = half * 4 + hh
                            R.op('pe', lambda e, tr=tr, hh=hh, h=h, kc_=kc_: e.transpose(
                                tr[:, hh * 128:(hh + 1) * 128], kc_.bf(h * 128, h * 128 + 128), IDB[:]),
                                reads=kc_.b + [B_C], writes=[trb])
                        for hh in range(4):
                            h = half * 4 + hh
                            R.op('act', lambda e, tr=tr, hh=hh, h=h, blk=blk: e.activation(
                                out=KT[:, h * SEQ + blk * 128:h * SEQ + blk * 128 + 128], in_=tr[:, hh * 128:(hh + 1) * 128],
                                func=AF.Copy), reads=[trb], writes=[B_KT[h][blk // 4]])
                    kc_.free()
                    R.op('pool', lambda e, blk=blk, i=i: e.dma_start(
                        out=bview(VA[:, blk * 1040:(blk + 1) * 1040], 8, 130)[:, :, 0:128],
                        in_=cdv[i * 1024 + blk * 128:i * 1024 + (blk + 1) * 128, :].rearrange("p (h v) -> p h v", h=8)),
                        writes=[B_VA[blk]], dma_key="cv%d" % (blk % 2))
                for h in range(8):
                    R.op('act', lambda e, h=h, tb=tb: e.activation(out=KT[:, h * SEQ + 1024:h * SEQ + 1152],
                                                                   in_=knT[tb][h].bf(0, 128), func=AF.Copy),
                         reads=knT[tb][h].b, writes=[B_KT[h][2]])
                R.op('dve', lambda e, tb=tb: e.tensor_copy(out=bview(VA[:, 8 * 1040:9 * 1040], 8, 130)[:, :, 0:128],
                                                         in_=bview(vnew[tb].bf(0, 1024), 8, 128)),
                     reads=vnew[tb].b, writes=[B_VA[8]])
                o0s = {}

                def emit_S(h, cmap, kb, i=i, r0=r0):
                    p0, p1 = cmap * 64, cmap * 64 + 64
                    k0, k1 = (0, 128) if kb < 8 else (r0, r0 + 64)
                    ps, psb_ = mm_next()
                    R.op('pe', lambda e: e.matmul(
                        ps[k0:k1, 0:64], lhsT=KT[p0:p1, h * SEQ + kb * 128 + k0:h * SEQ + kb * 128 + k1],
                        rhs=QT[h].rows_bf(p0, p1, i * 64, i * 64 + 64), start=True, stop=True),
                        reads=[B_KT[h][kb // 4]] + QT[h].b, writes=[psb_])
                    E = A.alloc(1)
                    R.op('act', lambda e: e.activation(
                        out=E.rows_bf(k0, k1, 0, 64), in_=ps[k0:k1, 0:64], func=AF.Exp), reads=[psb_], writes=E.b)
                    return E

                def emit_PV(h, cmap, kb, E, i=i, r0=r0, tb=tb):
                    k0, k1 = (0, 128) if kb < 8 else (r0, r0 + 64)
                    if cmap == 0 and kb == 0:
                        o0s[h] = A.alloc(2)
                    pa, pab = acc(i)
                    R.op('pe', lambda e: e.matmul(
                        pa[r0:r0 + 64, 0:129], lhsT=E.rows_bf(k0, k1, 0, 64),
                        rhs=VA[k0:k1, kb * 1040 + h * 130:kb * 1040 + h * 130 + 129], start=(kb == 0), stop=(kb == 8)),
                        reads=E.b + [B_VA[kb]], writes=[pab])
                    E.free()
                    if kb == 8:
                        pv_finish_diff(cmap, [(i, tb)], (r0, r0 + 64), h, o0s[h], od)
                        if cmap == 1:
                            o0s.pop(h).free()

                pend = []
                for stp in [(h, cmap, kb) for h in range(8) for cmap in range(2) for kb in range(9)]:
                    E = emit_S(*stp)
                    pend.append(stp + (E,))
                    if len(pend) > PIPE_DEPTH:
                        emit_PV(*pend.pop(0))
                while pend:
                    emit_PV(*pend.pop(0))
            for tbp in range(2):
                for h in range(8):
                    knT[tbp][h].free()

        def sample_load_mem(i):
            for kb in range(2):
                kc_ = A.alloc(2)
                R.op('pool', lambda e, kc_=kc_, kb=kb: e.dma_start(out=kc_.bf(0, 1024),
                                                                in_=cmk[i * 256 + kb * 128:i * 256 + (kb + 1) * 128, :]),
                     writes=kc_.b, dma_key="cm%d" % kb)
                for half in range(2):
                    tr, trb = tr_next()
                    for cc in range(4):
                        ci = half * 4 + cc
                        R.op('pe', lambda e, tr=tr, cc=cc, ci=ci, kc_=kc_: e.transpose(
                            tr[:, cc * 128:(cc + 1) * 128], kc_.bf(ci * 128, ci * 128 + 128), IDB[:]),
                            reads=kc_.b + [B_C], writes=[trb])
                    for cc in range(4):
                        ci = half * 4 + cc
                        R.op('act', lambda e, tr=tr, cc=cc, ci=ci, kb=kb: e.activation(
                            out=KM[:, ci * 256 + kb * 128:ci * 256 + kb * 128 + 128], in_=tr[:, cc * 128:(cc + 1) * 128],
                            func=AF.Copy), reads=[trb], writes=[B_KM])
                kc_.free()
                R.op('pool', lambda e, kb=kb: e.dma_start(
                    out=bview(VM[:, kb * 1032:(kb + 1) * 1032], 4, 258)[:, :, 0:256],
                    in_=cmv[i * 256 + kb * 128:i * 256 + (kb + 1) * 128, :].rearrange("p (h v) -> p h v", h=4)),
                    writes=[B_VM[kb]], dma_key="cw%d" % kb)

        def mem_kv(b, xn_pre):
            TB = 2
            r0 = b * 256
            xnT = xstage_b(xn_pre, TB, P_GM)
            kn = [A.alloc(2) for _ in range(TB)]
            kof = [A.alloc(4) for _ in range(TB)]

            def cons_k(half):
                def f(tb, ps, psb_):
                    r, rb = group_stats(ps[:, 0:512], psb_, 2, 256)
                    ko = kof[tb]
                    c0, c1 = half * 512, half * 512 + 512
                    R.op('dve', lambda e: e.tensor_tensor(out=bview(ko.f(c0, c1), 2, 256), in0=bview(ps[:, 0:512], 2, 256),
                                                          in1=bcast_last(r[:, 0:2], 256), op=ALU.mult),
                         reads=[psb_, rb], writes=ko.bF(c0, c1))
                    R.op('pool', lambda e: e.tensor_tensor(out=bview(ko.f(c0, c1), 2, 256), in0=bview(ko.f(c0, c1), 2, 256),
                                                           in1=bcast_mid(CBC[:, 320:576], 2), op=ALU.mult),
                         reads=ko.bF(c0, c1) + [B_C], writes=ko.bF(c0, c1))
                    R.op('act', lambda e: e.activation(out=kn[tb].bf(c0, c1), in_=ko.f(c0, c1), func=AF.Copy),
                         reads=ko.bF(c0, c1), writes=kn[tb].bb(c0, c1))
                return f
            for g in range(2):
                tproj('mkv', 0, g * 512, xnT, TB, cons_k(g))
            def st_k(kof=kof):
                for tb in range(TB):
                    R.op('sp', lambda e, tb=tb: e.dma_start(out=mk_p[r0 + tb * 128:r0 + (tb + 1) * 128, :], in_=kof[tb].f(0, 1024)),
                         reads=kof[tb].b, dma_key="ko%d" % (tb % 2))
                    kof[tb].free()
            defer(st_k)
            for ci in range(8):
                def ev(tr, trb, ci=ci):
                    R.op('act', lambda e: e.activation(out=KM[:, ci * 256:ci * 256 + 256], in_=tr[:, 0:256], func=AF.Copy),
                         reads=[trb], writes=[B_KM])
                transpose_blocks(kn, ci * 128, TB, ev)
            for x_ in kn:
                x_.free()
            hoist()
            vof = [A.alloc(4) for _ in range(TB)]

            def cons_v(half):
                def f(tb, ps, psb_):
                    c0, c1 = half * 512, half * 512 + 512
                    R.op('act', lambda e: e.activation(out=vof[tb].f(c0, c1), in_=ps[:, 0:512], func=AF.Copy),
                         reads=[psb_], writes=vof[tb].bF(c0, c1))
                    if DBG_V == 1:
                        for g2 in range(2):
                            R.op('dve', lambda e, g2=g2: e.tensor_copy(
                                out=VM[:, tb * 1032 + half * 516 + g2 * 258:tb * 1032 + half * 516 + g2 * 258 + 256],
                                in_=ps[:, g2 * 256:(g2 + 1) * 256]), reads=[psb_], writes=[B_VM[tb]])
                    elif DBG_V == 2:
                        R.op('act', lambda e: e.activation(
                            out=bview(VM[:, tb * 1032 + half * 516:tb * 1032 + half * 516 + 516], 2, 258)[:, :, 0:256],
                            in_=bview(ps[:, 0:512], 2, 256), func=AF.Copy), reads=[psb_], writes=[B_VM[tb]])
                    else:
                        R.op('dve', lambda e: e.tensor_copy(
                            out=bview(VM[:, tb * 1032 + half * 516:tb * 1032 + half * 516 + 516], 2, 258)[:, :, 0:256],
                            in_=bview(ps[:, 0:512], 2, 256)), reads=[psb_], writes=[B_VM[tb]])
                return f
            for g in range(2):
                tproj('mkv', 0, 1024 + g * 512, xnT, TB, cons_v(g))
            def st_v(vof=vof):
                for tb in range(TB):
                    R.op('sp', lambda e, tb=tb: e.dma_start(out=mv_p[r0 + tb * 128:r0 + (tb + 1) * 128, :], in_=vof[tb].f(0, 1024)),
                         reads=vof[tb].b, dma_key="vo%d" % (tb % 2))
                    vof[tb].free()
            defer(st_v)
            for p in xnT:
                p.free()

        units = []
        for b in range(nb_run):
            units.append(('mem', b, None))
            for j in range(ntiles):
                units.append(('main', b, j))
        if do_sample:
            units.append(('sample', None, None))
        prepared = {}
        cur = {'k': 0}

        def prep(k):
            if k >= len(units) or k in prepared:
                return
            kind, b, j = units[k]
            if kind == 'mem':
                prepared[k] = xstage_a(lambda tb, b=b: mp[b * 256 + tb * 128:b * 256 + (tb + 1) * 128, :], 2)
            elif kind == 'main':
                r0_ = b * SEQ + j * 512
                prepared[k] = xstage_a(lambda tb, r0_=r0_: xp[r0_ + tb * 128:r0_ + (tb + 1) * 128, :], 4)
            else:
                prepared[k] = xstage_a(lambda tb: xsm[tb * 128:(tb + 1) * 128, :], 2)

        def hoist():
            prep(cur['k'] + 1)

        for k, (kind, b, j) in enumerate(units):
            cur['k'] = k
            prep(k)
            xn_pre = prepared.pop(k)
            if kind == 'mem':
                mem_kv(b, xn_pre)
            elif kind == 'main':
                main_tile(512, 4, False, b, j, xn_pre)
            else:
                main_tile(256, 2, True, None, None, xn_pre)
        flush_pending(True)
        assert ws_state['cur'] == len(plan)
        R.emit(nc, st)
    return nc


_CACHE = {}


def pack_inputs(x_prompt, x_sample, mem_prompt, cache_diff_k, cache_diff_v, cache_mem_k, cache_mem_v,
                state_hgrn, g_norm, w_in, hg_lb_logits, g_hg_out, g_dq, g_dk, lam_q1, lam_k1,
                lam_q2, lam_k2, g_dsub, g_mem, w_mkv, g_mq, g_mk, w_branch, w_out, n_cores=8):
    f32 = np.float32
    A_ = lambda a: np.ascontiguousarray(np.asarray(a, dtype=f32))
    pk = lambda v, n: A_(v).reshape(n, 128).T
    cpk = np.zeros((128, 40), f32)
    cpk[:, 0:8] = pk(g_norm[0], 8)
    cpk[:, 8:16] = pk(g_mem[0], 8)
    cpk[:, 16:24] = pk(hg_lb_logits[0], 8)
    cpk[:, 24:32] = pk(hg_lb_logits[1], 8)
    cpk[:, 32] = A_(g_hg_out[0])
    cpk[:, 33] = np.tile(A_(g_dq[0]), 2)
    cpk[:, 34] = np.tile(A_(g_dk[0]), 2)
    cpk[:, 35] = A_(g_dsub[0])
    cpk[:, 36:38] = pk(g_mq[0], 2)
    cpk[:, 38:40] = pk(g_mk[0], 2)
    cbc = np.zeros((128, 576), f32)
    cbc[:, 0:64] = A_(lam_q1[0])[None, :]
    cbc[:, 64:128] = A_(lam_k1[0])[None, :]
    cbc[:, 128:192] = A_(lam_q2[0])[None, :]
    cbc[:, 192:256] = A_(lam_k2[0])[None, :]
    cbc[:, 256:320] = A_(g_dk[0])[None, :]
    cbc[:, 320:576] = A_(g_mk[0])[None, :]
    cfix = np.zeros((128, 320), f32)
    cfix[:, 0:128] = np.eye(128, dtype=f32)
    s_ = np.arange(128)[:, None] % 64
    t_ = np.arange(64)[None, :]
    cfix[:, 128:192] = (s_ <= t_).astype(f32)
    cfix[:, 192:256] = 1.0
    cfix[:, 192] = 0.0
    cfix[:, 256] = 1.0
    w_in_ = A_(w_in[0])
    w_mkv_ = A_(w_mkv[0])
    w_br_ = A_(w_branch[0]).reshape(3072, 1024)
    w_out_ = A_(w_out[0])
    in_maps = []
    for c in range(n_cores):
        sl = slice(c * NB, (c + 1) * NB)
        in_maps.append({
            "xp": A_(x_prompt[sl]).reshape(NB * SEQ, 1024),
            "xsm": A_(x_sample[sl]).reshape(256, 1024),
            "mp": A_(mem_prompt[sl]).reshape(NB * 256, 1024),
            "cdk": A_(cache_diff_k[0, sl]).reshape(NB * 1024, 1024),
            "cdv": A_(cache_diff_v[0, sl]).reshape(NB * 1024, 1024),
            "cmk": A_(cache_mem_k[0, sl]).reshape(NB * 256, 1024),
            "cmv": A_(cache_mem_v[0, sl]).reshape(NB * 256, 1024),
            "st0": A_(state_hgrn[0, sl]),
            "w_in": w_in_, "w_mkv": w_mkv_, "w_br": w_br_, "w_out": w_out_,
            "cpk": cpk, "cbc": cbc, "cfix": cfix,
        })
    return in_maps


def kernel(**inputs):
    f32 = np.float32
    if 'nc' not in _CACHE:
        _CACHE['nc'] = build_program()
    nc = _CACHE['nc']
    in_maps = pack_inputs(**inputs)
    res = run_bass_kernel_spmd(nc, in_maps, core_ids=list(range(8))).results
    cat = lambda k: np.concatenate([np.asarray(r[k], dtype=f32) for r in res], axis=0)
    y_p = cat("y_p").reshape(32, SEQ, 1024)
    y_s = cat("y_s").reshape(32, 64, 1024)
    st_p = cat("st_p").reshape(1, 32, 8, 128, 128)
    st_s = cat("st_s").reshape(1, 32, 8, 128, 128)
    dk_p = cat("dk_p").reshape(1, 32, SEQ, 8, 2, 64)
    dv_p = cat("dv_p").reshape(1, 32, SEQ, 8, 128)
    dk_s = cat("dk_s").reshape(1, 32, 64, 8, 2, 64)
    dv_s = cat("dv_s").reshape(1, 32, 64, 8, 128)
    mk_p = cat("mk_p").reshape(1, 32, 256, 4, 256)
    mv_p = cat("mv_p").reshape(1, 32, 256, 4, 256)
    return (y_p, y_s, st_p, st_s, dk_p, dv_p, dk_s, dv_s, mk_p, mv_p)
```

```python
import contextlib
import numpy as np
import concourse.bass as bass
import concourse.mybir as mybir
from concourse.bass_utils import run_bass_kernel_spmd
from concourse.alu_op_type import AluOpType as ALU

F32 = mybir.dt.float32
BF16 = mybir.dt.bfloat16
AF = mybir.ActivationFunctionType
AX = mybir.AxisListType
ENGS = ('pe', 'act', 'dve', 'pool', 'sp')
EPS = 1e-6
NB = 4
SEQ = 2048
NPIECE = 92
DBG_BR = {0, 1, 2}
PIPE_DEPTH = 4
STORE_DELAY = 2
CONV_AHEAD = 3
DBG_CLOSURE = False
DBG_MAXOPS = None
DBG_V = 0
DBG_SKIP = set()
CLOSURE_SNAPS = []


class Buf:
    __slots__ = ('name', 'w', 'r', 'psum')

    def __init__(self, name, psum=False):
        self.name = name
        self.w = None
        self.r = {}
        self.psum = psum


class Op:
    __slots__ = ('eng', 'fn', 'deps', 'inc', 'val', 'sem', 'dma', 'idx', 'clock', 'waits')


def _flat(x):
    out = []
    for b in x:
        if b is None:
            continue
        if isinstance(b, Buf):
            out.append(b)
        else:
            out.extend(_flat(b))
    return out


class Rec:
    def __init__(self):
        self.q = {e: [] for e in ENGS}
        self.dma_keys = {}

    def op(self, eng, fn, reads=(), writes=(), dma_key=None):
        self.nrec = getattr(self, 'nrec', 0) + 1
        if DBG_MAXOPS is not None and self.nrec > DBG_MAXOPS:
            return None
        if self.nrec in DBG_SKIP:
            return None
        if DBG_MAXOPS is not None and self.nrec == DBG_MAXOPS:
            print("LAST OP", eng, "line", fn.__code__.co_firstlineno)
        o = Op()
        o.eng = eng
        o.fn = fn
        o.inc = False
        o.val = None
        o.sem = None
        o.dma = dma_key
        o.idx = self.nrec
        if DBG_CLOSURE and fn.__closure__:
            o_snap = [id(c.cell_contents) for c in fn.__closure__]
            CLOSURE_SNAPS.append((fn, o_snap))
        deps = {}
        rl = _flat(reads)
        wl = _flat(writes)
        for b in rl:
            if b.w is not None:
                deps[id(b.w)] = b.w
            if b.psum:
                for k_, d in b.r.items():
                    if k_ != eng:
                        deps[id(d)] = d
        for b in wl:
            if b.w is not None:
                deps[id(b.w)] = b.w
            for d in b.r.values():
                deps[id(d)] = d
        if dma_key is not None:
            ent = self.dma_keys.setdefault(dma_key, [0, None])
            if ent[1] is not None:
                deps[id(ent[1])] = ent[1]
            ent[0] += 1
            ent[1] = o
            o.val = 16 * ent[0]
        dl = []
        for d in deps.values():
            if d is o:
                continue
            if d.dma is None and d.eng == 'pe' and eng == 'pe' and dma_key is None:
                continue
            d.inc = True
            dl.append(d)
        o.deps = dl
        rkey = eng if dma_key is None else ('dma', dma_key)
        for b in rl:
            b.r[rkey] = o
        for b in wl:
            b.w = o
            b.r = {}
        self.q[eng].append(o)
        return o

    def emit(self, nc, stack):
        esem = {e: stack.enter_context(nc.semaphore("sem_" + e)) for e in ENGS}
        dsem = {k: stack.enter_context(nc.semaphore("dsem_%d" % i)) for i, k in enumerate(self.dma_keys)}
        for e in ENGS:
            c = 0
            for o in self.q[e]:
                if o.dma is not None:
                    o.sem = dsem[o.dma]
                else:
                    o.sem = esem[e]
                    if o.inc:
                        c += 1
                        o.val = c
        know = {e: {} for e in ENGS}
        for o in sorted((o for e in ENGS for o in self.q[e]), key=lambda o: o.idx):
            kn = know[o.eng]
            o.waits = []
            for d in o.deps:
                if kn.get(id(d.sem), 0) < d.val:
                    o.waits.append((d.sem, d.val))
                    for kk, vv in d.clock.items():
                        if kn.get(kk, 0) < vv:
                            kn[kk] = vv
            if o.dma is not None or o.inc:
                o.clock = dict(kn)
                o.clock[id(o.sem)] = o.val
        engobj = {'pe': 'tensor', 'act': 'scalar', 'dve': 'vector', 'pool': 'gpsimd', 'sp': 'sync'}
        fin = [(dsem[k], 16 * v[0]) for k, v in self.dma_keys.items()]
        block = stack.enter_context(nc.Block())
        for e in ENGS:
            q = self.q[e]

            def body(eng, q=q, e=e):
                for o in q:
                    for ws_, wv_ in o.waits:
                        eng.wait_ge(ws_, wv_)
                    inst = o.fn(eng)
                    if o.dma is not None:
                        inst.then_inc(o.sem, 16)
                    elif o.inc:
                        inst.then_inc(o.sem, 1)
                if e == 'sp':
                    for s, v in fin:
                        eng.wait_ge(s, v)
            getattr(block, engobj[e])(body)


class Reg:
    def __init__(self, ar, p0, n):
        self.ar = ar
        self.p0 = p0
        self.n = n

    def bf(self, c0=0, c1=None):
        c1 = self.n * 512 if c1 is None else c1
        return self.ar.t[:, self.p0 * 512 + c0:self.p0 * 512 + c1]

    def f(self, c0=0, c1=None):
        c1 = self.n * 256 if c1 is None else c1
        return self.ar.tf[:, self.p0 * 256 + c0:self.p0 * 256 + c1]

    def rows_bf(self, r0, r1, c0, c1):
        return self.ar.t[r0:r1, self.p0 * 512 + c0:self.p0 * 512 + c1]

    @property
    def b(self):
        return self.ar.bufs[self.p0:self.p0 + self.n]

    def bb(self, c0, c1):
        return self.ar.bufs[self.p0 + c0 // 512:self.p0 + (c1 - 1) // 512 + 1]

    def bF(self, c0, c1):
        return self.ar.bufs[self.p0 + c0 // 256:self.p0 + (c1 - 1) // 256 + 1]

    def free(self):
        self.ar.release(self)


class Arena:
    def __init__(self, t, n):
        self.t = t
        self.tf = t.bitcast(F32)
        self.n = n
        self.used = [False] * n
        self.bufs = [Buf("a%d" % i) for i in range(n)]
        self.ptr = 0
        self.peak = 0

    def alloc(self, k):
        n = self.n
        for s in range(n):
            p = (self.ptr + s) % n
            if p + k > n:
                continue
            if not any(self.used[p:p + k]):
                for i in range(p, p + k):
                    self.used[i] = True
                self.ptr = (p + k) % n
                self.peak = max(self.peak, sum(self.used))
                return Reg(self, p, k)
        raise RuntimeError("arena full: want %d, used %d" % (k, sum(self.used)))

    def release(self, r):
        for i in range(r.p0, r.p0 + r.n):
            assert self.used[i]
            self.used[i] = False


def bview(ap, g, j):
    return ap.rearrange("p (g j) -> p g j", g=g, j=j)


def bcast_mid(ap, g):
    return ap.unsqueeze(1).to_broadcast([ap.shape[0], g, ap.shape[1]])


def bcast_last(ap, j):
    return ap.unsqueeze(2).to_broadcast([ap.shape[0], ap.shape[1], j])


def build_program(nb_run=NB, ntiles=4, do_sample=True):
    nc = bass.Bass("TRN2", target_bir_lowering=False)
    R = Rec()

    def din(name, shape):
        return nc.dram_tensor(name, shape, F32, kind="ExternalInput").ap()

    def dout(name, shape):
        return nc.dram_tensor(name, shape, F32, kind="ExternalOutput").ap()

    xp = din("xp", [NB * SEQ, 1024])
    xsm = din("xsm", [256, 1024])
    mp = din("mp", [NB * 256, 1024])
    cdk = din("cdk", [NB * 1024, 1024])
    cdv = din("cdv", [NB * 1024, 1024])
    cmk = din("cmk", [NB * 256, 1024])
    cmv = din("cmv", [NB * 256, 1024])
    st0 = din("st0", [NB, 8, 128, 128])
    w_in = din("w_in", [1024, 13312])
    w_mkv = din("w_mkv", [1024, 2048])
    w_br = din("w_br", [3072, 1024])
    w_out = din("w_out", [1024, 1024])
    cpk = din("cpk", [128, 40])
    cbc = din("cbc", [128, 576])
    cfix = din("cfix", [128, 320])

    y_p = dout("y_p", [NB * SEQ, 1024])
    y_s = dout("y_s", [256, 1024])
    st_p = dout("st_p", [NB, 8, 128, 128])
    st_s = dout("st_s", [NB, 8, 128, 128])
    dk_p = dout("dk_p", [NB * SEQ, 1024])
    dv_p = dout("dv_p", [NB * SEQ, 1024])
    dk_s = dout("dk_s", [256, 1024])
    dv_s = dout("dv_s", [256, 1024])
    mk_p = dout("mk_p", [NB * 256, 1024])
    mv_p = dout("mv_p", [NB * 256, 1024])

    wb_in = nc.dram_tensor("wb_in", [1024, 13312], BF16, kind="Internal").ap()
    wb_mkv = nc.dram_tensor("wb_mkv", [1024, 2048], BF16, kind="Internal").ap()
    wb_br = nc.dram_tensor("wb_br", [3072, 1024], BF16, kind="Internal").ap()
    wb_out = nc.dram_tensor("wb_out", [1024, 1024], BF16, kind="Internal").ap()
    B_wb = {'in': Buf("wb_in"), 'mkv': Buf("wb_mkv"), 'br': Buf("wb_br"), 'out': Buf("wb_out")}

    with contextlib.ExitStack() as st:
        def sb(name, shape, dt):
            return st.enter_context(nc.sbuf_tensor(name, shape, dt))

        KT = sb("KT", [128, 8 * SEQ], BF16)
        B_KT = [[Buf("KT%d_%d" % (h, j)) for j in range(4)] for h in range(8)]
        VA = sb("VA", [128, 16 * 1040], BF16)
        B_VA = [Buf("VA%d" % i) for i in range(16)]
        KM = sb("KM", [128, 8 * 256], BF16)
        B_KM = Buf("KM")
        VM = sb("VM", [128, 2 * 1032], BF16)
        B_VM = [Buf("VM0"), Buf("VM1")]
        WS = [sb("ws%d" % i, [128, 8 * 512], BF16) for i in range(3)]
        B_WS = [Buf("ws%d" % i) for i in range(3)]
        SF = sb("SF", [128, 1024], F32)
        SB = sb("SB", [128, 1024], BF16)
        B_SF = [Buf("SF%d" % h) for h in range(8)]
        B_SB = [Buf("SB%d" % h) for h in range(8)]
        CPK = sb("CPK", [128, 40], F32)
        CBC = sb("CBC", [128, 576], F32)
        CFX = sb("CFX", [128, 192], F32)
        IDB = sb("IDB", [128, 128], BF16)
        MHG = sb("MHG", [128, 64], BF16)
        DER = sb("DER", [128, 64], F32)
        MH = sb("MH", [128, 16], F32)
        AEND = sb("AEND", [128, 64], F32)
        B_AEND = [Buf("AEND%d" % h) for h in range(8)]
        FM1 = [sb("FM1_%d" % i, [128, 512], F32) for i in range(2)]
        B_FM1 = [Buf("FM1_0"), Buf("FM1_1")]
        SM = sb("SM", [128, 512], F32)
        B_SM = [Buf("SM%d" % i) for i in range(32)]
        B_C = Buf("consts")
        art = sb("arena", [128, NPIECE * 512], BF16)
        A = Arena(art, NPIECE)
        PS = [st.enter_context(nc.psum_tensor("ps%d" % i, [128, 512], F32)) for i in range(8)]
        PSB = [p.bitcast(BF16) for p in PS]
        B_PS = [Buf("ps%d" % i, True) for i in range(8)]
        B_TR = [Buf("tr0", True), Buf("tr1", True)]
        state = {'bk': 0, 'wide': True, 'sm': 0, 'fm1': 0}

        def bank_next():
            bset = (0, 1, 2, 3, 4, 5, 6, 7) if state['wide'] else (0, 1, 2, 3)
            i = bset[state['bk'] % len(bset)]
            state['bk'] += 1
            return i

        def mm_next():
            i = bank_next()
            return PS[i], B_PS[i]

        def tr_next():
            i = bank_next()
            return PSB[i][:, 0:512], B_PS[i]

        def set_wide(w):
            state['wide'] = w

        def acc(i):
            return PS[4 + i], B_PS[4 + i]

        def sm_next():
            i = state['sm']
            state['sm'] = (i + 1) % 32
            return SM[:, i * 16:(i + 1) * 16], B_SM[i]

        C1, C0, NC1 = 0, 8, 16
        GHG, GQ8, GK2, GDS, LAMN = 24, 25, 26, 27, 28
        GMQ = 29
        GMKP = 31
        P_GN, P_GM, P_L0, P_L1, P_GHG, P_GQ, P_GK, P_GDS, P_GMQ, P_GMK = 0, 8, 16, 24, 32, 33, 34, 35, 36, 38

        R.op('sp', lambda e: e.dma_start(out=CPK[:], in_=cpk[:, :]), writes=[B_C], dma_key="c0")
        R.op('sp', lambda e: e.dma_start(out=CBC[:], in_=cbc[:, :]), writes=[B_C], dma_key="c1")
        R.op('sp', lambda e: e.dma_start(out=CFX[:, 0:128], in_=cfix[:, 192:320]), writes=[B_C], dma_key="c2")
        R.op('pool', lambda e: e.dma_start(out=IDB[:], in_=cfix[:, 0:128]), writes=[B_C], dma_key="c3")
        R.op('pool', lambda e: e.dma_start(out=MHG[:], in_=cfix[:, 128:192]), writes=[B_C], dma_key="c4")
        R.op('pool', lambda e: e.memset(MH[:], -0.5), writes=[B_C])
        for i in range(2):
            R.op('pool', lambda e, i=i: e.memset(FM1[i][:], 0.0), writes=[B_FM1[i]])
        R.op('pool', lambda e: e.memset(AEND[:], 1.0), writes=B_AEND)
        R.op('dve', lambda e: e.tensor_tensor(out=DER[:, 32:40], in0=CPK[:, P_L0:P_L0 + 8], in1=CPK[:, P_L1:P_L1 + 8],
                                              op=ALU.subtract), reads=[B_C], writes=[B_C])
        R.op('act', lambda e: e.activation(out=DER[:, 40:48], in_=DER[:, 32:40], func=AF.Tanh, scale=0.5),
             reads=[B_C], writes=[B_C])
        R.op('dve', lambda e: e.tensor_scalar(out=DER[:, C1:C1 + 8], in0=DER[:, 40:48], scalar1=-0.25, scalar2=0.25,
                                              op0=ALU.mult, op1=ALU.add), reads=[B_C], writes=[B_C])
        R.op('dve', lambda e: e.tensor_scalar(out=DER[:, C0:C0 + 8], in0=DER[:, 40:48], scalar1=0.25, scalar2=0.75,
                                              op0=ALU.mult, op1=ALU.add), reads=[B_C], writes=[B_C])
        R.op('dve', lambda e: e.tensor_scalar(out=DER[:, NC1:NC1 + 8], in0=DER[:, 40:48], scalar1=0.25, scalar2=-0.25,
                                              op0=ALU.mult, op1=ALU.add), reads=[B_C], writes=[B_C])

        def cscale(dst, src, n, s):
            R.op('dve', lambda e: e.tensor_scalar(out=DER[:, dst:dst + n], in0=CPK[:, src:src + n], scalar1=s,
                                                  scalar2=None, op0=ALU.mult), reads=[B_C], writes=[B_C])
        cscale(GHG, P_GHG, 1, 0.5)
        cscale(GQ8, P_GQ, 1, 0.125)
        cscale(GK2, P_GK, 1, 1.0)
        cscale(GDS, P_GDS, 1, 0.4)
        cscale(GMQ, P_GMQ, 2, 1.0 / 16.0)
        cscale(GMKP, P_GMK, 2, 1.0)
        R.op('dve', lambda e: e.tensor_tensor(out=DER[:, 48:64].bitcast(F32), in0=CBC[:, 0:16], in1=CBC[:, 0:16], op=ALU.mult),
             reads=[B_C], writes=[B_C])
        sc1 = A.alloc(1)
        R.op('dve', lambda e: e.tensor_tensor(out=sc1.f(0, 64), in0=CBC[:, 0:64], in1=CBC[:, 64:128], op=ALU.mult),
             reads=[B_C], writes=sc1.b)
        R.op('dve', lambda e: e.tensor_tensor(out=sc1.f(64, 128), in0=CBC[:, 128:192], in1=CBC[:, 192:256], op=ALU.mult),
             reads=[B_C], writes=sc1.b)
        R.op('dve', lambda e: e.tensor_reduce(out=DER[:, 48:50], in_=bview(sc1.f(0, 128), 2, 64), axis=AX.X, op=ALU.add),
             reads=sc1.b, writes=[B_C])
        R.op('act', lambda e: e.activation(out=DER[:, 50:52], in_=DER[:, 48:50], func=AF.Exp), reads=[B_C], writes=[B_C])
        R.op('dve', lambda e: e.scalar_tensor_tensor(out=DER[:, LAMN:LAMN + 1], in0=DER[:, 51:52], scalar=-0.2,
                                                     in1=DER[:, 50:51], op0=ALU.add, op1=ALU.subtract),
             reads=[B_C], writes=[B_C])
        sc1.free()
        for blk in range(16):
            R.op('pool', lambda e, blk=blk: e.memset(
                bview(VA[:, blk * 1040:(blk + 1) * 1040], 8, 130)[:, :, 128:129], 1.0), writes=[B_VA[blk]])
        for kb in range(2):
            R.op('pool', lambda e, kb=kb: e.memset(
                bview(VM[:, kb * 1032:(kb + 1) * 1032], 4, 258)[:, :, 256:257], 1.0), writes=[B_VM[kb]])

        def col(c):
            return DER[:, c:c + 1]

        WSRC = {'in': wb_in, 'mkv': wb_mkv, 'br': wb_br, 'out': wb_out}
        plan = []

        def plan_tile(kind):
            p = []
            if kind == 'mem':
                p += [('mkv', 0, c) for c in range(0, 2048, 512)]
                return p
            p += [('in', 0, c) for c in range(0, 4096, 512)]
            for n in range(3):
                if n == 1:
                    for g in range(2):
                        p += [('in', 0, 4096 + g * 512), ('in', 0, 5120 + g * 512),
                              ('in', 0, 6144 + g * 512), ('in', 0, 7168 + g * 512)]
                if n == 2:
                    p += [('in', 0, c) for c in range(8192, 10240, 512)]
                p += [('in', 0, 10240 + n * 1024 + g * 512) for g in range(2)]
                p += [('br', n * 1024, g * 512) for g in range(2)]
            p += [('out', 0, 0), ('out', 0, 512)]
            return p

        for b in range(nb_run):
            plan += plan_tile('mem')
            for j in range(ntiles):
                plan += plan_tile('main')
        if do_sample:
            plan += plan_tile('main')
        ws_state = {'issued': 0, 'cur': 0}

        WF32 = {'in': w_in, 'mkv': w_mkv, 'br': w_br, 'out': w_out}
        B_cvt = {}
        conv_state = {'next': 0}

        def conv_ensure(n):
            while conv_state['next'] < min(n, len(plan)):
                key = plan[conv_state['next']]
                conv_state['next'] += 1
                if key in B_cvt:
                    continue
                src, r0, c0 = key
                B_cvt[key] = Buf("cvt_%s_%d_%d" % key)
                R.op('pool', lambda e, src=src, r0=r0, c0=c0: e.dma_start(
                    out=WSRC[src][r0:r0 + 1024, c0:c0 + 512], in_=WF32[src][r0:r0 + 1024, c0:c0 + 512]),
                    writes=[B_cvt[key]], dma_key="wc%d" % (len(B_cvt) % 2))

        def ws_issue_upto(n):
            while ws_state['issued'] < min(n, len(plan)):
                i = ws_state['issued']
                conv_ensure(i + 1 + CONV_AHEAD)
                src, r0, c0 = plan[i]
                s = i % 3
                srcap = WSRC[src][r0:r0 + 1024, c0:c0 + 512].rearrange("(kc p) c -> p kc c", p=128)
                R.op('sp', lambda e, s=s, srcap=srcap: e.dma_start(
                    out=WS[s][:].rearrange("p (kc c) -> p kc c", kc=8), in_=srcap),
                    reads=[B_cvt[plan[i]]], writes=[B_WS[s]], dma_key="w%d" % s)
                ws_state['issued'] += 1

        pending = []

        def defer(fn, n=STORE_DELAY):
            pending.append([n, fn])

        def flush_pending(everything=False):
            for it in list(pending):
                it[0] -= 1
                if everything or it[0] <= 0:
                    pending.remove(it)
                    it[1]()

        def ws_take(src, r0, c0):
            i = ws_state['cur']
            assert plan[i] == (src, r0, c0), (i, plan[i], (src, r0, c0))
            ws_issue_upto(i + 3)
            flush_pending()
            ws_state['cur'] += 1
            s = i % 3
            return WS[s], B_WS[s]

        def wcol(wt, kc, c0, c1):
            return wt[:, kc * 512 + c0:kc * 512 + c1]

        def rstd_into(dst_ap, dst_b, ss_ap, ss_b, n_groups, inv_n):
            tmp, tb_ = sm_next()
            R.op('dve', lambda e: e.tensor_scalar(out=tmp[:, 0:n_groups], in0=ss_ap, scalar1=inv_n, scalar2=EPS,
                                                  op0=ALU.mult, op1=ALU.add), reads=[ss_b], writes=[tb_])
            R.op('pool', lambda e: e.tensor_tensor(out=dst_ap, in0=tmp[:, 0:n_groups], in1=MH[:, 0:n_groups], op=ALU.pow),
                 reads=[tb_, B_C], writes=[dst_b])

        def group_stats(ps_ap, ps_b, g, j):
            junk = A.alloc(1)
            R.op('act', lambda e: e.activation(out=junk.bf(0, g * j), in_=ps_ap, func=AF.Square), reads=[ps_b], writes=junk.b)
            ss, ssb = sm_next()
            R.op('dve', lambda e: e.tensor_reduce(out=ss[:, 0:g], in_=bview(junk.bf(0, g * j), g, j), axis=AX.X, op=ALU.add),
                 reads=junk.b, writes=[ssb])
            junk.free()
            r, rb = sm_next()
            rstd_into(r[:, 0:g], rb, ss[:, 0:g], ssb, g, 1.0 / j)
            return r, rb

        def xstage_a(src_rows, TB):
            xn = []
            for tb in range(TB):
                xs = A.alloc(4)
                R.op('sp', lambda e, xs=xs, tb=tb: e.dma_start(out=xs.f(0, 1024), in_=src_rows(tb)), writes=xs.b,
                     dma_key="xin%d" % (tb % 2))
                junk = A.alloc(2)
                ss, ssb = sm_next()
                R.op('act', lambda e, xs=xs, junk=junk, ss=ss: e.activation(out=junk.bf(0, 1024), in_=xs.f(0, 1024),
                                                                           func=AF.Square, accum_out=ss[:, 0:1]),
                     reads=xs.b, writes=junk.b + [ssb])
                junk.free()
                r, rb = sm_next()
                rstd_into(r[:, 0:1], rb, ss[:, 0:1], ssb, 1, 1.0 / 1024)
                xb = A.alloc(2)
                R.op('act', lambda e, xs=xs, xb=xb, r=r: e.activation(out=xb.bf(0, 1024), in_=xs.f(0, 1024), func=AF.Copy,
                                                                      scale=r[:, 0:1]), reads=xs.b + [rb], writes=xb.b)
                xs.free()
                xn.append(xb)
            return xn

        def xstage_b(xn, TB, gcol):
            NT = TB * 128
            xnT = []
            for kc in range(8):
                tr, trb = tr_next()
                for tb in range(TB):
                    R.op('pe', lambda e, tr=tr, tb=tb, kc=kc: e.transpose(
                        tr[:, tb * 128:(tb + 1) * 128], xn[tb].bf(kc * 128, (kc + 1) * 128), IDB[:]),
                        reads=xn[tb].b + [B_C], writes=[trb])
                p = A.alloc(1)
                R.op('dve', lambda e, tr=tr, p=p, kc=kc: e.tensor_scalar(
                    out=p.bf(0, NT), in0=tr[:, 0:NT], scalar1=CPK[:, gcol + kc:gcol + kc + 1], scalar2=None, op0=ALU.mult),
                    reads=[trb, B_C], writes=p.b)
                xnT.append(p)
            for x_ in xn:
                x_.free()
            return xnT

        def fproj(src, r0, c0, rhs, rhs_bufs, NT, consume):
            wt, wb_ = ws_take(src, r0, c0)
            for ch in range(4):
                ps, psb_ = mm_next()
                for kc in range(8):
                    R.op('pe', lambda e, ps=ps, kc=kc, ch=ch: e.matmul(
                        ps[:, 0:NT], lhsT=wcol(wt, kc, ch * 128, (ch + 1) * 128), rhs=rhs[kc].bf(0, NT),
                        start=(kc == 0), stop=(kc == 7)),
                        reads=[wb_] + rhs_bufs[kc], writes=[psb_])
                consume(ch, ps, psb_)

        def tproj(src, r0, c0, xnT, TB, consume):
            wt, wb_ = ws_take(src, r0, c0)
            for tb in range(TB):
                ps, psb_ = mm_next()
                for kc in range(8):
                    R.op('pe', lambda e, ps=ps, kc=kc, tb=tb: e.matmul(
                        ps[:, 0:512], lhsT=xnT[kc].bf(tb * 128, (tb + 1) * 128), rhs=wcol(wt, kc, 0, 512),
                        start=(kc == 0), stop=(kc == 7)),
                        reads=[wb_] + xnT[kc].b, writes=[psb_])
                consume(tb, ps, psb_)

        def transpose_blocks(srcs, c0, TB, evac):
            tr, trb = tr_next()
            for tb in range(TB):
                R.op('pe', lambda e, tb=tb: e.transpose(tr[:, tb * 128:(tb + 1) * 128], srcs[tb].bf(c0, c0 + 128), IDB[:]),
                     reads=srcs[tb].bb(c0, c0 + 128) + [B_C], writes=[trb])
            evac(tr, trb)

        def gate_chunk(ps, psb_, NT):
            tz = A.alloc(1)
            R.op('act', lambda e: e.activation(out=tz.bf(0, NT), in_=ps[:, 0:NT], func=AF.Tanh, scale=0.5),
                 reads=[psb_], writes=tz.b)
            u = A.alloc(1)
            R.op('dve', lambda e: e.scalar_tensor_tensor(out=u.bf(0, NT), in0=tz.bf(0, NT), scalar=1.0, in1=ps[:, 0:NT],
                                                         op0=ALU.add, op1=ALU.mult), reads=tz.b + [psb_], writes=u.b)
            tz.free()
            return u

        def norm_rows_bf(src_regs, TB, g, j):
            outs = []
            for tb in range(TB):
                junk = A.alloc(2)
                R.op('act', lambda e, tb=tb, junk=junk: e.activation(out=junk.bf(0, 1024), in_=src_regs[tb].bf(0, 1024),
                                                                     func=AF.Square), reads=src_regs[tb].b, writes=junk.b)
                ss, ssb = sm_next()
                R.op('dve', lambda e, junk=junk, ss=ss: e.tensor_reduce(out=ss[:, 0:g], in_=bview(junk.bf(0, 1024), g, j),
                                                                        axis=AX.X, op=ALU.add), reads=junk.b, writes=[ssb])
                junk.free()
                r, rb = sm_next()
                rstd_into(r[:, 0:g], rb, ss[:, 0:g], ssb, g, 1.0 / j)
                o = A.alloc(2)
                R.op('dve', lambda e, tb=tb, o=o, r=r: e.tensor_tensor(
                    out=bview(o.bf(0, 1024), g, j), in0=bview(src_regs[tb].bf(0, 1024), g, j), in1=bcast_last(r[:, 0:g], j),
                    op=ALU.mult), reads=src_regs[tb].b + [rb], writes=o.b)
                outs.append(o)
            return outs

        def merge_gates(n, NT):
            tgs = []
            for g in range(2):
                def cons_g(ch, ps, psb_):
                    tg = A.alloc(1)
                    R.op('act', lambda e: e.activation(out=tg.bf(0, NT), in_=ps[:, 0:NT], func=AF.Tanh, scale=0.5),
                         reads=[psb_], writes=tg.b)
                    tgs.append(tg)
                fproj('in', 0, 10240 + n * 1024 + g * 512, XNT['r'], XNT['b'], NT, cons_g)
            return tgs

        def merge_proj(n, yg, NT, hacc, tgs):
            yg_b = [p.b for p in yg]
            for g in range(2):
                def cons_b(ch, ps, psb_):
                    dch = g * 4 + ch
                    tg = tgs[dch]
                    if n not in DBG_BR:
                        if n == 0:
                            R.op('pool', lambda e: e.memset(hacc[dch].bf(0, NT), 0.0), writes=hacc[dch].b)
                        R.op('dve', lambda e: e.tensor_copy(out=tg.bf(0, NT), in_=ps[:, 0:NT]), reads=[psb_], writes=tg.b)
                    elif n == 0:
                        R.op('dve', lambda e: e.scalar_tensor_tensor(out=hacc[dch].bf(0, NT), in0=tg.bf(0, NT), scalar=1.0,
                                                                     in1=ps[:, 0:NT], op0=ALU.add, op1=ALU.mult),
                             reads=tg.b + [psb_], writes=hacc[dch].b)
                    else:
                        tmp = A.alloc(1)
                        R.op('dve', lambda e: e.scalar_tensor_tensor(out=tmp.bf(0, NT), in0=tg.bf(0, NT), scalar=1.0,
                                                                     in1=ps[:, 0:NT], op0=ALU.add, op1=ALU.mult),
                             reads=tg.b + [psb_], writes=tmp.b)
                        R.op('pool', lambda e: e.tensor_tensor(out=hacc[dch].bf(0, NT), in0=hacc[dch].bf(0, NT),
                                                               in1=tmp.bf(0, NT), op=ALU.add),
                             reads=tmp.b + hacc[dch].b, writes=hacc[dch].b)
                        tmp.free()
                    tg.free()
                fproj('br', n * 1024, g * 512, yg, yg_b, NT, cons_b)

        XNT = {}

        def hgrn_stage(NT, TB, sample, b):
            xnT, xb_ = XNT['r'], XNT['b']
            NCH = NT // 64
            qraw = [None] * 8
            qdec = [None] * 8
            kinv = [None] * 8
            ua = [None] * 8

            def cons_q(base):
                def f(ch, ps, psb_):
                    h = base + ch
                    q = A.alloc(1)
                    R.op('act', lambda e: e.activation(out=q.bf(0, NT), in_=ps[:, 0:NT], func=AF.Copy), reads=[psb_], writes=q.b)
                    qraw[h] = q
                return f
            for g in range(2):
                fproj('in', 0, g * 512, xnT, xb_, NT, cons_q(g * 4))

            def cons_f(base):
                def f(ch, ps, psb_):
                    h = base + ch
                    t = A.alloc(2)
                    R.op('act', lambda e: e.activation(out=t.f(0, NT), in_=ps[:, 0:NT], func=AF.Tanh, scale=0.5),
                         reads=[psb_], writes=t.b)
                    fm0 = A.alloc(2)
                    R.op('act', lambda e: e.activation(out=fm0.f(0, NT), in_=t.f(0, NT), func=AF.Identity,
                                                       scale=col(C1 + h), bias=col(C0 + h)),
                         reads=t.b + [B_C], writes=fm0.b)
                    pi = state['fm1']
                    state['fm1'] = 1 - pi
                    fm1, fm1b = FM1[pi], B_FM1[pi]
                    st0v = bview(fm0.f(0, NT), NCH, 64)[:, :, 0]
                    R.op('dve', lambda e: e.tensor_copy(out=bview(fm1[:, 0:NT], NCH, 64)[:, :, 0], in_=st0v),
                         reads=fm0.b, writes=[fm1b])
                    R.op('dve', lambda e: e.memset(st0v, 0.0), writes=fm0.b)
                    Acp = A.alloc(2)
                    R.op('dve', lambda e: e.tensor_tensor_scan(out=Acp.f(0, NT), data0=fm0.f(0, NT), data1=fm1[:, 0:NT],
                                                               initial=0.0, op0=ALU.mult, op1=ALU.add),
                         reads=fm0.b + [fm1b], writes=Acp.b)
                    fm0.free()
                    R.op('dve', lambda e: e.tensor_copy(out=AEND[:, h * 8:h * 8 + NCH],
                                                        in_=bview(Acp.f(0, NT), NCH, 64)[:, :, 63]),
                         reads=Acp.b, writes=[B_AEND[h]])
                    rA = A.alloc(2)
                    R.op('dve', lambda e: e.reciprocal(out=rA.f(0, NT), in_=Acp.f(0, NT)), reads=Acp.b, writes=rA.b)
                    k_ = A.alloc(2)
                    R.op('act', lambda e: e.activation(out=k_.f(0, NT), in_=t.f(0, NT), func=AF.Identity,
                                                       scale=col(NC1 + h), bias=col(C1 + h)),
                         reads=t.b + [B_C], writes=k_.b)
                    t.free()
                    ki = A.alloc(1)
                    R.op('pool', lambda e: e.tensor_tensor(out=ki.bf(0, NT), in0=k_.f(0, NT), in1=rA.f(0, NT), op=ALU.mult),
                         reads=k_.b + rA.b, writes=ki.b)
                    k_.free()
                    rA.free()
                    qd = A.alloc(1)
                    R.op('pool', lambda e: e.tensor_tensor(out=qd.bf(0, NT), in0=qraw[h].bf(0, NT), in1=Acp.f(0, NT), op=ALU.mult),
                         reads=qraw[h].b + Acp.b, writes=qd.b)
                    Acp.free()
                    qraw[h].free()
                    kinv[h] = ki
                    qdec[h] = qd
                return f
            for g in range(2):
                fproj('in', 0, 1024 + g * 512, xnT, xb_, NT, cons_f(g * 4))

            vhg = [A.alloc(2) for _ in range(TB)]

            def cons_v(half):
                def f(tb, ps, psb_):
                    R.op('act', lambda e: e.activation(out=vhg[tb].bf(half * 512, half * 512 + 512), in_=ps[:, 0:512], func=AF.Copy),
                         reads=[psb_], writes=vhg[tb].bb(half * 512, half * 512 + 512))
                return f
            for g in range(2):
                tproj('in', 0, 2048 + g * 512, xnT, TB, cons_v(g))

            def cons_z(base):
                def f(ch, ps, psb_):
                    ua[base + ch] = gate_chunk(ps, psb_, NT)
                return f
            for g in range(2):
                fproj('in', 0, 3072 + g * 512, xnT, xb_, NT, cons_z(g * 4))

            set_wide(False)
            kinvT = [A.alloc(2) for _ in range(TB)]
            for tb in range(TB):
                for half in range(2):
                    tr, trb = tr_next()
                    for hh in range(4):
                        h = half * 4 + hh
                        R.op('pe', lambda e, tr=tr, hh=hh, h=h, tb=tb: e.transpose(
                            tr[:, hh * 128:(hh + 1) * 128], kinv[h].bf(tb * 128, (tb + 1) * 128), IDB[:]),
                            reads=kinv[h].b + [B_C], writes=[trb])
                    R.op('act', lambda e, tr=tr, tb=tb, half=half: e.activation(
                        out=kinvT[tb].bf(half * 512, half * 512 + 512), in_=tr[:, 0:512], func=AF.Copy),
                        reads=[trb], writes=kinvT[tb].bb(half * 512, half * 512 + 512))

            on = []
            pend_norm = []
            for tb in range(TB):
                for cc in range(2):
                    c = tb * 2 + cc
                    r0, r1 = cc * 64, cc * 64 + 64
                    if sample:
                        R.op('sp', lambda e, c=c: e.dma_start(out=SF[:].rearrange("p (h v) -> p h v", h=8),
                                                              in_=st0[c].rearrange("h k v -> k h v")),
                             writes=B_SF, dma_key="sld")
                        R.op('act', lambda e: e.activation(out=SB[:], in_=SF[:], func=AF.Copy), reads=B_SF, writes=B_SB)
                    ps1, ps1b = PS[2 + c % 2], B_PS[2 + c % 2]
                    ob = (4, 5) if tb % 2 == 0 else (0, 1)
                    for h in range(8):
                        R.op('pe', lambda e, h=h, c=c, ps1=ps1, r0=r0, r1=r1: e.matmul(
                            ps1[r0:r1, h * 64:(h + 1) * 64], lhsT=kinv[h].bf(c * 64, c * 64 + 64),
                            rhs=qdec[h].bf(c * 64, c * 64 + 64), start=True, stop=True),
                            reads=kinv[h].b + qdec[h].b, writes=[ps1b])
                    attm = A.alloc(1)
                    R.op('dve', lambda e, ps1=ps1, attm=attm, r0=r0, r1=r1: e.tensor_tensor(
                        out=bview(attm.rows_bf(r0, r1, 0, 512), 8, 64), in0=bview(ps1[r0:r1, 0:512], 8, 64),
                        in1=bcast_mid(MHG[r0:r1, :], 8), op=ALU.mult), reads=[ps1b, B_C], writes=attm.b)
                    while pend_norm:
                        pend_norm.pop(0)()
                    for h in range(8):
                        pa, pab = acc(2 + h // 4)
                        cs = (h % 4) * 128
                        R.op('pe', lambda e, h=h, pa=pa, cs=cs, tb=tb, r0=r0, r1=r1: e.matmul(
                            pa[:, cs:cs + 128], lhsT=kinvT[tb].rows_bf(r0, r1, h * 128, h * 128 + 128),
                            rhs=vhg[tb].rows_bf(r0, r1, h * 128, h * 128 + 128), start=True, stop=True),
                            reads=kinvT[tb].b + vhg[tb].b, writes=[pab])
                    for h in range(8):
                        pa, pab = PS[ob[h // 4]], B_PS[ob[h // 4]]
                        cs = (h % 4) * 128
                        R.op('pe', lambda e, h=h, pa=pa, cs=cs, tb=tb, attm=attm, r0=r0, r1=r1: e.matmul(
                            pa[r0:r1, cs:cs + 128], lhsT=attm.rows_bf(r0, r1, h * 64, h * 64 + 64),
                            rhs=vhg[tb].rows_bf(r0, r1, h * 128, h * 128 + 128), start=True, stop=False),
                            reads=attm.b + vhg[tb].b, writes=[pab])
                        R.op('pe', lambda e, h=h, pa=pa, cs=cs, c=c, r0=r0, r1=r1: e.matmul(
                            pa[r0:r1, cs:cs + 128], lhsT=qdec[h].bf(c * 64, c * 64 + 64),
                            rhs=SB[:, h * 128:(h + 1) * 128], start=False, stop=True),
                            reads=qdec[h].b + [B_SB[h]], writes=[pab])
                    attm.free()
                    for hb in range(2):
                        pa, pab = acc(2 + hb)
                        sfv = SF[:, hb * 512:(hb + 1) * 512]
                        aeb = AEND[:, hb * 32:(hb + 1) * 32].rearrange("p (h c) -> p h c", c=8)[:, :, c]
                        bsf = B_SF[hb * 4:hb * 4 + 4]
                        R.op('dve', lambda e, sfv=sfv, pa=pa: e.tensor_tensor(out=sfv, in0=sfv, in1=pa[:, 0:512], op=ALU.add),
                             reads=[pab] + bsf, writes=bsf)
                        R.op('dve', lambda e, sfv=sfv, aeb=aeb: e.tensor_tensor(
                            out=bview(sfv, 4, 128), in0=bview(sfv, 4, 128), in1=bcast_last(aeb, 128), op=ALU.mult),
                            reads=bsf + B_AEND[hb * 4:hb * 4 + 4], writes=bsf)
                        R.op('act', lambda e, sfv=sfv, hb=hb: e.activation(out=SB[:, hb * 512:(hb + 1) * 512], in_=sfv, func=AF.Copy),
                             reads=bsf, writes=B_SB[hb * 4:hb * 4 + 4])
                    if sample:
                        R.op('sp', lambda e, c=c: e.dma_start(out=st_s[c].rearrange("h k v -> k h v"),
                                                              in_=SF[:].rearrange("p (h v) -> p h v", h=8)),
                             reads=B_SF, dma_key="sst")
                def norm_o(ob=ob):
                    o_n = A.alloc(2)
                    for i in range(2):
                        pa, pab = PS[ob[i]], B_PS[ob[i]]
                        r, rb = group_stats(pa[:, 0:512], pab, 4, 128)
                        R.op('dve', lambda e, pa=pa, o_n=o_n, i=i, r=r: e.tensor_tensor(
                            out=bview(o_n.bf(i * 512, i * 512 + 512), 4, 128), in0=bview(pa[:, 0:512], 4, 128),
                            in1=bcast_last(r[:, 0:4], 128), op=ALU.mult), reads=[pab, rb], writes=o_n.bb(i * 512, i * 512 + 512))
                    on.append(o_n)
                pend_norm.append(norm_o)
            while pend_norm:
                pend_norm.pop(0)()
            if (not sample) and b is not None:
                R.op('sp', lambda e: e.dma_start(out=st_p[b].rearrange("h k v -> k h v"),
                                                 in_=SF[:].rearrange("p (h v) -> p h v", h=8)), reads=B_SF, dma_key="sst")
            for x_ in kinv + qdec + vhg + kinvT:
                x_.free()
            set_wide(True)
            return on, ua

        def gated_y(src, us, TB, NT, scale):
            ys = []
            for c in range(8):
                y = A.alloc(1)

                def ev(tr, trb, y=y, c=c):
                    R.op('dve', lambda e: e.scalar_tensor_tensor(out=y.bf(0, NT), in0=tr[:, 0:NT], scalar=scale,
                                                                 in1=us[c].bf(0, NT), op0=ALU.mult, op1=ALU.mult),
                         reads=[trb, B_C] + us[c].b, writes=y.b)
                transpose_blocks(src, c * 128, TB, ev)
                us[c].free()
                ys.append(y)
            for x_ in src:
                x_.free()
            return ys

        def pv_finish_diff(cmap, tbs, rows, h, o0, od):
            for (ai, tb) in tbs:
                pa, pab = acc(ai)
                r0, r1 = rows
                rr, rrb = sm_next()
                R.op('dve', lambda e, pa=pa, rr=rr: e.reciprocal(out=rr[r0:r1, 0:1], in_=pa[r0:r1, 128:129]),
                     reads=[pab], writes=[rrb])
                if cmap == 0:
                    R.op('dve', lambda e, pa=pa, rr=rr, ai=ai: e.tensor_scalar(
                        out=o0.ar.tf[r0:r1, o0.p0 * 256 + ai * 128:o0.p0 * 256 + ai * 128 + 128], in0=pa[r0:r1, 0:128],
                        scalar1=rr[r0:r1, 0:1], scalar2=None, op0=ALU.mult), reads=[pab, rrb], writes=o0.b)
                else:
                    R.op('dve', lambda e, rr=rr: e.tensor_scalar(out=rr[r0:r1, 1:2], in0=rr[r0:r1, 0:1],
                                                                 scalar1=DER[r0:r1, LAMN:LAMN + 1], scalar2=None, op0=ALU.mult),
                         reads=[rrb, B_C], writes=[rrb])
                    R.op('dve', lambda e, pa=pa, rr=rr, ai=ai, tb=tb: e.scalar_tensor_tensor(
                        out=od[tb].rows_bf(r0, r1, h * 128, h * 128 + 128), in0=pa[r0:r1, 0:128], scalar=rr[r0:r1, 1:2],
                        in1=o0.ar.tf[r0:r1, o0.p0 * 256 + ai * 128:o0.p0 * 256 + ai * 128 + 128], op0=ALU.mult, op1=ALU.add),
                        reads=[pab, rrb] + o0.b, writes=od[tb].bb(h * 128, h * 128 + 128))

        def diff_attn_prompt(j, QZ, od):
            NT = 512
            nkb = 4 * j + 4
            steps = [(h, cmap, kb) for h in range(8) for cmap in range(2) for kb in range(nkb)]
            o0s = {}

            def emit_S(h, cmap, kb):
                q0 = max(kb - 4 * j, 0)
                ncol = NT - q0 * 128
                ps, psb_ = mm_next()
                qz = QZ[h][cmap]
                R.op('pe', lambda e: e.matmul(
                    ps[:, 0:ncol], lhsT=KT[:, h * SEQ + kb * 128:h * SEQ + kb * 128 + 128],
                    rhs=qz.bf(q0 * 128, NT), start=True, stop=True),
                    reads=[B_KT[h][kb // 4]] + qz.b, writes=[psb_])
                E = A.alloc(1)
                R.op('act', lambda e: e.activation(out=E.bf(0, ncol), in_=ps[:, 0:ncol], func=AF.Exp),
                     reads=[psb_], writes=E.b)
                if kb >= 4 * j:
                    R.op('pool', lambda e: e.memset(E.rows_bf(64, 128, 0, 64), 0.0), writes=E.b)
                return E

            def emit_PV(h, cmap, kb, E):
                q0 = max(kb - 4 * j, 0)
                if cmap == 0 and kb == 0:
                    o0s[h] = A.alloc(2)
                for qb in range(q0, 4):
                    pa, pab = acc(qb)
                    R.op('pe', lambda e, pa=pa, qb=qb: e.matmul(
                        pa[:, 0:129], lhsT=E.bf((qb - q0) * 128, (qb - q0) * 128 + 128),
                        rhs=VA[:, kb * 1040 + h * 130:kb * 1040 + h * 130 + 129],
                        start=(kb == 0), stop=(kb == 4 * j + qb)),
                        reads=E.b + [B_VA[kb]], writes=[pab])
                    if kb == 4 * j + qb:
                        pv_finish_diff(cmap, [(qb, qb)], (0, 128), h, o0s[h], od)
                E.free()
                if cmap == 1 and kb == nkb - 1:
                    o0s.pop(h).free()

            pend = []
            for stp in steps:
                E = emit_S(*stp)
                pend.append(stp + (E,))
                if len(pend) > PIPE_DEPTH:
                    emit_PV(*pend.pop(0))
            while pend:
                emit_PV(*pend.pop(0))

        def mem_attn(NT, TB, QM, om, qsegs):
            c_lo = min(s[0] for s in qsegs)
            c_hi = max(s[0] + s[1] for s in qsegs)
            for h in range(4):
                Es = []
                for kb in range(2):
                    ps, psb_ = mm_next()
                    for dc in range(2):
                        ci = h * 2 + dc
                        R.op('pe', lambda e, ps=ps, ci=ci, kb=kb, dc=dc: e.matmul(
                            ps[:, c_lo:c_hi], lhsT=KM[:, ci * 256 + kb * 128:ci * 256 + kb * 128 + 128],
                            rhs=QM[ci].bf(c_lo, c_hi), start=(dc == 0), stop=(dc == 1)),
                            reads=[B_KM] + QM[ci].b, writes=[psb_])
                    E = A.alloc(1)
                    R.op('act', lambda e, ps=ps, E=E: e.activation(out=E.bf(c_lo, c_hi), in_=ps[:, c_lo:c_hi], func=AF.Exp),
                         reads=[psb_], writes=E.b)
                    Es.append(E)
                for (c0, ncl, ai, tb, r0) in qsegs:
                    pa, pab = acc(ai)
                    for kb in range(2):
                        R.op('pe', lambda e, pa=pa, kb=kb, c0=c0, ncl=ncl, r0=r0, h=h, Es=Es: e.matmul(
                            pa[r0:r0 + ncl, 0:257], lhsT=Es[kb].bf(c0, c0 + ncl),
                            rhs=VM[:, kb * 1032 + h * 258:kb * 1032 + h * 258 + 257], start=(kb == 0), stop=(kb == 1)),
                            reads=Es[kb].b + [B_VM[kb]], writes=[pab])
                    rr, rrb = sm_next()
                    R.op('dve', lambda e, pa=pa, rr=rr, r0=r0, ncl=ncl: e.reciprocal(out=rr[r0:r0 + ncl, 0:1],
                                                                                 in_=pa[r0:r0 + ncl, 256:257]),
                         reads=[pab], writes=[rrb])
                    R.op('dve', lambda e, pa=pa, rr=rr, r0=r0, ncl=ncl, tb=tb, h=h: e.tensor_scalar(
                        out=om[tb].rows_bf(r0, r0 + ncl, h * 256, h * 256 + 256), in0=pa[r0:r0 + ncl, 0:256],
                        scalar1=rr[r0:r0 + ncl, 0:1], scalar2=None, op0=ALU.mult),
                        reads=[pab, rrb], writes=om[tb].bb(h * 256, h * 256 + 256))
                for E in Es:
                    E.free()

        def main_tile(NT, TB, sample, b, j, xn_pre):
            if sample:
                src_rows = lambda tb: xsm[tb * 128:(tb + 1) * 128, :]
                yout, dkout, dvout, row0 = y_s, dk_s, dv_s, 0
            else:
                row0 = b * SEQ + j * 512
                src_rows = lambda tb: xp[row0 + tb * 128:row0 + (tb + 1) * 128, :]
                yout, dkout, dvout = y_p, dk_p, dv_p
            xnT = xstage_b(xn_pre, TB, P_GN)
            XNT['r'] = xnT
            XNT['b'] = [p.b for p in xnT]
            xb_ = XNT['b']
            if (not sample) and j == 0:
                R.op('pool', lambda e: e.memset(SF[:], 0.0), writes=B_SF)
                R.op('pool', lambda e: e.memset(SB[:], 0.0), writes=B_SB)
            hacc = [A.alloc(1) for _ in range(8)]
            on, ua = hgrn_stage(NT, TB, sample, b if (not sample and j == ntiles - 1) else None)
            tgs = merge_gates(0, NT)
            ya = gated_y(on, ua, TB, NT, col(GHG))
            merge_proj(0, ya, NT, hacc, tgs)
            for y in ya:
                y.free()

            qn = [A.alloc(2) for _ in range(TB)]

            def cons_qk(dst, half, fp32_out=None):
                def f(tb, ps, psb_):
                    r, rb = group_stats(ps[:, 0:512], psb_, 8, 64)
                    if fp32_out is None:
                        R.op('dve', lambda e: e.tensor_tensor(out=bview(dst[tb].bf(half * 512, half * 512 + 512), 8, 64),
                                                              in0=bview(ps[:, 0:512], 8, 64), in1=bcast_last(r[:, 0:8], 64),
                                                              op=ALU.mult), reads=[psb_, rb], writes=dst[tb].bb(half * 512, half * 512 + 512))
                    else:
                        ko = fp32_out[tb]
                        R.op('dve', lambda e: e.tensor_tensor(out=bview(ko.f(half * 512, half * 512 + 512), 8, 64),
                                                              in0=bview(ps[:, 0:512], 8, 64), in1=bcast_last(r[:, 0:8], 64),
                                                              op=ALU.mult), reads=[psb_, rb], writes=ko.bF(half * 512, half * 512 + 512))
                        R.op('dve', lambda e: e.tensor_tensor(out=bview(dst[tb].bf(half * 512, half * 512 + 512), 8, 64),
                                                              in0=bview(ps[:, 0:512], 8, 64), in1=bcast_last(r[:, 0:8], 64),
                                                              op=ALU.mult), reads=[psb_, rb], writes=dst[tb].bb(half * 512, half * 512 + 512))
                        R.op('pool', lambda e: e.tensor_tensor(out=bview(ko.f(half * 512, half * 512 + 512), 8, 64),
                                                               in0=bview(ko.f(half * 512, half * 512 + 512), 8, 64),
                                                               in1=bcast_mid(CBC[:, 256:320], 8), op=ALU.mult),
                             reads=ko.bF(half * 512, half * 512 + 512) + [B_C], writes=ko.bF(half * 512, half * 512 + 512))
                return f
            kn = [A.alloc(2) for _ in range(TB)]
            kof = [A.alloc(4) for _ in range(TB)]
            def st_k(kof=kof):
                for tb in range(TB):
                    R.op('sp', lambda e, tb=tb: e.dma_start(out=dkout[row0 + tb * 128:row0 + (tb + 1) * 128, :], in_=kof[tb].f(0, 1024)),
                         reads=kof[tb].b, dma_key="ko%d" % (tb % 2))
                    kof[tb].free()
            vof = [A.alloc(4) for _ in range(TB)]
            vnew = [A.alloc(2) for _ in range(TB)] if sample else None

            def cons_dv(half):
                def f(tb, ps, psb_):
                    R.op('act', lambda e: e.activation(out=vof[tb].f(half * 512, half * 512 + 512), in_=ps[:, 0:512], func=AF.Copy),
                         reads=[psb_], writes=vof[tb].bF(half * 512, half * 512 + 512))
                    if sample:
                        R.op('dve', lambda e: e.tensor_copy(out=vnew[tb].bf(half * 512, half * 512 + 512), in_=ps[:, 0:512]),
                             reads=[psb_], writes=vnew[tb].bb(half * 512, half * 512 + 512))
                    else:
                        blk = j * 4 + tb
                        R.op('dve', lambda e: e.tensor_copy(
                            out=bview(VA[:, blk * 1040 + half * 520:blk * 1040 + half * 520 + 520], 4, 130)[:, :, 0:128],
                            in_=bview(ps[:, 0:512], 4, 128)), reads=[psb_], writes=[B_VA[blk]])
                return f
            ub = [None] * 8

            def cons_zb(base):
                def f(ch, ps, psb_):
                    ub[base + ch] = gate_chunk(ps, psb_, NT)
                return f
            for g in range(2):
                tproj('in', 0, 4096 + g * 512, xnT, TB, cons_qk(qn, g))
                tproj('in', 0, 5120 + g * 512, xnT, TB, cons_qk(kn, g, kof))
                tproj('in', 0, 6144 + g * 512, xnT, TB, cons_dv(g))
                fproj('in', 0, 7168 + g * 512, xnT, xb_, NT, cons_zb(g * 4))
            defer(st_k)

            def st_v(vof=vof):
                for tb in range(TB):
                    R.op('sp', lambda e, tb=tb: e.dma_start(out=dvout[row0 + tb * 128:row0 + (tb + 1) * 128, :], in_=vof[tb].f(0, 1024)),
                         reads=vof[tb].b, dma_key="vo%d" % (tb % 2))
                    vof[tb].free()
            defer(st_v)
            QT = []
            for h in range(8):
                if sample:
                    q = A.alloc(1)

                    def ev(tr, trb, q=q):
                        R.op('dve', lambda e: e.tensor_scalar(out=q.bf(0, NT), in0=tr[:, 0:NT], scalar1=col(GQ8), scalar2=None,
                                                              op0=ALU.mult), reads=[trb, B_C], writes=q.b)
                    transpose_blocks(qn, h * 128, TB, ev)
                    QT.append(q)
                else:
                    qz = [A.alloc(1), A.alloc(1)]
                    R.op('pool', lambda e, qz=qz: e.memset(qz[0].rows_bf(64, 128, 0, NT), 0.0), writes=qz[0].b)
                    R.op('pool', lambda e, qz=qz: e.memset(qz[1].rows_bf(0, 64, 0, NT), 0.0), writes=qz[1].b)

                    def ev(tr, trb, qz=qz):
                        for m in range(2):
                            r0, r1 = m * 64, m * 64 + 64
                            R.op('dve', lambda e, m=m, r0=r0, r1=r1: e.tensor_scalar(
                                out=qz[m].rows_bf(r0, r1, 0, NT), in0=tr[r0:r1, 0:NT], scalar1=DER[r0:r1, GQ8:GQ8 + 1],
                                scalar2=None, op0=ALU.mult), reads=[trb, B_C], writes=qz[m].b)
                    transpose_blocks(qn, h * 128, TB, ev)
                    QT.append(qz)
            for x_ in qn:
                x_.free()
            ktok0 = 1024 if sample else j * 512
            kbuf = (lambda h: B_KT[h][2]) if sample else (lambda h: B_KT[h][j])
            if not sample:
                for h in range(8):
                    def ev(tr, trb, h=h):
                        R.op('act', lambda e: e.activation(out=KT[:, h * SEQ + ktok0:h * SEQ + ktok0 + NT], in_=tr[:, 0:NT], func=AF.Copy,
                                                           scale=col(GK2)),
                             reads=[trb, B_C], writes=[kbuf(h)])
                    transpose_blocks(kn, h * 128, TB, ev)
                for x_ in kn:
                    x_.free()
            od = [A.alloc(2) for _ in range(TB)]
            set_wide(False)
            if not sample:
                diff_attn_prompt(j, QT, od)
            else:
                sample_diff_attn(QT, kn, vnew, od)
                for x_ in kn + vnew:
                    x_.free()
            set_wide(True)
            for q in QT:
                if sample:
                    q.free()
                else:
                    q[0].free()
                    q[1].free()
            odn = norm_rows_bf(od, TB, 8, 128)
            for x_ in od:
                x_.free()
            tgs = merge_gates(1, NT)
            yb = gated_y(odn, ub, TB, NT, col(GDS))
            merge_proj(1, yb, NT, hacc, tgs)
            for y in yb:
                y.free()

            hoist()
            qmn = [A.alloc(2) for _ in range(TB)]

            def cons_mq(half):
                def f(tb, ps, psb_):
                    r, rb = group_stats(ps[:, 0:512], psb_, 2, 256)
                    R.op('dve', lambda e: e.tensor_tensor(out=bview(qmn[tb].bf(half * 512, half * 512 + 512), 2, 256),
                                                          in0=bview(ps[:, 0:512], 2, 256), in1=bcast_last(r[:, 0:2], 256),
                                                          op=ALU.mult), reads=[psb_, rb], writes=qmn[tb].bb(half * 512, half * 512 + 512))
                return f
            for g in range(2):
                tproj('in', 0, 8192 + g * 512, xnT, TB, cons_mq(g))
            um = [None] * 8

            def cons_zm(base):
                def f(ch, ps, psb_):
                    um[base + ch] = gate_chunk(ps, psb_, NT)
                return f
            for g in range(2):
                fproj('in', 0, 9216 + g * 512, xnT, xb_, NT, cons_zm(g * 4))
            QM = []
            for ci in range(8):
                q = A.alloc(1)

                def ev(tr, trb, q=q, ci=ci):
                    R.op('dve', lambda e: e.tensor_scalar(out=q.bf(0, NT), in0=tr[:, 0:NT], scalar1=col(GMQ + ci % 2),
                                                          scalar2=None, op0=ALU.mult), reads=[trb, B_C], writes=q.b)
                transpose_blocks(qmn, ci * 128, TB, ev)
                QM.append(q)
            for x_ in qmn:
                x_.free()
            om = [A.alloc(2) for _ in range(TB)]
            set_wide(False)
            if not sample:
                mem_attn(NT, TB, QM, om, [(qb * 128, 128, qb, qb, 0) for qb in range(4)])
            else:
                for i in range(4):
                    sample_load_mem(i)
                    mem_attn(NT, TB, QM, om, [(i * 64, 64, i % 4, i // 2, (i % 2) * 64)])
            for q in QM:
                q.free()
            set_wide(True)
            tgs = merge_gates(2, NT)
            ym = gated_y(om, um, TB, NT, 0.5)
            merge_proj(2, ym, NT, hacc, tgs)
            for y in ym:
                y.free()
            for p in xnT:
                p.free()

            xres = []
            for tb in range(TB):
                xr = A.alloc(4)
                R.op('sp', lambda e, xr=xr, tb=tb: e.dma_start(out=xr.f(0, 1024), in_=src_rows(tb)), writes=xr.b,
                     dma_key="xr%d" % (tb % 2))
                xres.append(xr)

            def cons_out(half):
                def f(tb, ps, psb_):
                    xr = xres[tb]
                    R.op('dve', lambda e: e.scalar_tensor_tensor(out=xr.f(half * 512, half * 512 + 512), in0=ps[:, 0:512],
                                                                 scalar=0.5, in1=xr.f(half * 512, half * 512 + 512),
                                                                 op0=ALU.mult, op1=ALU.add),
                         reads=[psb_] + xr.bF(half * 512, half * 512 + 512), writes=xr.bF(half * 512, half * 512 + 512))
                return f
            for g in range(2):
                tproj('out', 0, g * 512, hacc, TB, cons_out(g))
            def st_y(xres=xres):
                for tb in range(TB):
                    R.op('sp', lambda e, tb=tb: e.dma_start(out=yout[row0 + tb * 128:row0 + (tb + 1) * 128, :], in_=xres[tb].f(0, 1024)),
                         reads=xres[tb].b, dma_key="yo%d" % (tb % 2))
                    xres[tb].free()
            defer(st_y)
            for p in hacc:
                p.free()

        def sample_diff_attn(QT, kn, vnew, od):
            NT = 256
            knT = [[None] * 8 for _ in range(2)]
            for tbp in range(2):
                for h in range(8):
                    p = A.alloc(1)

                    def ev(tr, trb, p=p):
                        R.op('act', lambda e: e.activation(out=p.bf(0, 128), in_=tr[:, 0:128], func=AF.Copy, scale=col(GK2)),
                             reads=[trb, B_C], writes=p.b)
                    transpose_blocks([kn[tbp]], h * 128, 1, ev)
                    knT[tbp][h] = p
            for i in range(4):
                tb, r0 = i // 2, (i % 2) * 64
                for blk in range(8):
                    kc_ = A.alloc(2)
                    R.op('pool', lambda e, kc_=kc_, blk=blk, i=i: e.dma_start(
                        out=kc_.bf(0, 1024), in_=cdk[i * 1024 + blk * 128:i * 1024 + (blk + 1) * 128, :]),
                        writes=kc_.b, dma_key="ck%d" % (blk % 2))
                    for half in range(2):
                        tr, trb = tr_next()
                        for hh in range(4):
                            h = half * 4 + hh
                            R.op('pe', lambda e, tr=tr, hh=hh, h=h, kc_=kc_: e.transpose(
                                tr[:, hh * 128:(hh + 1) * 128], kc_.bf(h * 128, h * 128 + 128), IDB[:]),
                                reads=kc_.b + [B_C], writes=[trb])
                        for hh in range(4):
                            h = half * 4 + hh
                            R.op('act', lambda e, tr=tr, hh=hh, h=h, blk=blk: e.activation(
                                out=KT[:, h * SEQ + blk * 128:h * SEQ + blk * 128 + 128], in_=tr[:, hh * 128:(hh + 1) * 128],
                                func=AF.Copy), reads=[trb], writes=[B_KT[h][blk // 4]])
                    kc_.free()
                    R.op('pool', lambda e, blk=blk, i=i: e.dma_start(
                        out=bview(VA[:, blk * 1040:(blk + 1) * 1040], 8, 130)[:, :, 0:128],
                        in_=cdv[i * 1024 + blk * 128:i * 1024 + (blk + 1) * 128, :].rearrange("p (h v) -> p h v", h=8)),
                        writes=[B_VA[blk]], dma_key="cv%d" % (blk % 2))
                for h in range(8):
                    R.op('act', lambda e, h=h, tb=tb: e.activation(out=KT[:, h * SEQ + 1024:h * SEQ + 1152],
                                                                   in_=knT[tb][h].bf(0, 128), func=AF.Copy),
                         reads=knT[tb][h].b, writes=[B_KT[h][2]])
                R.op('dve', lambda e, tb=tb: e.tensor_copy(out=bview(VA[:, 8 * 1040:9 * 1040], 8, 130)[:, :, 0:128],
                                                         in_=bview(vnew[tb].bf(0, 1024), 8, 128)),
                     reads=vnew[tb].b, writes=[B_VA[8]])
                o0s = {}

                def emit_S(h, cmap, kb, i=i, r0=r0):
                    p0, p1 = cmap * 64, cmap * 64 + 64
                    k0, k1 = (0, 128) if kb < 8 else (r0, r0 + 64)
                    ps, psb_ = mm_next()
                    R.op('pe', lambda e: e.matmul(
                        ps[k0:k1, 0:64], lhsT=KT[p0:p1, h * SEQ + kb * 128 + k0:h * SEQ + kb * 128 + k1],
                        rhs=QT[h].rows_bf(p0, p1, i * 64, i * 64 + 64), start=True, stop=True),
                        reads=[B_KT[h][kb // 4]] + QT[h].b, writes=[psb_])
                    E = A.alloc(1)
                    R.op('act', lambda e: e.activation(
                        out=E.rows_bf(k0, k1, 0, 64), in_=ps[k0:k1, 0:64], func=AF.Exp), reads=[psb_], writes=E.b)
                    return E

                def emit_PV(h, cmap, kb, E, i=i, r0=r0, tb=tb):
                    k0, k1 = (0, 128) if kb < 8 else (r0, r0 + 64)
                    if cmap == 0 and kb == 0:
                        o0s[h] = A.alloc(2)
                    pa, pab = acc(i)
                    R.op('pe', lambda e: e.matmul(
                        pa[r0:r0 + 64, 0:129], lhsT=E.rows_bf(k0, k1, 0, 64),
                        rhs=VA[k0:k1, kb * 1040 + h * 130:kb * 1040 + h * 130 + 129], start=(kb == 0), stop=(kb == 8)),
                        reads=E.b + [B_VA[kb]], writes=[pab])
                    E.free()
                    if kb == 8:
                        pv_finish_diff(cmap, [(i, tb)], (r0, r0 + 64), h, o0s[h], od)
                        if cmap == 1:
                            o0s.pop(h).free()

                pend = []
                for stp in [(h, cmap, kb) for h in range(8) for cmap in range(2) for kb in range(9)]:
                    E = emit_S(*stp)
                    pend.append(stp + (E,))
                    if len(pend) > PIPE_DEPTH:
                        emit_PV(*pend.pop(0))
                while pend:
                    emit_PV(*pend.pop(0))
            for tbp in range(2):
                for h in range(8):
                    knT[tbp][h].free()

        def sample_load_mem(i):
            for kb in range(2):
                kc_ = A.alloc(2)
                R.op('pool', lambda e, kc_=kc_, kb=kb: e.dma_start(out=kc_.bf(0, 1024),
                                                                in_=cmk[i * 256 + kb * 128:i * 256 + (kb + 1) * 128, :]),
                     writes=kc_.b, dma_key="cm%d" % kb)
                for half in range(2):
                    tr, trb = tr_next()
                    for cc in range(4):
                        ci = half * 4 + cc
                        R.op('pe', lambda e, tr=tr, cc=cc, ci=ci, kc_=kc_: e.transpose(
                            tr[:, cc * 128:(cc + 1) * 128], kc_.bf(ci * 128, ci * 128 + 128), IDB[:]),
                            reads=kc_.b + [B_C], writes=[trb])
                    for cc in range(4):
                        ci = half * 4 + cc
                        R.op('act', lambda e, tr=tr, cc=cc, ci=ci, kb=kb: e.activation(
                            out=KM[:, ci * 256 + kb * 128:ci * 256 + kb * 128 + 128], in_=tr[:, cc * 128:(cc + 1) * 128],
                            func=AF.Copy), reads=[trb], writes=[B_KM])
                kc_.free()
                R.op('pool', lambda e, kb=kb: e.dma_start(
                    out=bview(VM[:, kb * 1032:(kb + 1) * 1032], 4, 258)[:, :, 0:256],
                    in_=cmv[i * 256 + kb * 128:i * 256 + (kb + 1) * 128, :].rearrange("p (h v) -> p h v", h=4)),
                    writes=[B_VM[kb]], dma_key="cw%d" % kb)

        def mem_kv(b, xn_pre):
            TB = 2
            r0 = b * 256
            xnT = xstage_b(xn_pre, TB, P_GM)
            kn = [A.alloc(2) for _ in range(TB)]
            kof = [A.alloc(4) for _ in range(TB)]

            def cons_k(half):
                def f(tb, ps, psb_):
                    r, rb = group_stats(ps[:, 0:512], psb_, 2, 256)
                    ko = kof[tb]
                    c0, c1 = half * 512, half * 512 + 512
                    R.op('dve', lambda e: e.tensor_tensor(out=bview(ko.f(c0, c1), 2, 256), in0=bview(ps[:, 0:512], 2, 256),
                                                          in1=bcast_last(r[:, 0:2], 256), op=ALU.mult),
                         reads=[psb_, rb], writes=ko.bF(c0, c1))
                    R.op('pool', lambda e: e.tensor_tensor(out=bview(ko.f(c0, c1), 2, 256), in0=bview(ko.f(c0, c1), 2, 256),
                                                           in1=bcast_mid(CBC[:, 320:576], 2), op=ALU.mult),
                         reads=ko.bF(c0, c1) + [B_C], writes=ko.bF(c0, c1))
                    R.op('act', lambda e: e.activation(out=kn[tb].bf(c0, c1), in_=ko.f(c0, c1), func=AF.Copy),
                         reads=ko.bF(c0, c1), writes=kn[tb].bb(c0, c1))
                return f
            for g in range(2):
                tproj('mkv', 0, g * 512, xnT, TB, cons_k(g))
            def st_k(kof=kof):
                for tb in range(TB):
                    R.op('sp', lambda e, tb=tb: e.dma_start(out=mk_p[r0 + tb * 128:r0 + (tb + 1) * 128, :], in_=kof[tb].f(0, 1024)),
                         reads=kof[tb].b, dma_key="ko%d" % (tb % 2))
                    kof[tb].free()
            defer(st_k)
            for ci in range(8):
                def ev(tr, trb, ci=ci):
                    R.op('act', lambda e: e.activation(out=KM[:, ci * 256:ci * 256 + 256], in_=tr[:, 0:256], func=AF.Copy),
                         reads=[trb], writes=[B_KM])
                transpose_blocks(kn, ci * 128, TB, ev)
            for x_ in kn:
                x_.free()
            hoist()
            vof = [A.alloc(4) for _ in range(TB)]

            def cons_v(half):
                def f(tb, ps, psb_):
                    c0, c1 = half * 512, half * 512 + 512
                    R.op('act', lambda e: e.activation(out=vof[tb].f(c0, c1), in_=ps[:, 0:512], func=AF.Copy),
                         reads=[psb_], writes=vof[tb].bF(c0, c1))
                    if DBG_V == 1:
                        for g2 in range(2):
                            R.op('dve', lambda e, g2=g2: e.tensor_copy(
                                out=VM[:, tb * 1032 + half * 516 + g2 * 258:tb * 1032 + half * 516 + g2 * 258 + 256],
                                in_=ps[:, g2 * 256:(g2 + 1) * 256]), reads=[psb_], writes=[B_VM[tb]])
                    elif DBG_V == 2:
                        R.op('act', lambda e: e.activation(
                            out=bview(VM[:, tb * 1032 + half * 516:tb * 1032 + half * 516 + 516], 2, 258)[:, :, 0:256],
                            in_=bview(ps[:, 0:512], 2, 256), func=AF.Copy), reads=[psb_], writes=[B_VM[tb]])
                    else:
                        R.op('dve', lambda e: e.tensor_copy(
                            out=bview(VM[:, tb * 1032 + half * 516:tb * 1032 + half * 516 + 516], 2, 258)[:, :, 0:256],
                            in_=bview(ps[:, 0:512], 2, 256)), reads=[psb_], writes=[B_VM[tb]])
                return f
            for g in range(2):
                tproj('mkv', 0, 1024 + g * 512, xnT, TB, cons_v(g))
            def st_v(vof=vof):
                for tb in range(TB):
                    R.op('sp', lambda e, tb=tb: e.dma_start(out=mv_p[r0 + tb * 128:r0 + (tb + 1) * 128, :], in_=vof[tb].f(0, 1024)),
                         reads=vof[tb].b, dma_key="vo%d" % (tb % 2))
                    vof[tb].free()
            defer(st_v)
            for p in xnT:
                p.free()

        units = []
        for b in range(nb_run):
            units.append(('mem', b, None))
            for j in range(ntiles):
                units.append(('main', b, j))
        if do_sample:
            units.append(('sample', None, None))
        prepared = {}
        cur = {'k': 0}

        def prep(k):
            if k >= len(units) or k in prepared:
                return
            kind, b, j = units[k]
            if kind == 'mem':
                prepared[k] = xstage_a(lambda tb, b=b: mp[b * 256 + tb * 128:b * 256 + (tb + 1) * 128, :], 2)
            elif kind == 'main':
                r0_ = b * SEQ + j * 512
                prepared[k] = xstage_a(lambda tb, r0_=r0_: xp[r0_ + tb * 128:r0_ + (tb + 1) * 128, :], 4)
            else:
                prepared[k] = xstage_a(lambda tb: xsm[tb * 128:(tb + 1) * 128, :], 2)

        def hoist():
            prep(cur['k'] + 1)

        for k, (kind, b, j) in enumerate(units):
            cur['k'] = k
            prep(k)
            xn_pre = prepared.pop(k)
            if kind == 'mem':
                mem_kv(b, xn_pre)
            elif kind == 'main':
                main_tile(512, 4, False, b, j, xn_pre)
            else:
                main_tile(256, 2, True, None, None, xn_pre)
        flush_pending(True)
        assert ws_state['cur'] == len(plan)
        R.emit(nc, st)
    return nc


_CACHE = {}


def pack_inputs(x_prompt, x_sample, mem_prompt, cache_diff_k, cache_diff_v, cache_mem_k, cache_mem_v,
                state_hgrn, g_norm, w_in, hg_lb_logits, g_hg_out, g_dq, g_dk, lam_q1, lam_k1,
                lam_q2, lam_k2, g_dsub, g_mem, w_mkv, g_mq, g_mk, w_branch, w_out, n_cores=8):
    f32 = np.float32
    A_ = lambda a: np.ascontiguousarray(np.asarray(a, dtype=f32))
    pk = lambda v, n: A_(v).reshape(n, 128).T
    cpk = np.zeros((128, 40), f32)
    cpk[:, 0:8] = pk(g_norm[0], 8)
    cpk[:, 8:16] = pk(g_mem[0], 8)
    cpk[:, 16:24] = pk(hg_lb_logits[0], 8)
    cpk[:, 24:32] = pk(hg_lb_logits[1], 8)
    cpk[:, 32] = A_(g_hg_out[0])
    cpk[:, 33] = np.tile(A_(g_dq[0]), 2)
    cpk[:, 34] = np.tile(A_(g_dk[0]), 2)
    cpk[:, 35] = A_(g_dsub[0])
    cpk[:, 36:38] = pk(g_mq[0], 2)
    cpk[:, 38:40] = pk(g_mk[0], 2)
    cbc = np.zeros((128, 576), f32)
    cbc[:, 0:64] = A_(lam_q1[0])[None, :]
    cbc[:, 64:128] = A_(lam_k1[0])[None, :]
    cbc[:, 128:192] = A_(lam_q2[0])[None, :]
    cbc[:, 192:256] = A_(lam_k2[0])[None, :]
    cbc[:, 256:320] = A_(g_dk[0])[None, :]
    cbc[:, 320:576] = A_(g_mk[0])[None, :]
    cfix = np.zeros((128, 320), f32)
    cfix[:, 0:128] = np.eye(128, dtype=f32)
    s_ = np.arange(128)[:, None] % 64
    t_ = np.arange(64)[None, :]
    cfix[:, 128:192] = (s_ <= t_).astype(f32)
    cfix[:, 192:256] = 1.0
    cfix[:, 192] = 0.0
    cfix[:, 256] = 1.0
    w_in_ = A_(w_in[0])
    w_mkv_ = A_(w_mkv[0])
    w_br_ = A_(w_branch[0]).reshape(3072, 1024)
    w_out_ = A_(w_out[0])
    in_maps = []
    for c in range(n_cores):
        sl = slice(c * NB, (c + 1) * NB)
        in_maps.append({
            "xp": A_(x_prompt[sl]).reshape(NB * SEQ, 1024),
            "xsm": A_(x_sample[sl]).reshape(256, 1024),
            "mp": A_(mem_prompt[sl]).reshape(NB * 256, 1024),
            "cdk": A_(cache_diff_k[0, sl]).reshape(NB * 1024, 1024),
            "cdv": A_(cache_diff_v[0, sl]).reshape(NB * 1024, 1024),
            "cmk": A_(cache_mem_k[0, sl]).reshape(NB * 256, 1024),
            "cmv": A_(cache_mem_v[0, sl]).reshape(NB * 256, 1024),
            "st0": A_(state_hgrn[0, sl]),
            "w_in": w_in_, "w_mkv": w_mkv_, "w_br": w_br_, "w_out": w_out_,
            "cpk": cpk, "cbc": cbc, "cfix": cfix,
        })
    return in_maps


def kernel(**inputs):
    f32 = np.float32
    if 'nc' not in _CACHE:
        _CACHE['nc'] = build_program()
    nc = _CACHE['nc']
    in_maps = pack_inputs(**inputs)
    res = run_bass_kernel_spmd(nc, in_maps, core_ids=list(range(8))).results
    cat = lambda k: np.concatenate([np.asarray(r[k], dtype=f32) for r in res], axis=0)
    y_p = cat("y_p").reshape(32, SEQ, 1024)
    y_s = cat("y_s").reshape(32, 64, 1024)
    st_p = cat("st_p").reshape(1, 32, 8, 128, 128)
    st_s = cat("st_s").reshape(1, 32, 8, 128, 128)
    dk_p = cat("dk_p").reshape(1, 32, SEQ, 8, 2, 64)
    dv_p = cat("dv_p").reshape(1, 32, SEQ, 8, 128)
    dk_s = cat("dk_s").reshape(1, 32, 64, 8, 2, 64)
    dv_s = cat("dv_s").reshape(1, 32, 64, 8, 128)
    mk_p = cat("mk_p").reshape(1, 32, 256, 4, 256)
    mv_p = cat("mv_p").reshape(1, 32, 256, 4, 256)
    return (y_p, y_s, st_p, st_s, dk_p, dv_p, dk_s, dv_s, mk_p, mv_p)
```
